# Optimizing a Trainium2 kernel written in Bass

```python
import jax, jax.numpy as jnp
from jax import lax
import numpy as np

D_MODEL = 1024
BATCH = 32
SEQ = 2048
DEPTH = 2

HG_HEADS = 4
HG_KDIM = 128
HG_VDIM = 128
RET_HEADS = 4
RET_KDIM = 128
RET_VDIM = 256
MLA_HEADS = 4
MLA_Q_RANK = 256
MLA_KV_RANK = 128
MLA_NOPE = 128
MLA_ROPE = 64
MLA_VDIM = 128
N_BRANCH = 3
D_FF = -(-8 * D_MODEL // (3 * 256)) * 256
CHUNK = 64
Q_BLOCK = 128
ROPE_BASE = 10000.0
EPS = 1e-6
MASK_NEG = -1e30
EXP_CLIP = 60.0
IN_WIDTHS = (
    HG_HEADS * HG_KDIM, HG_HEADS * HG_KDIM, HG_HEADS * HG_VDIM, HG_HEADS * HG_VDIM,
    RET_HEADS * RET_KDIM, RET_HEADS * RET_KDIM, RET_HEADS * RET_VDIM, RET_HEADS * RET_VDIM,
    MLA_Q_RANK, MLA_KV_RANK, MLA_ROPE,
    N_BRANCH * D_MODEL,
)
N_IN = sum(IN_WIDTHS)

kernel_name = 'hybrid_hgrn2_retention_mla_gated_block'


def _rms_norm(x, w):
    xf = x.astype(jnp.float32)
    y = xf * lax.rsqrt(jnp.mean(xf * xf, axis=-1, keepdims=True) + EPS)
    return (y * w.astype(jnp.float32)).astype(x.dtype)


def _head_rms_norm(o, w):
    b, s, h, d = o.shape
    o = o * lax.rsqrt(jnp.mean(o * o, axis=-1, keepdims=True) + EPS)
    return o.reshape(b, s, h * d) * w.astype(jnp.float32)


def _heads(t, n):
    return t.reshape(t.shape[0], t.shape[1], n, -1)


def _rope(t, positions):
    half = t.shape[-1] // 2
    inv = ROPE_BASE ** (-jnp.arange(half, dtype=jnp.float32) / half)
    ang = positions.astype(jnp.float32)[..., None] * inv
    cos = jnp.cos(ang)[:, :, None, :]
    sin = jnp.sin(ang)[:, :, None, :]
    t1, t2 = t[..., :half], t[..., half:]
    return jnp.concatenate([t1 * cos - t2 * sin, t1 * sin + t2 * cos], axis=-1)


def _to_chunks(t):
    b, s, h, d = t.shape
    return t.reshape(b, s // CHUNK, CHUNK, h, d).transpose(1, 0, 3, 2, 4)


def _from_chunks(t):
    n, b, h, c, d = t.shape
    return t.transpose(1, 0, 3, 2, 4).reshape(b, n * c, h, d)


def _hgrn2_mix(q, k, v, log_f):
    b_, _, h_, kd = q.shape
    vd = v.shape[-1]
    qc, kc, vc = _to_chunks(q), _to_chunks(k), _to_chunks(v)
    cum = jnp.cumsum(_to_chunks(log_f), axis=3)
    causal = jnp.tril(jnp.ones((CHUNK, CHUNK), dtype=bool))[:, :, None]

    def step(state, inp):
        q_, k_, v_, c_ = inp
        o_inter = jnp.einsum('bhtk,bhkv->bhtv', q_ * jnp.exp(c_), state)
        diff = c_[:, :, :, None, :] - c_[:, :, None, :, :]
        decay = jnp.where(causal, jnp.exp(jnp.where(causal, diff, 0.0)), 0.0)
        attn = jnp.einsum('bhtsk,bhsk->bhts', decay * q_[:, :, :, None, :], k_)
        o = o_inter + jnp.einsum('bhts,bhsv->bhtv', attn, v_)
        c_last = c_[:, :, -1:, :]
        k_dec = k_ * jnp.exp(c_last - c_)
        state = state * jnp.exp(c_last[:, :, 0, :, None]) + jnp.einsum('bhsk,bhsv->bhkv', k_dec, v_)
        return state, o

    s0 = jnp.zeros((b_, h_, kd, vd), jnp.float32)
    _, o = lax.scan(step, s0, (qc, kc, vc, cum))
    return _from_chunks(o)


def _retention_mix(q, k, v, log_gamma):
    b_, _, h_, kd = q.shape
    vd = v.shape[-1]
    idx = jnp.arange(CHUNK, dtype=jnp.float32)
    causal = jnp.tril(jnp.ones((CHUNK, CHUNK), dtype=bool))[None]
    lg = log_gamma[:, None, None]
    dist = jnp.where(causal, (idx[:, None] - idx[None, :])[None], 0.0)
    intra = jnp.where(causal, jnp.exp(dist * lg), 0.0)
    q_decay = jnp.exp((idx + 1.0)[None, :] * log_gamma[:, None])[..., None]
    k_decay = jnp.exp((CHUNK - 1.0 - idx)[None, :] * log_gamma[:, None])[..., None]
    chunk_decay = jnp.exp(CHUNK * log_gamma)[:, None, None]

    def step(state, inp):
        q_, k_, v_ = inp
        attn = jnp.einsum('bhtd,bhsd->bhts', q_, k_) * intra
        o = jnp.einsum('bhts,bhsv->bhtv', attn, v_) + jnp.einsum('bhtd,bhdv->bhtv', q_ * q_decay, state)
        state = state * chunk_decay + jnp.einsum('bhsd,bhsv->bhdv', k_ * k_decay, v_)
        return state, o

    s0 = jnp.zeros((b_, h_, kd, vd), jnp.float32)
    _, o = lax.scan(step, s0, (_to_chunks(q), _to_chunks(k), _to_chunks(v)))
    return _from_chunks(o)


def _mla_attend(q, k, v):
    b_, s_, h_, dqk = q.shape
    nb = s_ // Q_BLOCK
    qb = q.reshape(b_, nb, Q_BLOCK, h_, dqk).transpose(1, 0, 2, 3, 4)
    kpos = jnp.arange(s_)
    scale = dqk ** -0.5

    def block(args):
        i, q_ = args
        qpos = i * Q_BLOCK + jnp.arange(Q_BLOCK)
        sc = jnp.einsum('bqhd,bkhd->bhqk', q_, k).astype(jnp.float32) * scale
        sc = jnp.where(kpos[None, :] <= qpos[:, None], sc, MASK_NEG)
        p = jax.nn.softmax(sc, axis=-1).astype(v.dtype)
        return jnp.einsum('bhqk,bkhv->bqhv', p, v)

    o = lax.map(block, (jnp.arange(nb), qb))
    return o.transpose(1, 0, 2, 3, 4).reshape(b_, s_, h_, v.shape[-1])


def setup_inputs(seed: int = 0) -> dict:
    key = jax.random.key(seed)
    ks = jax.random.split(key, 20)
    f32 = jnp.float32

    def nrm(k, shape, scale):
        return jax.random.normal(k, shape, f32) * scale

    def gain(k, shape):
        return 1.0 + 0.02 * jax.random.normal(k, shape, f32)

    x = jax.random.normal(ks[0], (BATCH, SEQ, D_MODEL), f32)
    positions = jnp.broadcast_to(jnp.arange(SEQ, dtype=jnp.int32), (BATCH, SEQ))
    return {
        'x': x,
        'positions': positions,
        'norm_mix_w': gain(ks[1], (DEPTH, D_MODEL)),
        'w_in': nrm(ks[2], (DEPTH, D_MODEL, N_IN), D_MODEL ** -0.5),
        'hg_lower_bounds': nrm(ks[3], (DEPTH, HG_HEADS * HG_KDIM), 0.5),
        'hg_norm_w': gain(ks[4], (DEPTH, HG_HEADS * HG_VDIM)),
        'ret_norm_w': gain(ks[5], (DEPTH, RET_HEADS * RET_VDIM)),
        'mla_q_norm_w': gain(ks[6], (DEPTH, MLA_Q_RANK)),
        'mla_w_uq': nrm(ks[7], (DEPTH, MLA_Q_RANK, MLA_HEADS * (MLA_NOPE + MLA_ROPE)), MLA_Q_RANK ** -0.5),
        'mla_kv_norm_w': gain(ks[8], (DEPTH, MLA_KV_RANK)),
        'mla_w_ukv': nrm(ks[9], (DEPTH, MLA_KV_RANK, MLA_HEADS * (MLA_NOPE + MLA_VDIM)), MLA_KV_RANK ** -0.5),
        'w_br_a': nrm(ks[10], (DEPTH, HG_HEADS * HG_VDIM, D_MODEL), (HG_HEADS * HG_VDIM) ** -0.5),
        'w_br_b': nrm(ks[11], (DEPTH, RET_HEADS * RET_VDIM, D_MODEL), (RET_HEADS * RET_VDIM) ** -0.5),
        'w_br_c': nrm(ks[12], (DEPTH, MLA_HEADS * MLA_VDIM, D_MODEL), (MLA_HEADS * MLA_VDIM) ** -0.5),
        'w_out': nrm(ks[13], (DEPTH, D_MODEL, D_MODEL), 0.5 * D_MODEL ** -0.5),
        'norm_ffn_w': gain(ks[14], (DEPTH, D_MODEL)),
        'w_ffn_in': nrm(ks[15], (DEPTH, D_MODEL, 2 * D_FF), D_MODEL ** -0.5),
        'w_ffn_out': nrm(ks[16], (DEPTH, D_FF, D_MODEL), 0.5 * D_FF ** -0.5),
        'final_norm_w': gain(ks[17], (D_MODEL,)),
    }


def reference(x, positions, norm_mix_w, w_in, hg_lower_bounds, hg_norm_w, ret_norm_w,
              mla_q_norm_w, mla_w_uq, mla_kv_norm_w, mla_w_ukv, w_br_a, w_br_b, w_br_c,
              w_out, norm_ffn_w, w_ffn_in, w_ffn_out, final_norm_w):
    f32 = jnp.float32
    b_, s_, d_ = x.shape
    split_points = [int(p) for p in np.cumsum(IN_WIDTHS)[:-1]]
    lb_p = jax.nn.softmax(hg_lower_bounds.astype(f32), axis=0)
    lb_all = jnp.cumsum(lb_p, axis=0) - lb_p[0]
    log_gamma = jnp.log1p(-jnp.exp2(-5.0 - jnp.arange(RET_HEADS, dtype=f32)))

    for l in range(DEPTH):
        h = _rms_norm(x, norm_mix_w[l])
        proj = (h @ w_in[l]).astype(f32)
        hq, hf, hi, hgate, rq, rk, rv, rgate, cq, ckv, kr, mg = jnp.split(proj, split_points, axis=-1)

        lb = lb_all[l]
        log_f = jax.nn.log_sigmoid(hf) + jnp.log1p(lb * jnp.exp(jnp.minimum(-hf, EXP_CLIP)))
        k_hg = (1.0 - lb) * jax.nn.sigmoid(-hf)
        o_hg = _hgrn2_mix(_heads(jax.nn.silu(hq), HG_HEADS), _heads(k_hg, HG_HEADS),
                          _heads(hi, HG_HEADS), _heads(log_f, HG_HEADS))
        y_a = _head_rms_norm(o_hg, hg_norm_w[l]) * jax.nn.silu(hgate)

        q_r = _rope(_heads(rq, RET_HEADS), positions)
        k_r = _rope(_heads(rk, RET_HEADS), positions) * (RET_KDIM ** -0.5)
        o_r = _retention_mix(q_r, k_r, _heads(rv, RET_HEADS), log_gamma)
        y_b = _head_rms_norm(o_r, ret_norm_w[l]) * jax.nn.silu(rgate)

        q_m = (_rms_norm(cq, mla_q_norm_w[l]) @ mla_w_uq[l]).astype(f32)
        q_m = q_m.reshape(b_, s_, MLA_HEADS, MLA_NOPE + MLA_ROPE)
        q_nope, q_pe = q_m[..., :MLA_NOPE], _rope(q_m[..., MLA_NOPE:], positions)
        kv = (_rms_norm(ckv, mla_kv_norm_w[l]) @ mla_w_ukv[l]).astype(f32)
        kv = kv.reshape(b_, s_, MLA_HEADS, MLA_NOPE + MLA_VDIM)
        k_nope, v_m = kv[..., :MLA_NOPE], kv[..., MLA_NOPE:]
        k_pe = _rope(kr[:, :, None, :], positions)
        q_full = jnp.concatenate([q_nope, q_pe], axis=-1)
        k_full = jnp.concatenate([k_nope, jnp.broadcast_to(k_pe, (b_, s_, MLA_HEADS, MLA_ROPE))], axis=-1)
        y_c = _mla_attend(q_full, k_full, v_m).reshape(b_, s_, MLA_HEADS * MLA_VDIM)

        gates = jax.nn.sigmoid(mg).reshape(b_, s_, N_BRANCH, d_)
        merged = (gates[:, :, 0, :] * (y_a @ w_br_a[l])
                  + gates[:, :, 1, :] * (y_b @ w_br_b[l])
                  + gates[:, :, 2, :] * (y_c @ w_br_c[l]))
        x = x + (merged.astype(x.dtype) @ w_out[l])

        h2 = _rms_norm(x, norm_ffn_w[l])
        gu = h2 @ w_ffn_in[l]
        x = x + (jax.nn.silu(gu[..., :D_FF]) * gu[..., D_FF:]) @ w_ffn_out[l]

    return _rms_norm(x, final_norm_w)
```

```python
import numpy as np
import ml_dtypes
from contextlib import ExitStack
import concourse.bass as bass
import concourse.mybir as mybir
from concourse.bass_utils import run_bass_kernel_spmd

F32, BF16, I32 = mybir.dt.float32, mybir.dt.bfloat16, mybir.dt.int32
AF = mybir.ActivationFunctionType
ALU = mybir.AluOpType

D = 1024
SEQ = 2048
NT = 16
TT = 512
NIN = 8640
DFF = 2816
EPS = 1e-6
S192 = 192.0 ** -0.5
LOG_GAMMA = [float(np.log1p(-(2.0 ** (-5.0 - h)))) for h in range(4)]
SLOT = 5120

BLK = ["hi", "hgate", "hq", "hf", "rq", "rk", "rv0", "rv1", "rg0", "rg1", "c", "mla"] + \
      [f"M{j}" for j in range(8)] + ["wout0", "wout1"] + [f"ffin{j}" for j in range(11)] + \
      [f"ffout{i}" for i in range(6)]
BIDX = {n: i for i, n in enumerate(BLK)}
NBLK = len(BLK)
WIN_C0 = {"hq": 0, "hf": 512, "hi": 1024, "hgate": 1536, "rq": 2048, "rk": 2560, "rv0": 3072,
          "rv1": 3584, "rg0": 4096, "rg1": 4608, "c": 5120}
FFOUT_PARTS = [(n, kc0, nk) for n in range(2) for (kc0, nk) in ((0, 8), (8, 8), (16, 6))]


def blk_size(name):
    if name == "c":
        return 8 * 448
    if name == "mla":
        return 3072
    if name.startswith("M"):
        return 5120
    if name.startswith("ffout"):
        return FFOUT_PARTS[int(name[5:])][2] * 512
    return 4096


class Buf:
    __slots__ = ("name", "w", "r", "psum")

    def __init__(self, name, psum=False):
        self.name = name
        self.w = None
        self.r = {}
        self.psum = psum


def flat(x):
    out = []

    def rec(y):
        if y is None:
            return
        if isinstance(y, Buf):
            out.append(y)
        elif hasattr(y, "bufs"):
            out.extend(y.bufs)
        else:
            for z in y:
                rec(z)
    rec(x)
    return out


class Eng:
    def __init__(self, h, key, is_pe=False):
        self.h = h
        self.key = key
        self.is_pe = is_pe
        self.sem = None
        self.cnt = 0
        self.seen = {}
        self.pend = False


class Stream:
    def __init__(self, kk, name):
        self.sem = kk.newsem(name)
        self.cnt = 0
        self.key = ("dma", name)
        self.gb = []


class K:
    SEM_LIMIT = 24000

    def __init__(self, nc, st):
        self.nc = nc
        self.st = st
        self.nsem = 0
        self.E = {}
        for key, h in (("pe", nc.tensor), ("act", nc.scalar), ("dve", nc.vector),
                       ("pool", nc.gpsimd), ("sp", nc.sync)):
            e = Eng(h, key, key == "pe")
            e.sem = self.newsem(key)
            self.E[key] = e
        self.ninst = 0

    def newsem(self, name):
        self.nsem += 1
        return self.st.enter_context(self.nc.semaphore(f"s{self.nsem}_{name}"))

    def _wait(self, E, r, w, skip_key=None):
        deps = {}

        def add(t):
            if t is None:
                return
            sem, val, key = t
            if key == E.key and E.is_pe:
                return
            if skip_key is not None and key == skip_key:
                return
            k = id(sem)
            if k not in deps or deps[k][1] < val:
                deps[k] = (sem, val)
        for b in r:
            add(b.w)
        for b in w:
            add(b.w)
            for k, t in b.r.items():
                add(t)
        for k, (sem, val) in deps.items():
            if E.seen.get(k, 0) < val:
                E.h.wait_ge(sem, val)
                E.seen[k] = val

    def op(self, eng, fn, r=(), w=(), inc=True):
        E = self.E[eng]
        r = flat(r)
        w = flat(w)
        w = w + [b for b in r if b.psum and b not in w]
        r = [b for b in r if not b.psum]
        self._wait(E, r, w)
        ins = fn(E.h)
        self.ninst += 1
        if inc:
            if E.cnt >= self.SEM_LIMIT and not E.pend:
                E.sem = self.newsem(E.key)
                E.cnt = 0
            E.cnt += 1
            ins.then_inc(E.sem, 1)
            tick = (E.sem, E.cnt, E.key)
            E.pend = False
        else:
            assert E.is_pe
            tick = (E.sem, E.cnt + 1, E.key)
            E.pend = True
        for b in r:
            b.r[E.key] = tick
        for b in w:
            b.w = tick
            b.r = {}
        return ins

    def dma(self, q, stream, out, in_, r=(), w=(), skip_same=False, **kw):
        E = self.E[q]
        r = flat(r)
        w = flat(w)
        self._wait(E, r, w, skip_key=stream.key if skip_same else None)
        ins = E.h.dma_start(out=out, in_=in_, **kw)
        self.ninst += 1
        stream.cnt += 16
        ins.then_inc(stream.sem, 16)
        tick = (stream.sem, stream.cnt, stream.key)
        if not skip_same:
            stream.gb = []
        for b in w:
            if b not in stream.gb:
                stream.gb.append(b)
        for b in r:
            b.r[stream.key] = tick
        for b in stream.gb:
            b.w = tick
            b.r = {}
        return ins

    def wait_all(self, eng, bufs):
        E = self.E[eng]
        self._wait(E, flat(bufs), flat(bufs))


class PT:
    def __init__(self, t, bufs):
        self.t = t
        self.bufs = bufs


class Tmp:
    def __init__(self, ap, bufs):
        self.ap = ap
        self.bufs = bufs


class Arena:
    PAGE = 512

    def __init__(self, nc, st, name, nelem_bf16):
        self.t = st.enter_context(nc.sbuf_tensor(name, [128, nelem_bf16], BF16))
        self.n = nelem_bf16
        self.pages = [Buf(f"{name}_p{i}") for i in range((nelem_bf16 + self.PAGE - 1) // self.PAGE)]
        self.off = 0
        self.hi = 0

    def reset(self, off=0):
        self.off = off

    def alloc(self, free_shape, dt, parts=128, align=False):
        if align:
            self.off = (self.off + self.PAGE - 1) // self.PAGE * self.PAGE
        n = int(np.prod(free_shape))
        nb = n * (2 if dt == F32 else 1)
        if dt == F32 and self.off % 2:
            self.off += 1
        o = self.off
        assert o + nb <= self.n, f"arena overflow {o + nb} > {self.n}"
        self.off += nb
        self.hi = max(self.hi, self.off)
        ap = self.t[0:parts, o:o + nb]
        if dt == F32:
            ap = ap.bitcast(F32)
        if len(free_shape) == 2:
            ap = ap.rearrange("p (a b) -> p a b", b=free_shape[1])
        elif len(free_shape) == 3:
            ap = ap.rearrange("p (a b c) -> p a b c", b=free_shape[1], c=free_shape[2])
        bufs = self.pages[o // self.PAGE:(o + nb - 1) // self.PAGE + 1]
        return Tmp(ap, bufs)


class _Stop(Exception):
    pass


def build(nseq=4, nlayers=2, dbg=False, stop=None):
    nc = bass.Bass("TRN2", target_bir_lowering=False)
    dr = {}

    def din(name, shape, dt=F32):
        dr[name] = nc.dram_tensor(name, list(shape), dt, kind="ExternalInput").ap()
        return dr[name]

    x_d = din("x", [nseq, SEQ, D])
    pos_d = din("positions", [nseq, SEQ], I32)
    nmw_d = din("norm_mix_w", [2, D])
    win_d = din("w_in", [2, D, NIN])
    hlb_d = din("hg_lower_bounds", [2, 512])
    hnw_d = din("hg_norm_w", [2, 512])
    rnw_d = din("ret_norm_w", [2, 1024])
    qnw_d = din("mla_q_norm_w", [2, 256])
    wuq_d = din("mla_w_uq", [2, 256, 768])
    kvnw_d = din("mla_kv_norm_w", [2, 128])
    wukv_d = din("mla_w_ukv", [2, 128, 1024])
    wba_d = din("w_br_a", [2, 512, D])
    wbb_d = din("w_br_b", [2, 1024, D])
    wbc_d = din("w_br_c", [2, 512, D])
    wout_d = din("w_out", [2, D, D])
    nfw_d = din("norm_ffn_w", [2, D])
    wfi_d = din("w_ffn_in", [2, D, 2 * DFF])
    wfo_d = din("w_ffn_out", [2, DFF, D])
    fnw_d = din("final_norm_w", [D])
    cident_d = din("c_ident", [128, 128], BF16)
    chgmask_d = din("c_hgmask", [128, 64], BF16)
    ccausal_d = din("c_causal", [128, 128], BF16)
    cones_d = din("c_ones", [128, 128], BF16)
    creset_d = din("c_reset", [128, 512])
    cdmask_d = din("c_dmask", [128, 4, 128])
    cqdec_d = din("c_qdec", [128, 4, 128])
    ckdec_d = din("c_kdec", [128, 4])
    cinvr_d = din("c_invr", [128, 64])
    cinvm_d = din("c_invm", [128, 32])
    out_d = nc.dram_tensor("out", [nseq, SEQ, D], F32, kind="ExternalOutput").ap()
    wscr = nc.dram_tensor("wscr", [2, NBLK, 128, SLOT], BF16, kind="Internal").ap()

    with ExitStack() as st:
        kk = K(nc, st)

        def sb(name, shape, dt, nb=1):
            t = st.enter_context(nc.sbuf_tensor(name, list(shape), dt))
            return PT(t, [Buf(f"{name}{i}") for i in range(nb)])

        def psb(name, shape, dt):
            t = st.enter_context(nc.psum_tensor(name, list(shape), dt))
            return PT(t, [Buf(name, psum=True)])

        xres = sb("xres", [128, NT, D], F32, NT)
        NSLOT = 3
        wsl = [sb(f"wsl{i}", [128, SLOT], BF16) for i in range(NSLOT)]
        wst = [Stream(kk, f"w{i}") for i in range(NSLOT)]
        hT = sb("hT", [128, 8, TT], BF16)
        kvnT = sb("kvnT", [128, SEQ], BF16, NT)
        kvtok = sb("kvtok", [128, NT, 128], BF16, NT)
        kpeT = sb("kpeT", [64, SEQ], BF16, NT)
        big = sb("big", [128, 24, TT], BF16, 4)
        yA, yB, yC, mTb = big.bufs
        ropeR = sb("ropeR", [128, NT, 2, 64], BF16)
        ropeM = sb("ropeM", [128, NT, 2, 32], BF16)
        shg_f = sb("shg_f", [128, 4, 128], F32, 4)
        shg_b = sb("shg_b", [128, 4, 128], BF16, 4)
        sr_f = sb("sr_f", [128, 4, 256], F32, 4)
        sr_b = sb("sr_b", [128, 4, 256], BF16, 4)
        ident = sb("ident", [128, 128], BF16)
        hgmask = sb("hgmask", [128, 64], BF16)
        causal = sb("causal", [128, 128], BF16)
        ones = sb("ones", [128, 128], BF16)
        resetc = sb("resetc", [128, 512], F32)
        dmask = sb("dmask", [128, 4, 128], F32)
        qdec = sb("qdec", [128, 4, 128], F32)
        kdecc = sb("kdecc", [128, 4], F32)
        invr = sb("invr", [128, 64], F32)
        invm = sb("invm", [128, 32], F32)
        gains = sb("gains", [128, 2, 32], F32)
        lbt = sb("lbt", [128, 2, 2, 4], F32)
        mhalf = sb("mhalf", [128, 8], F32)
        cols = sb("cols", [128, 64], F32)
        colbufs = [Buf(f"col{i}") for i in range(16)]
        colrr = [0]

        def newcol(n=4):
            i = colrr[0] % 16
            colrr[0] += 1
            return Tmp(cols.t[:, i * 4:i * 4 + n], [colbufs[i]])

        ar = Arena(nc, st, "arena", 22 * 1024)

        psT = [psb(f"psT{i}", [128, 1024], BF16) for i in range(2)]
        pmm = [psb(f"pmm{i}", [128, 512], F32) for i in range(3)]
        pacc = [psb(f"pacc{i}", [128, 512], F32) for i in range(2)]
        pS = psb("pS", [128, 512], F32)
        rr = {"T": 0, "mm": 0, "acc": 0, "wide": 0}
        pwide = pmm + pacc

        def nxt(kind):
            lst = {"T": psT, "mm": pmm, "acc": pacc, "wide": pwide}[kind]
            p = lst[rr[kind] % len(lst)]
            rr[kind] += 1
            return p

        def chk(name):
            if stop == name:
                if name in ("mla", "merge", "wout"):
                    kk.op("act", lambda h: h.activation(out=xres.t[:, 0:12, :].rearrange("p a b -> p (a b)"), in_=big.t[:].rearrange("p a b -> p (a b)"), func=AF.Copy), r=big.bufs, w=xres.bufs[0:12])
                if name == "rope":
                    kk.op("act", lambda h: h.activation(out=xres.t[:, 12:14, :].rearrange("p a b -> p (a b)"), in_=ropeR.t[:].rearrange("p a b c -> p (a b c)"), func=AF.Copy), r=ropeR, w=xres.bufs[12:14])
                    kk.op("act", lambda h: h.activation(out=xres.t[:, 14, :], in_=ropeM.t[:].rearrange("p a b c -> p (a b c)"), func=AF.Copy), r=ropeM, w=xres.bufs[14])
                raise _Stop()

        cst = Stream(kk, "const")
        xst = Stream(kk, "xload")
        osts = [Stream(kk, f"ostore{i}") for i in range(2)]
        pst_lds = [Stream(kk, f"pre_ld{i}") for i in range(2)]
        pst_sts = [Stream(kk, f"pre_st{i}") for i in range(2)]
        posst = Stream(kk, "posld")
        fnwst = Stream(kk, "fnwld")

        for t_, d_ in ((ident, cident_d), (hgmask, chgmask_d), (causal, ccausal_d), (ones, cones_d),
                       (resetc, creset_d), (dmask, cdmask_d), (qdec, cqdec_d), (kdecc, ckdec_d),
                       (invr, cinvr_d), (invm, cinvm_d)):
            kk.dma("sp", cst, t_.t[:], d_[:], w=t_, skip_same=(cst.cnt > 0))
        for l in range(2):
            for (src, n, c0) in ((nmw_d, 8, 0), (nfw_d, 8, 8), (hnw_d, 4, 16), (rnw_d, 8, 20),
                                 (qnw_d, 2, 28), (kvnw_d, 1, 30)):
                for k_ in range(n):
                    kk.dma("sp", cst, gains.t[:, l, c0 + k_:c0 + k_ + 1], src[l, k_ * 128:(k_ + 1) * 128].rearrange("(p o) -> p o", o=1),
                           w=gains, skip_same=True)
        kk.op("pool", lambda h: h.memset(mhalf.t[:], -0.5), w=mhalf)
        lbraw = ar.alloc([2, 4], F32)
        for l in range(2):
            for k_ in range(4):
                kk.dma("sp", cst, lbraw.ap[:, l, k_:k_ + 1], hlb_d[l, k_ * 128:(k_ + 1) * 128].rearrange("(p o) -> p o", o=1), w=lbraw,
                       skip_same=True)
        lbe = ar.alloc([2, 4], F32)
        lbs = ar.alloc([4], F32)
        lbp = ar.alloc([2, 4], F32)
        kk.op("act", lambda h: h.activation(out=lbe.ap, in_=lbraw.ap, func=AF.Exp), r=lbraw, w=lbe)
        kk.op("dve", lambda h: h.tensor_tensor(out=lbs.ap, in0=lbe.ap[:, 0, :], in1=lbe.ap[:, 1, :], op=ALU.add), r=lbe, w=lbs)
        kk.op("dve", lambda h: h.reciprocal(out=lbs.ap, in_=lbs.ap), r=lbs, w=lbs)
        for l in range(2):
            kk.op("dve", lambda h, l=l: h.tensor_tensor(out=lbp.ap[:, l, :], in0=lbe.ap[:, l, :], in1=lbs.ap, op=ALU.mult), r=[lbe, lbs], w=lbp)
        kk.op("dve", lambda h: h.tensor_tensor(out=lbt.t[:, 0, 0, :], in0=lbp.ap[:, 0, :], in1=lbp.ap[:, 0, :], op=ALU.subtract), r=lbp, w=lbt)
        cum1 = ar.alloc([4], F32)
        kk.op("dve", lambda h: h.tensor_tensor(out=cum1.ap, in0=lbp.ap[:, 0, :], in1=lbp.ap[:, 1, :], op=ALU.add), r=lbp, w=cum1)
        kk.op("dve", lambda h: h.tensor_tensor(out=lbt.t[:, 1, 0, :], in0=cum1.ap, in1=lbp.ap[:, 0, :], op=ALU.subtract), r=[cum1, lbp], w=lbt)
        for l in range(2):
            kk.op("dve", lambda h, l=l: h.tensor_scalar(out=lbt.t[:, l, 1, :], in0=lbt.t[:, l, 0, :], scalar1=-1.0, scalar2=1.0, op0=ALU.mult, op1=ALU.add), r=lbt, w=lbt)

        chk("consts")
        stg_f = [Tmp(xres.t[:, 5 * i:5 * i + 5, :].rearrange("p a b -> p (a b)"), xres.bufs[5 * i:5 * i + 5]) for i in range(2)]
        stg_b = [Tmp(xres.t[:, 10 + 3 * i:13 + 3 * i, :].rearrange("p a b -> p (a b)").bitcast(BF16)[:, 0:SLOT], xres.bufs[10 + 3 * i:13 + 3 * i]) for i in range(2)]
        cast_rr = [0]

        def cast(dst, src, gain):
            e = ("dve", "pool", "dve")[cast_rr[0] % 3]
            cast_rr[0] += 1
            if gain is None:
                if e == "act":
                    return ("act", lambda h: h.activation(out=dst, in_=src, func=AF.Copy))
                return (e, lambda h: h.tensor_copy(out=dst, in_=src))
            if e == "act":
                return ("act", lambda h: h.activation(out=dst, in_=src, func=AF.Copy, scale=gain))
            return (e, lambda h: h.tensor_scalar(out=dst, in0=src, scalar1=gain, scalar2=None, op0=ALU.mult))

        pre_i = [0]

        def prepass_block(l, name):
            i = pre_i[0] % 2
            pre_i[0] += 1
            sf, sbf = stg_f[i], stg_b[i]
            pst_ld, pst_st = pst_lds[i], pst_sts[i]
            pieces = []

            def g(c):
                return gains.t[:, l, c:c + 1]
            if name in WIN_C0:
                c0 = WIN_C0[name]
                ncol = 448 if name == "c" else 512
                kk.dma("sp", pst_ld, sf.ap[:, 0:8 * ncol].rearrange("p (k c) -> p k c", c=ncol),
                       win_d[l, :, c0:c0 + ncol].rearrange("(k p) c -> p k c", p=128), w=sf)
                for kc in range(8):
                    pieces.append((kc * ncol, ncol, kc * ncol, g(kc)))
            elif name == "mla":
                kk.dma("sp", pst_ld, sf.ap[:, 0:1536].rearrange("p (k c) -> p k c", c=768),
                       wuq_d[l].rearrange("(k p) c -> p k c", p=128), w=sf)
                kk.dma("sp", pst_ld, sf.ap[:, 1536:2560], wukv_d[l], w=sf, skip_same=True)
                for kc in range(2):
                    pieces.append((kc * 768, 768, kc * 768, g(28 + kc)))
                pieces.append((1536, 1024, 1536, g(30)))
            elif name.startswith("M"):
                j = int(name[1:])
                for b_ in range(3):
                    cg = 5568 + b_ * 1024 + j * 128
                    kk.dma("sp", pst_ld, sf.ap[:, 0:3072].rearrange("p (k b c) -> p k b c", b=3, c=128)[:, :, b_, :],
                           win_d[l, :, cg:cg + 128].rearrange("(k p) c -> p k c", p=128), w=sf, skip_same=(b_ > 0))
                kk.dma("sp", pst_ld, sf.ap[:, 3072:3584].rearrange("p (k c) -> p k c", c=128),
                       wba_d[l, :, j * 128:(j + 1) * 128].rearrange("(k p) c -> p k c", p=128), w=sf, skip_same=True)
                kk.dma("sp", pst_ld, sf.ap[:, 3584:4608].rearrange("p (k c) -> p k c", c=128),
                       wbb_d[l, :, j * 128:(j + 1) * 128].rearrange("(k p) c -> p k c", p=128), w=sf, skip_same=True)
                kk.dma("sp", pst_ld, sf.ap[:, 4608:5120].rearrange("p (k c) -> p k c", c=128),
                       wbc_d[l, :, j * 128:(j + 1) * 128].rearrange("(k p) c -> p k c", p=128), w=sf, skip_same=True)
                for kc in range(8):
                    pieces.append((kc * 384, 384, kc * 384, g(kc)))
                for kc in range(4):
                    pieces.append((3072 + kc * 128, 128, 3072 + kc * 128, g(16 + kc)))
                for kc in range(8):
                    pieces.append((3584 + kc * 128, 128, 3584 + kc * 128, g(20 + kc)))
                pieces.append((4608, 512, 4608, None))
            elif name.startswith("wout"):
                n = int(name[4:])
                kk.dma("sp", pst_ld, sf.ap[:, 0:4096].rearrange("p (k c) -> p k c", c=512),
                       wout_d[l, :, n * 512:(n + 1) * 512].rearrange("(k p) c -> p k c", p=128), w=sf)
                pieces.append((0, 4096, 0, None))
            elif name.startswith("ffin"):
                jb = int(name[4:])
                v = sf.ap[:, 0:4096].rearrange("p (k c) -> p k c", c=512)
                kk.dma("sp", pst_ld, v[:, :, 0:256],
                       wfi_d[l, :, jb * 256:(jb + 1) * 256].rearrange("(k p) c -> p k c", p=128), w=sf)
                kk.dma("sp", pst_ld, v[:, :, 256:512],
                       wfi_d[l, :, DFF + jb * 256:DFF + (jb + 1) * 256].rearrange("(k p) c -> p k c", p=128), w=sf, skip_same=True)
                for kc in range(8):
                    pieces.append((kc * 512, 512, kc * 512, g(8 + kc)))
            elif name.startswith("ffout"):
                n, kc0, nk = FFOUT_PARTS[int(name[5:])]
                kk.dma("sp", pst_ld, sf.ap[:, 0:nk * 512].rearrange("p (k c) -> p k c", c=512),
                       wfo_d[l, kc0 * 128:(kc0 + nk) * 128, n * 512:(n + 1) * 512].rearrange("(k p) c -> p k c", p=128), w=sf)
                pieces.append((0, nk * 512, 0, None))
            return (l, name, sf, sbf, pst_st, pieces)

        def prepass_finish(l, name, sf, sbf, pst_st, pieces):
            for (do, n, so, gn) in pieces:
                e, fn = cast(sbf.ap[:, do:do + n], sf.ap[:, so:so + n], gn)
                kk.op(e, fn, r=[sf, gains], w=sbf)
            if name == "mla":
                p = nxt("T")
                for hh in range(4):
                    kk.op("pe", lambda h, hh=hh: h.transpose(out=p.t[:, hh * 128:(hh + 1) * 128], in_=sbf.ap[:, 1536 + hh * 256:1536 + hh * 256 + 128], identity=ident.t[:]),
                          r=[sbf, ident], w=p, inc=(hh == 3))
                kk.op("dve", lambda h: h.tensor_copy(out=sbf.ap[:, 2560:3072], in_=p.t[:, 0:512]), r=p, w=sbf)
            sz = blk_size(name)
            kk.dma("act", pst_st, wscr[l, BIDX[name], :, 0:sz], sbf.ap[:, 0:sz], r=sbf)

        pend = None
        for l in range(nlayers):
            for name in BLK:
                cur = prepass_block(l, name)
                if pend is not None:
                    prepass_finish(*pend)
                pend = cur
        prepass_finish(*pend)
        for e in ("sp", "pool", "act"):
            kk.wait_all(e, [stg_f, stg_b])

        wq = []
        for s_ in range(nseq):
            for l in range(nlayers):
                for T in range(4):
                    for name in BLK:
                        wq.append((l, name))
        wstate = {"issued": 0, "used": 0}

        def w_issue(upto):
            while wstate["issued"] < min(upto, len(wq)):
                i = wstate["issued"]
                l, name = wq[i]
                sl = i % NSLOT
                sz = blk_size(name)
                kk.dma("sp", wst[sl], wsl[sl].t[:, 0:sz], wscr[l, BIDX[name], :, 0:sz], w=wsl[sl])
                wstate["issued"] += 1

        def wget(name, keep=0):
            i = wstate["used"]
            assert wq[i][1] == name, (wq[i], name)
            assert keep < NSLOT
            w_issue(i - keep + NSLOT)
            wstate["used"] += 1
            return wsl[i % NSLOT]

        def rstd_cols(ss, n, inv_d):
            t1 = newcol(n)
            kk.op("pool", lambda h: h.tensor_scalar(out=t1.ap, in0=ss.ap, scalar1=inv_d, scalar2=EPS, op0=ALU.mult, op1=ALU.add), r=ss, w=t1)
            t2 = newcol(n)
            kk.op("pool", lambda h: h.tensor_tensor(out=t2.ap, in0=t1.ap, in1=mhalf.t[:, 0:n], op=ALU.pow), r=[t1, mhalf], w=t2)
            return t2

        def norm_to_hT(T):
            ar.reset(0)
            junk = ar.alloc([D], BF16)
            hns = [ar.alloc([D], BF16) for _ in range(4)]
            ss = newcol(4)
            for j in range(4):
                t = 4 * T + j
                kk.op("act", lambda h, t=t, j=j: h.activation(out=junk.ap, in_=xres.t[:, t, :], func=AF.Square, accum_out=ss.ap[:, j:j + 1]), r=xres.bufs[t], w=[junk, ss])
            rs = rstd_cols(ss, 4, 1.0 / D)
            for j in range(4):
                t = 4 * T + j
                kk.op("dve", lambda h, t=t, j=j: h.tensor_scalar(out=hns[j].ap, in0=xres.t[:, t, :], scalar1=rs.ap[:, j:j + 1], scalar2=None, op0=ALU.mult), r=[xres.bufs[t], rs], w=hns[j])
            for j in range(4):
                p = nxt("T")
                for kc in range(8):
                    kk.op("pe", lambda h, kc=kc, j=j, p=p: h.transpose(out=p.t[:, kc * 128:(kc + 1) * 128], in_=hns[j].ap[:, kc * 128:(kc + 1) * 128], identity=ident.t[:]),
                          r=[hns[j], ident], w=p, inc=(kc == 7))
                kk.op("act", lambda h, j=j, p=p: h.activation(out=hT.t[:, :, j * 128:(j + 1) * 128], in_=p.t[:].rearrange("p (k c) -> p k c", c=128), func=AF.Copy), r=p, w=hT)

        def proj_tok(wap, ncol, j, p, rhsT=None):
            for kc in range(8):
                kk.op("pe", lambda h, kc=kc: h.matmul(p.t[:, 0:ncol], lhsT=hT.t[:, kc, j * 128:(j + 1) * 128], rhs=wap[:, kc, :], start=(kc == 0), stop=(kc == 7)),
                      r=[hT, curw[0]], w=p, inc=(kc == 7))

        def proj_feat(lhs_of_kc, p, nk=8, rhs_of_kc=None, rbufs=None):
            for kc in range(nk):
                rhs = hT.t[:, kc, :] if rhs_of_kc is None else rhs_of_kc(kc)
                kk.op("pe", lambda h, kc=kc, rhs=rhs: h.matmul(p.t[:, 0:TT], lhsT=lhs_of_kc(kc), rhs=rhs, start=(kc == 0), stop=(kc == nk - 1)),
                      r=[hT if rbufs is None else rbufs, curw[0]], w=p, inc=(kc == nk - 1))

        curw = [None]

        def transposes_to(src_of, n, dst_ap_of_group, rbufs, wbufs, evac="act", parts=128, width=128):
            i = 0
            while i < n:
                m = min(8, n - i)
                p = nxt("T")
                for q in range(m):
                    kk.op("pe", lambda h, q=q, i=i: h.transpose(out=p.t[0:width, q * 128:(q + 1) * 128], in_=src_of(i + q), identity=ident.t[:]),
                          r=[rbufs, ident], w=p, inc=(q == m - 1))
                dst_ap_of_group(i, m, p)
                i += m

        try:
          chk("prepass")
          for s_ in range(nseq):
              for q in range(4):
                  kk.dma("pool", xst, xres.t[:, 4 * q:4 * q + 4, :], x_d[s_, q * 512:(q + 1) * 512, :].rearrange("(t p) d -> p t d", p=128),
                         w=xres.bufs[4 * q:4 * q + 4], skip_same=(q > 0))
              ar.reset(0)
              chk("xload")
              posi = ar.alloc([NT], I32 if False else F32)
              posi_i = Tmp(posi.ap.bitcast(I32), posi.bufs)
              for t in range(NT):
                  kk.dma("pool", posst, posi_i.ap[:, t:t + 1], pos_d[s_, t * 128:(t + 1) * 128].rearrange("(p o) -> p o", o=1), w=posi, skip_same=(t > 0))
              posf = ar.alloc([NT], F32)
              kk.op("dve", lambda h: h.tensor_copy(out=posf.ap, in_=posi_i.ap), r=posi, w=posf)
              rope_base = ar.off
              for (tab, inv, half) in ((ropeR, invr, 64), (ropeM, invm, 32)):
                  ar.reset(rope_base)
                  ang = ar.alloc([NT, half], F32)
                  for t in range(NT):
                      kk.op("dve", lambda h, t=t: h.tensor_scalar(out=ang.ap[:, t, :], in0=inv.t[:, 0:half], scalar1=posf.ap[:, t:t + 1], scalar2=None, op0=ALU.mult), r=[inv, posf], w=ang)
                  a2 = ar.alloc([2, NT, half], F32)
                  inv2pi = float(1.0 / (2 * np.pi))
                  kk.op("dve", lambda h: h.tensor_scalar(out=a2.ap[:, 1], in0=ang.ap, scalar1=inv2pi, scalar2=None, op0=ALU.mult), r=ang, w=a2)
                  kk.op("dve", lambda h: h.tensor_scalar(out=a2.ap[:, 0], in0=ang.ap, scalar1=inv2pi, scalar2=0.25, op0=ALU.mult, op1=ALU.add), r=ang, w=a2)
                  kf = ar.alloc([2, NT, half], F32)
                  ki = Tmp(kf.ap.bitcast(I32), kf.bufs)
                  kk.op("dve", lambda h: h.tensor_copy(out=ki.ap, in_=a2.ap), r=a2, w=kf)
                  kf2 = ar.alloc([2, NT, half], F32)
                  kk.op("dve", lambda h: h.tensor_copy(out=kf2.ap, in_=ki.ap), r=kf, w=kf2)
                  kk.op("dve", lambda h: h.tensor_tensor(out=a2.ap, in0=a2.ap, in1=kf2.ap, op=ALU.subtract), r=[a2, kf2], w=a2)
                  kk.op("dve", lambda h: h.tensor_scalar(out=kf2.ap, in0=a2.ap, scalar1=0.5, scalar2=None, op0=ALU.is_gt), r=a2, w=kf2)
                  kk.op("dve", lambda h: h.tensor_tensor(out=a2.ap, in0=a2.ap, in1=kf2.ap, op=ALU.subtract), r=[a2, kf2], w=a2)
                  s2 = ar.alloc([2, NT, half], F32)
                  kk.op("act", lambda h: h.activation(out=s2.ap, in_=a2.ap, func=AF.Sin, scale=float(2 * np.pi * (1 - 1e-6))), r=a2, w=s2)
                  for c in range(2):
                      kk.op("dve", lambda h, c=c: h.tensor_copy(out=tab.t[:, :, c, :], in_=s2.ap[:, c]), r=s2, w=tab)

              chk("rope")
              for l in range(nlayers):
                  kk.op("pool", lambda h: h.memset(shg_f.t[:], 0.0), w=shg_f)
                  kk.op("pool", lambda h: h.memset(shg_b.t[:], 0.0), w=shg_b)
                  kk.op("pool", lambda h: h.memset(sr_f.t[:], 0.0), w=sr_f)
                  kk.op("pool", lambda h: h.memset(sr_b.t[:], 0.0), w=sr_b)
                  for T in range(4):
                      norm_to_hT(T)
                      chk("norm")
                      ar.reset(0)
                      v_hg = ar.alloc([4, 512], BF16)
                      gate_a = ar.alloc([4, 512], BF16)
                      y_a = ar.alloc([4, 512], BF16)
                      curw[0] = wget("hi")
                      wv = curw[0].t[:, 0:4096].rearrange("p (k c) -> p k c", c=512)
                      for j in range(4):
                          p = nxt("mm")
                          proj_tok(wv, 512, j, p)
                          kk.op("act", lambda h, j=j, p=p: h.activation(out=v_hg.ap[:, j, :], in_=p.t[:], func=AF.Copy), r=p, w=v_hg)
                      curw[0] = wget("hgate")
                      wv = curw[0].t[:, 0:4096].rearrange("p (k c) -> p k c", c=512)
                      for j in range(4):
                          p = nxt("mm")
                          proj_tok(wv, 512, j, p)
                          sg = ar.alloc([512], F32) if j == 0 else sg
                          kk.op("act", lambda h, p=p: h.activation(out=sg.ap, in_=p.t[:], func=AF.Sigmoid), r=p, w=sg)
                          kk.op("dve", lambda h, j=j, p=p: h.tensor_tensor(out=gate_a.ap[:, j, :], in0=p.t[:], in1=sg.ap, op=ALU.mult), r=[p, sg], w=gate_a)
                      wq_ = wget("hq")
                      wf_ = wget("hf", keep=1)
                      wqv = wq_.t[:, 0:4096].rearrange("p (k c) -> p k c", c=512)
                      wfv = wf_.t[:, 0:4096].rearrange("p (k c) -> p k c", c=512)
                      base_off = ar.off
                      t0 = ar.alloc([512], F32)
                      t1 = ar.alloc([512], F32)
                      t2 = ar.alloc([512], F32)
                      t3 = ar.alloc([512], F32)
                      q_f = ar.alloc([512], F32)
                      k_f = ar.alloc([512], F32)
                      ecs = [ar.alloc([512], F32) for _ in range(2)]
                      qes = [ar.alloc([512], BF16) for _ in range(2)]
                      kes = [ar.alloc([512], BF16) for _ in range(2)]
                      qcs = [ar.alloc([512], BF16) for _ in range(2)]
                      kdTs = [ar.alloc([512], BF16) for _ in range(2)]
                      kds = [ar.alloc([4, 128], BF16) for _ in range(2)]
                      ats = [ar.alloc([4, 64], BF16) for _ in range(2)]

                      def c3(ap):
                          return ap.rearrange("p (c t) -> p c t", t=64)
                      for hp in range(2):
                          pos_ = {}
                          for hh in range(2):
                              hd = 2 * hp + hh
                              ec, qe, ke, qc, kdT, kd, at = ecs[hh], qes[hh], kes[hh], qcs[hh], kdTs[hh], kds[hh], ats[hh]
                              curw[0] = wq_
                              p = nxt("mm")
                              proj_feat(lambda kc, hd=hd: wqv[:, kc, hd * 128:(hd + 1) * 128], p)
                              kk.op("act", lambda h, p=p: h.activation(out=t0.ap, in_=p.t[:], func=AF.Sigmoid), r=p, w=t0)
                              kk.op("dve", lambda h, p=p: h.tensor_tensor(out=q_f.ap, in0=p.t[:], in1=t0.ap, op=ALU.mult), r=[p, t0], w=q_f)
                              curw[0] = wf_
                              p = nxt("mm")
                              proj_feat(lambda kc, hd=hd: wfv[:, kc, hd * 128:(hd + 1) * 128], p)
                              kk.op("act", lambda h, p=p: h.activation(out=t0.ap, in_=p.t[:], func=AF.Sigmoid), r=p, w=t0)
                              kk.op("dve", lambda h, hd=hd: h.tensor_scalar(out=t1.ap, in0=t0.ap, scalar1=lbt.t[:, l, 1, hd:hd + 1], scalar2=lbt.t[:, l, 0, hd:hd + 1], op0=ALU.mult, op1=ALU.add), r=[t0, lbt], w=t1)
                              kk.op("act", lambda h: h.activation(out=t0.ap, in_=t1.ap, func=AF.Ln), r=t1, w=t0)
                              kk.op("dve", lambda h: h.tensor_scalar(out=k_f.ap, in0=t1.ap, scalar1=-1.0, scalar2=1.0, op0=ALU.mult, op1=ALU.add), r=t1, w=k_f)
                              kk.op("dve", lambda h: h.tensor_tensor_scan(out=t1.ap, data0=resetc.t[:], data1=t0.ap, initial=0.0, op0=ALU.mult, op1=ALU.add), r=[resetc, t0], w=t1)
                              kk.op("dve", lambda h: h.tensor_tensor(out=c3(t0.ap), in0=c3(t1.ap), in1=c3(t1.ap)[:, :, 31:32].broadcast_to([128, 8, 64]), op=ALU.subtract), r=t1, w=t0)
                              kk.op("dve", lambda h: h.tensor_tensor(out=c3(t2.ap), in0=c3(t1.ap), in1=c3(t1.ap)[:, :, 63:64].broadcast_to([128, 8, 64]), op=ALU.subtract), r=t1, w=t2)
                              kk.op("act", lambda h: h.activation(out=t3.ap, in_=t0.ap, func=AF.Exp), r=t0, w=t3)
                              kk.op("dve", lambda h, qe=qe: h.tensor_tensor(out=qe.ap, in0=q_f.ap, in1=t3.ap, op=ALU.mult), r=[q_f, t3], w=qe)
                              kk.op("act", lambda h: h.activation(out=t3.ap, in_=t0.ap, func=AF.Exp, scale=-1.0), r=t0, w=t3)
                              kk.op("dve", lambda h, ke=ke: h.tensor_tensor(out=ke.ap, in0=k_f.ap, in1=t3.ap, op=ALU.mult), r=[k_f, t3], w=ke)
                              kk.op("act", lambda h, ec=ec: h.activation(out=ec.ap, in_=t1.ap, func=AF.Exp), r=t1, w=ec)
                              kk.op("dve", lambda h, qc=qc, ec=ec: h.tensor_tensor(out=qc.ap, in0=q_f.ap, in1=ec.ap, op=ALU.mult), r=[q_f, ec], w=qc)
                              kk.op("act", lambda h: h.activation(out=t3.ap, in_=t2.ap, func=AF.Exp, scale=-1.0), r=t2, w=t3)
                              kk.op("dve", lambda h, kdT=kdT: h.tensor_tensor(out=kdT.ap, in0=k_f.ap, in1=t3.ap, op=ALU.mult), r=[k_f, t3], w=kdT)
                              pT_ = nxt("T")
                              for j in range(4):
                                  kk.op("pe", lambda h, j=j, kdT=kdT, pT_=pT_: h.transpose(out=pT_.t[:, j * 128:(j + 1) * 128], in_=kdT.ap[:, j * 128:(j + 1) * 128], identity=ident.t[:]),
                                        r=[kdT, ident], w=pT_, inc=(j == 3))
                              kk.op("act", lambda h, kd=kd, pT_=pT_: h.activation(out=kd.ap, in_=pT_.t[:, 0:512].rearrange("p (j c) -> p j c", c=128), func=AF.Copy), r=pT_, w=kd)
                              pa = nxt("mm")
                              for ci in range(8):
                                  j, hf_ = ci // 2, ci % 2
                                  kk.op("pe", lambda h, ci=ci, j=j, hf_=hf_, ke=ke, qe=qe, pa=pa: h.matmul(pa.t[hf_ * 64:(hf_ + 1) * 64, j * 64:(j + 1) * 64], lhsT=ke.ap[:, ci * 64:(ci + 1) * 64], rhs=qe.ap[:, ci * 64:(ci + 1) * 64], start=True, stop=True),
                                        r=[ke, qe], w=pa, inc=(ci == 7))
                              kk.op("dve", lambda h, at=at, pa=pa: h.tensor_tensor(out=at.ap, in0=pa.t[:, 0:256].rearrange("p (j t) -> p j t", t=64), in1=hgmask.t[:].unsqueeze(1).broadcast_to([128, 4, 64]), op=ALU.mult), r=[pa, hgmask], w=at)
                              pos_[hh] = nxt("acc")
                          for ci in range(8):
                              j, hf_ = ci // 2, ci % 2
                              P0, P1 = hf_ * 64, (hf_ + 1) * 64
                              for hh in range(2):
                                  hd = 2 * hp + hh
                                  ec, qc, kd, at, po = ecs[hh], qcs[hh], kds[hh], ats[hh], pos_[hh]
                                  kk.op("pe", lambda h, po=po, at=at, j=j, hd=hd, P0=P0, P1=P1: h.matmul(po.t[P0:P1, j * 128:(j + 1) * 128], lhsT=at.ap[P0:P1, j, :], rhs=v_hg.ap[P0:P1, j, hd * 128:(hd + 1) * 128], start=True, stop=False),
                                        r=[at, v_hg], w=po, inc=False)
                                  kk.op("pe", lambda h, po=po, qc=qc, ci=ci, j=j, hd=hd, P0=P0, P1=P1: h.matmul(po.t[P0:P1, j * 128:(j + 1) * 128], lhsT=qc.ap[:, ci * 64:(ci + 1) * 64], rhs=shg_b.t[:, hd, :], start=False, stop=True),
                                        r=[qc, shg_b.bufs[hd]], w=po, inc=True)
                                  pSx = nxt("mm")
                                  kk.op("pe", lambda h, kd=kd, j=j, hd=hd, P0=P0, P1=P1, pSx=pSx: h.matmul(pSx.t[:, 0:128], lhsT=kd.ap[P0:P1, j, :], rhs=v_hg.ap[P0:P1, j, hd * 128:(hd + 1) * 128], start=True, stop=True),
                                        r=[kd, v_hg], w=pSx)
                                  kk.op("dve", lambda h, ec=ec, ci=ci, hd=hd, pSx=pSx: h.scalar_tensor_tensor(out=shg_f.t[:, hd, :], in0=shg_f.t[:, hd, :], scalar=ec.ap[:, ci * 64 + 63:ci * 64 + 64], in1=pSx.t[:, 0:128], op0=ALU.mult, op1=ALU.add),
                                        r=[ec, pSx, shg_f.bufs[hd]], w=shg_f.bufs[hd])
                                  kk.op("act", lambda h, hd=hd: h.activation(out=shg_b.t[:, hd, :], in_=shg_f.t[:, hd, :], func=AF.Copy), r=shg_f.bufs[hd], w=shg_b.bufs[hd])
                          for hh in range(2):
                              hd = 2 * hp + hh
                              po = pos_[hh]
                              ss = newcol(4)
                              for j in range(4):
                                  kk.op("act", lambda h, j=j, po=po, ss=ss: h.activation(out=t3.ap[:, 0:128], in_=po.t[:, j * 128:(j + 1) * 128], func=AF.Square, accum_out=ss.ap[:, j:j + 1]), r=po, w=[t3, ss])
                              rs = rstd_cols(ss, 4, 1.0 / 128)
                              for j in range(4):
                                  kk.op("dve", lambda h, j=j, po=po, rs=rs, hd=hd: h.scalar_tensor_tensor(out=y_a.ap[:, j, hd * 128:(hd + 1) * 128], in0=po.t[:, j * 128:(j + 1) * 128], scalar=rs.ap[:, j:j + 1], in1=gate_a.ap[:, j, hd * 128:(hd + 1) * 128], op0=ALU.mult, op1=ALU.mult),
                                        r=[po, rs, gate_a], w=y_a)
                      for j in range(4):
                          p = nxt("T")
                          for kc in range(4):
                              kk.op("pe", lambda h, j=j, kc=kc, p=p: h.transpose(out=p.t[:, kc * 128:(kc + 1) * 128], in_=y_a.ap[:, j, kc * 128:(kc + 1) * 128], identity=ident.t[:]),
                                    r=[y_a, ident], w=p, inc=(kc == 3))
                          kk.op("act", lambda h, j=j, p=p: h.activation(out=big.t[:, 0:4, j * 128:(j + 1) * 128], in_=p.t[:, 0:512].rearrange("p (k c) -> p k c", c=128), func=AF.Copy), r=p, w=yA)

                      chk("hg")
                      wrq, wrk = wget("rq"), wget("rk", keep=1)
                      wrv0, wrv1, wrg0, wrg1 = None, None, None, None
                      ar.reset(0)
                      qT = ar.alloc([4, 4, 128], BF16)
                      kT = ar.alloc([4, 4, 128], BF16)
                      qdT = ar.alloc([4, 4, 128], BF16)
                      kdr = ar.alloc([4, 4, 128], BF16)
                      v_r = ar.alloc([4, 1024], BF16)
                      gate_b = ar.alloc([4, 1024], BF16)
                      tA = ar.alloc([512], F32)
                      tB = ar.alloc([512], F32)
                      qk_r = [ar.alloc([512], BF16) for _ in range(2)]

                      def v4(ap):
                          return ap.rearrange("p (h two d) -> p h two d", two=2, d=64)
                      for j in range(4):
                          t = 4 * T + j
                          cosb = ropeR.t[:, t, 0, :].unsqueeze(1).unsqueeze(1).broadcast_to([128, 4, 2, 64])
                          sinb = ropeR.t[:, t, 1, :].unsqueeze(1).broadcast_to([128, 4, 64])
                          for qi, wblk in enumerate((wrq, wrk)):
                              curw[0] = wblk
                              p = nxt("mm")
                              proj_tok(wblk.t[:, 0:4096].rearrange("p (k c) -> p k c", c=512), 512, j, p)
                              dst = qk_r[qi]
                              kk.op("dve", lambda h, p=p: h.tensor_tensor(out=v4(tA.ap), in0=v4(p.t[:]), in1=cosb, op=ALU.mult), r=[p, ropeR], w=tA)
                              kk.op("dve", lambda h, p=p: h.tensor_tensor(out=v4(tB.ap)[:, :, 0, :], in0=v4(p.t[:])[:, :, 1, :], in1=sinb, op=ALU.mult), r=[p, ropeR], w=tB)
                              kk.op("dve", lambda h, p=p: h.tensor_tensor(out=v4(tB.ap)[:, :, 1, :], in0=v4(p.t[:])[:, :, 0, :], in1=sinb, op=ALU.mult), r=[p, ropeR], w=tB)
                              kk.op("dve", lambda h, dst=dst: h.tensor_tensor(out=v4(dst.ap)[:, :, 0, :], in0=v4(tA.ap)[:, :, 0, :], in1=v4(tB.ap)[:, :, 0, :], op=ALU.subtract), r=[tA, tB], w=dst)
                              kk.op("dve", lambda h, dst=dst: h.tensor_tensor(out=v4(dst.ap)[:, :, 1, :], in0=v4(tA.ap)[:, :, 1, :], in1=v4(tB.ap)[:, :, 1, :], op=ALU.add), r=[tA, tB], w=dst)
                          chk("ret1")
                          q_r, k_r = qk_r
                          kk.op("dve", lambda h, j=j: h.tensor_tensor(out=kdr.ap[:, j], in0=k_r.ap.rearrange("p (h d) -> p h d", d=128), in1=kdecc.t[:].unsqueeze(2).broadcast_to([128, 4, 128]), op=ALU.mult), r=[k_r, kdecc], w=kdr)
                          p = nxt("T")
                          for hd in range(4):
                              kk.op("pe", lambda h, hd=hd, p=p: h.transpose(out=p.t[:, hd * 128:(hd + 1) * 128], in_=q_r.ap[:, hd * 128:(hd + 1) * 128], identity=ident.t[:]), r=[q_r, ident], w=p, inc=False)
                          for hd in range(4):
                              kk.op("pe", lambda h, hd=hd, p=p: h.transpose(out=p.t[:, 512 + hd * 128:512 + (hd + 1) * 128], in_=k_r.ap[:, hd * 128:(hd + 1) * 128], identity=ident.t[:]), r=[k_r, ident], w=p, inc=(hd == 3))
                          chk("ret1b")
                          kk.op("act", lambda h, j=j, p=p: h.activation(out=qT.ap[:, j], in_=p.t[:, 0:512].rearrange("p (h t) -> p h t", t=128), func=AF.Copy), r=p, w=qT)
                          kk.op("act", lambda h, j=j, p=p: h.activation(out=kT.ap[:, j], in_=p.t[:, 512:1024].rearrange("p (h t) -> p h t", t=128), func=AF.Copy), r=p, w=kT)
                          chk("ret1c")
                          kk.op("dve", lambda h, j=j, p=p: h.tensor_tensor(out=qdT.ap[:, j], in0=p.t[:, 0:512].rearrange("p (h t) -> p h t", t=128), in1=qdec.t[:], op=ALU.mult), r=[p, qdec], w=qdT)
                          chk("ret1d")
                          if j == 1:
                              chk("ret1e")
                      chk("ret2")
                      wrv0, wrv1 = wget("rv0"), wget("rv1", keep=1)
                      for j in range(4):
                          for n_, wblk in enumerate((wrv0, wrv1)):
                              curw[0] = wblk
                              p = nxt("mm")
                              proj_tok(wblk.t[:, 0:4096].rearrange("p (k c) -> p k c", c=512), 512, j, p)
                              kk.op("act", lambda h, j=j, n_=n_, p=p: h.activation(out=v_r.ap[:, j, n_ * 512:(n_ + 1) * 512], in_=p.t[:], func=AF.Copy), r=p, w=v_r)
                      wrg0, wrg1 = wget("rg0"), wget("rg1", keep=1)
                      for j in range(4):
                          for n_, wblk in enumerate((wrg0, wrg1)):
                              curw[0] = wblk
                              p = nxt("mm")
                              proj_tok(wblk.t[:, 0:4096].rearrange("p (k c) -> p k c", c=512), 512, j, p)
                              kk.op("act", lambda h, p=p: h.activation(out=tA.ap, in_=p.t[:], func=AF.Sigmoid), r=p, w=tA)
                              kk.op("dve", lambda h, j=j, n_=n_, p=p: h.tensor_tensor(out=gate_b.ap[:, j, n_ * 512:(n_ + 1) * 512], in0=p.t[:], in1=tA.ap, op=ALU.mult), r=[p, tA], w=gate_b)
                      chk("ret3")
                      at_r = ar.alloc([4, 128], BF16)
                      y_b = ar.alloc([1024], BF16)
                      junk = ar.alloc([256], F32)
                      g128 = [float(np.exp(128.0 * LOG_GAMMA[h_])) for h_ in range(4)]
                      for j in range(4):
                          pa = nxt("mm")
                          for hd in range(4):
                              kk.op("pe", lambda h, j=j, hd=hd, pa=pa: h.matmul(pa.t[:, hd * 128:(hd + 1) * 128], lhsT=kT.ap[:, j, hd, :], rhs=qT.ap[:, j, hd, :], start=True, stop=True),
                                    r=[kT, qT], w=pa, inc=(hd == 3))
                          kk.op("dve", lambda h, pa=pa: h.tensor_tensor(out=at_r.ap, in0=pa.t[:].rearrange("p (h t) -> p h t", t=128), in1=dmask.t[:], op=ALU.mult), r=[pa, dmask], w=at_r)
                          pos2 = [nxt("acc"), nxt("acc")]
                          ss = newcol(4)
                          for hd in range(4):
                              po = pos2[hd // 2]
                              oc = (hd % 2) * 256
                              kk.op("pe", lambda h, j=j, hd=hd, po=po, oc=oc: h.matmul(po.t[:, oc:oc + 256], lhsT=at_r.ap[:, hd, :], rhs=v_r.ap[:, j, hd * 256:(hd + 1) * 256], start=True, stop=False),
                                    r=[at_r, v_r], w=po, inc=False)
                              kk.op("pe", lambda h, j=j, hd=hd, po=po, oc=oc: h.matmul(po.t[:, oc:oc + 256], lhsT=qdT.ap[:, j, hd, :], rhs=sr_b.t[:, hd, :], start=False, stop=True),
                                    r=[qdT, sr_b.bufs[hd]], w=po, inc=True)
                              pSx = nxt("mm") if hd % 2 == 0 else pS
                              kk.op("pe", lambda h, j=j, hd=hd, pSx=pSx: h.matmul(pSx.t[:, 0:256], lhsT=kdr.ap[:, j, hd, :], rhs=v_r.ap[:, j, hd * 256:(hd + 1) * 256], start=True, stop=True),
                                    r=[kdr, v_r], w=pSx)
                              kk.op("dve", lambda h, hd=hd, pSx=pSx: h.scalar_tensor_tensor(out=sr_f.t[:, hd, :], in0=sr_f.t[:, hd, :], scalar=g128[hd], in1=pSx.t[:, 0:256], op0=ALU.mult, op1=ALU.add),
                                    r=[pSx, sr_f.bufs[hd]], w=sr_f.bufs[hd])
                              kk.op("act", lambda h, hd=hd: h.activation(out=sr_b.t[:, hd, :], in_=sr_f.t[:, hd, :], func=AF.Copy), r=sr_f.bufs[hd], w=sr_b.bufs[hd])
                              kk.op("act", lambda h, hd=hd, po=po, oc=oc, ss=ss: h.activation(out=junk.ap, in_=po.t[:, oc:oc + 256], func=AF.Square, accum_out=ss.ap[:, hd:hd + 1]), r=po, w=[junk, ss])
                          chk("ret4")
                          rs = rstd_cols(ss, 4, 1.0 / 256)
                          for hd in range(4):
                              po = pos2[hd // 2]
                              oc = (hd % 2) * 256
                              kk.op("dve", lambda h, j=j, hd=hd, po=po, oc=oc, rs=rs: h.scalar_tensor_tensor(out=y_b.ap[:, hd * 256:(hd + 1) * 256], in0=po.t[:, oc:oc + 256], scalar=rs.ap[:, hd:hd + 1], in1=gate_b.ap[:, j, hd * 256:(hd + 1) * 256], op0=ALU.mult, op1=ALU.mult),
                                    r=[po, rs, gate_b], w=y_b)
                          p = nxt("T")
                          for kc in range(8):
                              kk.op("pe", lambda h, kc=kc, p=p: h.transpose(out=p.t[:, kc * 128:(kc + 1) * 128], in_=y_b.ap[:, kc * 128:(kc + 1) * 128], identity=ident.t[:]), r=[y_b, ident], w=p, inc=(kc == 7))
                          kk.op("act", lambda h, j=j, p=p: h.activation(out=big.t[:, 4:12, j * 128:(j + 1) * 128], in_=p.t[:].rearrange("p (k c) -> p k c", c=128), func=AF.Copy), r=p, w=yB)

                      chk("ret")
                      wc = wget("c")
                      wm = wget("mla", keep=1)
                      ar.reset(0)
                      qnT = ar.alloc([2, 512], BF16)
                      qabsT = ar.alloc([4, 512], BF16)
                      qpeT = ar.alloc([4, 512], BF16, parts=64)
                      qn = ar.alloc([256], BF16)
                      kpe = ar.alloc([64], BF16)
                      junk = ar.alloc([256], F32)
                      tA = ar.alloc([256], F32)
                      tB = ar.alloc([256], F32)
                      qpe = ar.alloc([256], BF16)
                      qnopeT = ar.alloc([512], BF16)
                      wcv = wc.t[:, 0:8 * 448].rearrange("p (k c) -> p k c", c=448)
                      wuq = wm.t[:, 0:1536].rearrange("p (k c) -> p k c", c=768)
                      wukv = wm.t[:, 1536:2560]
                      wukT = wm.t[:, 2560:3072].rearrange("p (h c) -> p h c", c=128)

                      def rope_small(dst, src_ap, nh, t, rb, wb):
                          def v(ap):
                              return ap.rearrange("p (h two d) -> p h two d", two=2, d=32)
                          cosb = ropeM.t[:, t, 0, :].unsqueeze(1).unsqueeze(1).broadcast_to([128, nh, 2, 32])
                          sinb = ropeM.t[:, t, 1, :].unsqueeze(1).broadcast_to([128, nh, 32])
                          n = nh * 64
                          kk.op("dve", lambda h: h.tensor_tensor(out=v(tA.ap[:, 0:n]), in0=v(src_ap), in1=cosb, op=ALU.mult), r=[rb, ropeM], w=tA)
                          kk.op("dve", lambda h: h.tensor_tensor(out=v(tB.ap[:, 0:n])[:, :, 0, :], in0=v(src_ap)[:, :, 1, :], in1=sinb, op=ALU.mult), r=[rb, ropeM], w=tB)
                          kk.op("dve", lambda h: h.tensor_tensor(out=v(tB.ap[:, 0:n])[:, :, 1, :], in0=v(src_ap)[:, :, 0, :], in1=sinb, op=ALU.mult), r=[rb, ropeM], w=tB)
                          kk.op("dve", lambda h: h.tensor_tensor(out=v(dst)[:, :, 0, :], in0=v(tA.ap[:, 0:n])[:, :, 0, :], in1=v(tB.ap[:, 0:n])[:, :, 0, :], op=ALU.subtract), r=[tA, tB], w=wb)
                          kk.op("dve", lambda h: h.tensor_tensor(out=v(dst)[:, :, 1, :], in0=v(tA.ap[:, 0:n])[:, :, 1, :], in1=v(tB.ap[:, 0:n])[:, :, 1, :], op=ALU.add), r=[tA, tB], w=wb)

                      for j in range(4):
                          t = 4 * T + j
                          curw[0] = wc
                          p = nxt("mm")
                          proj_tok(wcv, 448, j, p)
                          ss = newcol(2)
                          kk.op("act", lambda h, p=p, ss=ss: h.activation(out=junk.ap, in_=p.t[:, 0:256], func=AF.Square, accum_out=ss.ap[:, 0:1]), r=p, w=[junk, ss])
                          kk.op("act", lambda h, p=p, ss=ss: h.activation(out=junk.ap[:, 0:128], in_=p.t[:, 256:384], func=AF.Square, accum_out=ss.ap[:, 1:2]), r=p, w=[junk, ss])
                          t1_ = newcol(2)
                          kk.op("pool", lambda h, ss=ss, t1_=t1_: h.tensor_scalar(out=t1_.ap[:, 0:1], in0=ss.ap[:, 0:1], scalar1=1.0 / 256, scalar2=EPS, op0=ALU.mult, op1=ALU.add), r=ss, w=t1_)
                          kk.op("pool", lambda h, ss=ss, t1_=t1_: h.tensor_scalar(out=t1_.ap[:, 1:2], in0=ss.ap[:, 1:2], scalar1=1.0 / 128, scalar2=EPS, op0=ALU.mult, op1=ALU.add), r=ss, w=t1_)
                          rs = newcol(2)
                          kk.op("pool", lambda h, rs=rs, t1_=t1_: h.tensor_tensor(out=rs.ap, in0=t1_.ap, in1=mhalf.t[:, 0:2], op=ALU.pow), r=[t1_, mhalf], w=rs)
                          kk.op("dve", lambda h, p=p, rs=rs: h.tensor_scalar(out=qn.ap, in0=p.t[:, 0:256], scalar1=rs.ap[:, 0:1], scalar2=None, op0=ALU.mult), r=[p, rs], w=qn)
                          kk.op("dve", lambda h, p=p, rs=rs, t=t: h.tensor_scalar(out=kvtok.t[:, t, :], in0=p.t[:, 256:384], scalar1=rs.ap[:, 1:2], scalar2=None, op0=ALU.mult), r=[p, rs], w=kvtok.bufs[t])
                          rope_small(kpe.ap, p.t[:, 384:448], 1, t, p, kpe)
                          pt_ = nxt("T")
                          kk.op("pe", lambda h, pt_=pt_: h.transpose(out=pt_.t[:, 0:128], in_=qn.ap[:, 0:128], identity=ident.t[:]), r=[qn, ident], w=pt_, inc=False)
                          kk.op("pe", lambda h, pt_=pt_: h.transpose(out=pt_.t[:, 128:256], in_=qn.ap[:, 128:256], identity=ident.t[:]), r=[qn, ident], w=pt_, inc=False)
                          kk.op("pe", lambda h, pt_=pt_, t=t: h.transpose(out=pt_.t[:, 256:384], in_=kvtok.t[:, t, :], identity=ident.t[:]), r=[kvtok.bufs[t], ident], w=pt_, inc=False)
                          kk.op("pe", lambda h, pt_=pt_: h.transpose(out=pt_.t[0:64, 384:512], in_=kpe.ap, identity=ident.t[:]), r=[kpe, ident], w=pt_, inc=True)
                          kk.op("act", lambda h, j=j, pt_=pt_: h.activation(out=qnT.ap[:, :, j * 128:(j + 1) * 128], in_=pt_.t[:, 0:256].rearrange("p (k c) -> p k c", c=128), func=AF.Copy), r=pt_, w=qnT)
                          kk.op("act", lambda h, t=t, pt_=pt_: h.activation(out=kvnT.t[:, t * 128:(t + 1) * 128], in_=pt_.t[:, 256:384], func=AF.Copy), r=pt_, w=kvnT.bufs[t])
                          kk.op("act", lambda h, t=t, pt_=pt_: h.activation(out=kpeT.t[:, t * 128:(t + 1) * 128], in_=pt_.t[0:64, 384:512], func=AF.Copy), r=pt_, w=kpeT.bufs[t])
                      curw[0] = wm
                      for hd in range(4):
                          p = nxt("mm")
                          proj_feat(lambda kc, hd=hd: wuq[:, kc, hd * 192:hd * 192 + 128], p, nk=2, rhs_of_kc=lambda kc: qnT.ap[:, kc, :], rbufs=qnT)
                          kk.op("act", lambda h, p=p: h.activation(out=qnopeT.ap, in_=p.t[:], func=AF.Copy, scale=S192), r=p, w=qnopeT)
                          p2 = nxt("mm")
                          kk.op("pe", lambda h, hd=hd, p2=p2: h.matmul(p2.t[:], lhsT=wukT[:, hd, :], rhs=qnopeT.ap, start=True, stop=True), r=[wm, qnopeT], w=p2)
                          kk.op("act", lambda h, hd=hd, p2=p2: h.activation(out=qabsT.ap[:, hd, :], in_=p2.t[:], func=AF.Copy), r=p2, w=qabsT)
                      wuq_pe = wuq.rearrange("p k (h d) -> p k h d", d=192)[:, :, :, 128:192]
                      for j in range(4):
                          t = 4 * T + j
                          p = nxt("mm")
                          for kc in range(2):
                              kk.op("pe", lambda h, kc=kc, j=j, p=p: h.matmul(p.t[:, 0:256].rearrange("p (h d) -> p h d", d=64), lhsT=qnT.ap[:, kc, j * 128:(j + 1) * 128], rhs=wuq_pe[:, kc], start=(kc == 0), stop=(kc == 1)),
                                    r=[qnT, wm], w=p, inc=(kc == 1))
                          rope_small(qpe.ap, p.t[:, 0:256], 4, t, p, qpe)
                          pt_ = nxt("T")
                          for hd in range(4):
                              kk.op("pe", lambda h, hd=hd, pt_=pt_: h.transpose(out=pt_.t[0:64, hd * 128:(hd + 1) * 128], in_=qpe.ap[:, hd * 64:(hd + 1) * 64], identity=ident.t[:]), r=[qpe, ident], w=pt_, inc=(hd == 3))
                          kk.op("act", lambda h, j=j, pt_=pt_: h.activation(out=qpeT.ap[:, :, j * 128:(j + 1) * 128], in_=pt_.t[0:64, 0:512].rearrange("p (h c) -> p h c", c=128), func=AF.Copy, scale=S192), r=pt_, w=qpeT)
                      pTs = [ar.alloc([512], BF16, align=True) for _ in range(3)]
                      ar.alloc([0], BF16, align=True)
                      olT = ar.alloc([512], BF16)
                      rinv = ar.alloc([512], F32)
                      prr = 0
                      for hd in range(4):
                          po_l, po_s = nxt("acc"), nxt("acc")
                          njb = 4 * T + 4
                          def emit_st(jb, hd=hd):
                              c0 = max(0, jb - 4 * T) * 128
                              p = nxt("mm")
                              kk.op("pe", lambda h, jb=jb, hd=hd, c0=c0, p=p: h.matmul(p.t[:, c0:512], lhsT=kvnT.t[:, jb * 128:(jb + 1) * 128], rhs=qabsT.ap[:, hd, c0:512], start=True, stop=False),
                                    r=[kvnT.bufs[jb], qabsT], w=p, inc=False)
                              kk.op("pe", lambda h, jb=jb, hd=hd, c0=c0, p=p: h.matmul(p.t[:, c0:512], lhsT=kpeT.t[:, jb * 128:(jb + 1) * 128], rhs=qpeT.ap[:, hd, c0:512], start=False, stop=True),
                                    r=[kpeT.bufs[jb], qpeT], w=p, inc=True)
                              return p
                          stq = [emit_st(0)]
                          for jb in range(njb):
                              c0 = max(0, jb - 4 * T) * 128
                              if jb + 1 < njb:
                                  stq.append(emit_st(jb + 1))
                              p = stq.pop(0)
                              pT_ = pTs[prr % 3]
                              prr += 1
                              kk.op("act", lambda h, c0=c0, p=p, pT_=pT_: h.activation(out=pT_.ap[:, c0:512], in_=p.t[:, c0:512], func=AF.Exp), r=p, w=pT_)
                              if jb >= 4 * T:
                                  kk.op("pool", lambda h, c0=c0, pT_=pT_: h.tensor_tensor(out=pT_.ap[:, c0:c0 + 128], in0=pT_.ap[:, c0:c0 + 128], in1=causal.t[:], op=ALU.mult), r=[pT_, causal], w=pT_)
                              kk.op("pe", lambda h, jb=jb, c0=c0, pT_=pT_, po_l=po_l: h.matmul(po_l.t[:, c0:512], lhsT=kvtok.t[:, jb, :], rhs=pT_.ap[:, c0:512], start=(jb == 0), stop=(jb == njb - 1)),
                                    r=[kvtok.bufs[jb], pT_], w=po_l, inc=(jb == njb - 1))
                              kk.op("pe", lambda h, jb=jb, c0=c0, pT_=pT_, po_s=po_s: h.matmul(po_s.t[:, c0:512], lhsT=ones.t[:], rhs=pT_.ap[:, c0:512], start=(jb == 0), stop=(jb == njb - 1)),
                                    r=[ones, pT_], w=po_s, inc=True)
                          kk.op("act", lambda h, po_l=po_l: h.activation(out=olT.ap, in_=po_l.t[:], func=AF.Copy), r=po_l, w=olT)
                          kk.op("dve", lambda h, po_s=po_s: h.reciprocal(out=rinv.ap, in_=po_s.t[:]), r=po_s, w=rinv)
                          p = nxt("mm")
                          kk.op("pe", lambda h, hd=hd, p=p: h.matmul(p.t[:], lhsT=wukv[:, hd * 256 + 128:hd * 256 + 256], rhs=olT.ap, start=True, stop=True), r=[wm, olT], w=p)
                          kk.op("dve", lambda h, hd=hd, p=p: h.tensor_tensor(out=big.t[:, 12 + hd, :], in0=p.t[:], in1=rinv.ap, op=ALU.mult), r=[p, rinv], w=yC)

                      chk("mla")
                      ar.reset(0)
                      gs = [ar.alloc([512], F32) for _ in range(3)]
                      ts = [ar.alloc([512], F32) for _ in range(3)]
                      m1 = ar.alloc([512], F32)
                      for jc in range(8):
                          wM = wget(f"M{jc}")
                          curw[0] = wM
                          wg = wM.t[:, 0:3072].rearrange("p (k b c) -> p k b c", b=3, c=128)
                          wb = wM.t[:, 3072:5120].rearrange("p (k c) -> p k c", c=128)
                          for b_, (k0, nk, yb) in enumerate(((0, 4, yA), (4, 8, yB), (12, 4, yC))):
                              pg = nxt("wide")
                              proj_feat(lambda kc, b_=b_: wg[:, kc, b_, :], pg)
                              kk.op("act", lambda h, b_=b_, pg=pg: h.activation(out=gs[b_].ap, in_=pg.t[:], func=AF.Sigmoid), r=pg, w=gs[b_])
                              pp = nxt("wide")
                              proj_feat(lambda kc, k0=k0: wb[:, k0 + kc, :], pp, nk=nk, rhs_of_kc=lambda kc, k0=k0: big.t[:, k0 + kc, :], rbufs=yb)
                              kk.op("dve", lambda h, b_=b_, pp=pp: h.tensor_tensor(out=ts[b_].ap, in0=pp.t[:], in1=gs[b_].ap, op=ALU.mult), r=[pp, gs[b_]], w=ts[b_])
                          kk.op("pool", lambda h: h.tensor_tensor(out=m1.ap, in0=ts[0].ap, in1=ts[1].ap, op=ALU.add), r=[ts[0], ts[1]], w=m1)
                          kk.op("pool", lambda h, jc=jc: h.tensor_tensor(out=big.t[:, 16 + jc, :], in0=m1.ap, in1=ts[2].ap, op=ALU.add), r=[m1, ts[2]], w=mTb)
                      chk("merge")
                      for n_ in range(2):
                          wo = wget(f"wout{n_}")
                          wov = wo.t[:, 0:4096].rearrange("p (k c) -> p k c", c=512)
                          for j in range(4):
                              t = 4 * T + j
                              p = nxt("wide")
                              for kc in range(8):
                                  kk.op("pe", lambda h, kc=kc, j=j, p=p: h.matmul(p.t[:], lhsT=big.t[:, 16 + kc, j * 128:(j + 1) * 128], rhs=wov[:, kc, :], start=(kc == 0), stop=(kc == 7)),
                                        r=[mTb, wo], w=p, inc=(kc == 7))
                              kk.op("dve", lambda h, t=t, n_=n_, p=p: h.tensor_tensor(out=xres.t[:, t, n_ * 512:(n_ + 1) * 512], in0=xres.t[:, t, n_ * 512:(n_ + 1) * 512], in1=p.t[:], op=ALU.add),
                                    r=[p, xres.bufs[t]], w=xres.bufs[t])
                      chk("wout")
                      norm_to_hT(T)
                      ar.reset(5120)
                      sgs = [ar.alloc([512], F32) for _ in range(2)]
                      t1s = [ar.alloc([512], F32) for _ in range(2)]
                      for jb in range(11):
                          wf = wget(f"ffin{jb}")
                          curw[0] = wf
                          wfv = wf.t[:, 0:4096].rearrange("p (k c) -> p k c", c=512)
                          for fc in range(2):
                              ff = jb * 2 + fc
                              pg = nxt("wide")
                              proj_feat(lambda kc, fc=fc: wfv[:, kc, fc * 128:(fc + 1) * 128], pg)
                              pu = nxt("wide")
                              proj_feat(lambda kc, fc=fc: wfv[:, kc, 256 + fc * 128:256 + (fc + 1) * 128], pu)
                              sg, t1_ = sgs[ff % 2], t1s[ff % 2]
                              kk.op("act", lambda h, pg=pg, sg=sg: h.activation(out=sg.ap, in_=pg.t[:], func=AF.Sigmoid), r=pg, w=sg)
                              kk.op("dve", lambda h, pg=pg, sg=sg, t1_=t1_: h.tensor_tensor(out=t1_.ap, in0=pg.t[:], in1=sg.ap, op=ALU.mult), r=[pg, sg], w=t1_)
                              kk.op("dve", lambda h, pu=pu, t1_=t1_, ff=ff: h.tensor_tensor(out=big.t[:, ff, :], in0=pu.t[:], in1=t1_.ap, op=ALU.mult), r=[pu, t1_], w=big.bufs)
                      for n_ in range(2):
                          wparts = [wget(f"ffout{n_ * 3 + i}", keep=i) for i in range(2)]
                          for j in range(4):
                              pass
                          pj = [nxt("mm"), nxt("mm"), nxt("mm"), pS]
                          kcs = [(0, 8), (8, 8), (16, 6)]
                          for pi in range(3):
                              wpart = wparts[pi] if pi < 2 else wget(f"ffout{n_ * 3 + 2}", keep=2)
                              kc0, nk = kcs[pi]
                              wpv = wpart.t[:, 0:nk * 512].rearrange("p (k c) -> p k c", c=512)
                              for j in range(4):
                                  for kc in range(nk):
                                      g = kc0 + kc
                                      kk.op("pe", lambda h, g=g, kc=kc, j=j, wpv=wpv: h.matmul(pj[j].t[:], lhsT=big.t[:, g, j * 128:(j + 1) * 128], rhs=wpv[:, kc, :], start=(g == 0), stop=(g == 21)),
                                            r=[big.bufs, wpart], w=pj[j], inc=(kc == nk - 1))
                          for j in range(4):
                              t = 4 * T + j
                              kk.op("dve", lambda h, t=t, n_=n_, j=j: h.tensor_tensor(out=xres.t[:, t, n_ * 512:(n_ + 1) * 512], in0=xres.t[:, t, n_ * 512:(n_ + 1) * 512], in1=pj[j].t[:], op=ALU.add),
                                    r=[pj[j], xres.bufs[t]], w=xres.bufs[t])
              ar.reset(0)
              obuf = [ar.alloc([D], F32) for _ in range(2)]
              junk = ar.alloc([D], BF16)
              fnw = ar.alloc([D], F32)
              kk.dma("pool", fnwst, fnw.ap, fnw_d.partition_broadcast(128), w=fnw)
              for t in range(NT):
                  xb = xres.bufs[t]
                  ss = newcol(1)
                  kk.op("act", lambda h, t=t, ss=ss: h.activation(out=junk.ap, in_=xres.t[:, t, :], func=AF.Square, accum_out=ss.ap), r=xb, w=[junk, ss])
                  rs = rstd_cols(ss, 1, 1.0 / D)
                  ob = obuf[t % 2]
                  kk.op("dve", lambda h, t=t, rs=rs, ob=ob: h.scalar_tensor_tensor(out=ob.ap, in0=xres.t[:, t, :], scalar=rs.ap, in1=fnw.ap, op0=ALU.mult, op1=ALU.mult), r=[xb, rs, fnw], w=ob)
                  kk.dma("pool", osts[t % 2], out_d[s_, t * 128:(t + 1) * 128, :], ob.ap, r=ob)
        except _Stop:
            for t in range(NT):
                kk.dma("sp", osts[t % 2], out_d[0, t * 128:(t + 1) * 128, :], xres.t[:, t, :], r=xres.bufs[t])
        for ost in osts:
            kk.E["pool"].h.wait_ge(ost.sem, ost.cnt)
            kk.E["sp"].h.wait_ge(ost.sem, ost.cnt)
        build.info = dict(ninst=kk.ninst, nsem=kk.nsem, arena_hi=ar.hi, sbuf_left=nc.sbuf_bytes_remaining)
    return nc


def host_consts():
    bf = ml_dtypes.bfloat16
    c = {}
    c["c_ident"] = np.eye(128, dtype=np.float32).astype(bf)
    s = np.arange(128)[:, None] % 64
    t = np.arange(64)[None, :]
    c["c_hgmask"] = (s <= t).astype(np.float32).astype(bf)
    s = np.arange(128)[:, None]
    t = np.arange(128)[None, :]
    c["c_causal"] = (s <= t).astype(np.float32).astype(bf)
    c["c_ones"] = np.ones((128, 128), np.float32).astype(bf)
    r = np.ones((128, 512), np.float32)
    r[:, ::64] = 0.0
    c["c_reset"] = r
    lg = np.array(LOG_GAMMA, np.float64)
    dm = np.zeros((128, 4, 128), np.float64)
    for h in range(4):
        dm[:, h, :] = np.where(s <= t, np.exp((t - s) * lg[h]), 0.0) * (128.0 ** -0.5)
    c["c_dmask"] = dm.astype(np.float32)
    qd = np.zeros((128, 4, 128), np.float64)
    for h in range(4):
        qd[:, h, :] = np.exp((np.arange(128)[None, :] + 1.0) * lg[h])
    c["c_qdec"] = qd.astype(np.float32)
    kd = np.zeros((128, 4), np.float64)
    for h in range(4):
        kd[:, h] = np.exp((127.0 - np.arange(128)) * lg[h]) * (128.0 ** -0.5)
    c["c_kdec"] = kd.astype(np.float32)
    c["c_invr"] = np.broadcast_to((10000.0 ** (-np.arange(64, dtype=np.float32) / 64)).astype(np.float32), (128, 64)).copy()
    c["c_invm"] = np.broadcast_to((10000.0 ** (-np.arange(32, dtype=np.float32) / 32)).astype(np.float32), (128, 32)).copy()
    return c


_NC_CACHE = {}


def kernel(**inputs):
    ncores = 8
    B = inputs["x"].shape[0]
    nseq = B // ncores
    key = (nseq, 2)
    if key not in _NC_CACHE:
        _NC_CACHE[key] = build(nseq=nseq, nlayers=2)
    nc = _NC_CACHE[key]
    consts = host_consts()
    in_maps = []
    for c in range(ncores):
        m = {}
        for k, v in inputs.items():
            v = np.asarray(v)
            if k in ("x", "positions"):
                m[k] = np.ascontiguousarray(v[c * nseq:(c + 1) * nseq])
            else:
                m[k] = np.ascontiguousarray(v)
        m.update(consts)
        in_maps.append(m)
    res = run_bass_kernel_spmd(nc, in_maps, core_ids=list(range(ncores)))
    out = np.concatenate([np.asarray(r["out"]) for r in res.results], axis=0)
    return out.astype(np.float32)
```

```python
import numpy as np
import ml_dtypes
from contextlib import ExitStack
import concourse.bass as bass
import concourse.mybir as mybir
from concourse.bass_utils import run_bass_kernel_spmd

F32, BF16, I32 = mybir.dt.float32, mybir.dt.bfloat16, mybir.dt.int32
AF = mybir.ActivationFunctionType
ALU = mybir.AluOpType

D = 1024
SEQ = 2048
NT = 16
TT = 512
NIN = 8640
DFF = 2816
EPS = 1e-6
S192 = 192.0 ** -0.5
LOG_GAMMA = [float(np.log1p(-(2.0 ** (-5.0 - h)))) for h in range(4)]
SLOT = 5120

BLK = ["hi", "hgate", "hq", "hf", "rq", "rk", "rv0", "rv1", "rg0", "rg1", "c", "mla"] + \
      [f"M{j}" for j in range(8)] + ["wout0", "wout1"] + [f"ffin{j}" for j in range(11)] + \
      [f"ffout{i}" for i in range(6)]
BIDX = {n: i for i, n in enumerate(BLK)}
NBLK = len(BLK)
WIN_C0 = {"hq": 0, "hf": 512, "hi": 1024, "hgate": 1536, "rq": 2048, "rk": 2560, "rv0": 3072,
          "rv1": 3584, "rg0": 4096, "rg1": 4608, "c": 5120}
FFOUT_PARTS = [(n, kc0, nk) for n in range(2) for (kc0, nk) in ((0, 8), (8, 8), (16, 6))]


def blk_size(name):
    if name == "c":
        return 8 * 448
    if name == "mla":
        return 3072
    if name.startswith("M"):
        return 5120
    if name.startswith("ffout"):
        return FFOUT_PARTS[int(name[5:])][2] * 512
    return 4096


class Buf:
    __slots__ = ("name", "w", "r", "psum")

    def __init__(self, name, psum=False):
        self.name = name
        self.w = None
        self.r = {}
        self.psum = psum


def flat(x):
    out = []

    def rec(y):
        if y is None:
            return
        if isinstance(y, Buf):
            out.append(y)
        elif hasattr(y, "bufs"):
            out.extend(y.bufs)
        else:
            for z in y:
                rec(z)
    rec(x)
    return out


class Eng:
    def __init__(self, h, key, is_pe=False):
        self.h = h
        self.key = key
        self.is_pe = is_pe
        self.sem = None
        self.cnt = 0
        self.seen = {}
        self.pend = False


class Stream:
    def __init__(self, kk, name):
        self.sem = kk.newsem(name)
        self.cnt = 0
        self.key = ("dma", name)
        self.gb = []


class K:
    SEM_LIMIT = 24000

    def __init__(self, nc, st):
        self.nc = nc
        self.st = st
        self.nsem = 0
        self.E = {}
        for key, h in (("pe", nc.tensor), ("act", nc.scalar), ("dve", nc.vector),
                       ("pool", nc.gpsimd), ("sp", nc.sync)):
            e = Eng(h, key, key == "pe")
            e.sem = self.newsem(key)
            self.E[key] = e
        self.ninst = 0

    def newsem(self, name):
        self.nsem += 1
        return self.st.enter_context(self.nc.semaphore(f"s{self.nsem}_{name}"))

    def _wait(self, E, r, w, skip_key=None):
        deps = {}

        def add(t):
            if t is None:
                return
            sem, val, key = t
            if key == E.key and E.is_pe:
                return
            if skip_key is not None and key == skip_key:
                return
            k = id(sem)
            if k not in deps or deps[k][1] < val:
                deps[k] = (sem, val)
        for b in r:
            add(b.w)
        for b in w:
            add(b.w)
            for k, t in b.r.items():
                add(t)
        for k, (sem, val) in deps.items():
            if E.seen.get(k, 0) < val:
                E.h.wait_ge(sem, val)
                E.seen[k] = val

    def op(self, eng, fn, r=(), w=(), inc=True):
        E = self.E[eng]
        r = flat(r)
        w = flat(w)
        w = w + [b for b in r if b.psum and b not in w]
        r = [b for b in r if not b.psum]
        self._wait(E, r, w)
        ins = fn(E.h)
        self.ninst += 1
        if inc:
            if E.cnt >= self.SEM_LIMIT and not E.pend:
                E.sem = self.newsem(E.key)
                E.cnt = 0
            E.cnt += 1
            ins.then_inc(E.sem, 1)
            tick = (E.sem, E.cnt, E.key)
            E.pend = False
        else:
            assert E.is_pe
            tick = (E.sem, E.cnt + 1, E.key)
            E.pend = True
        for b in r:
            b.r[E.key] = tick
        for b in w:
            b.w = tick
            b.r = {}
        return ins

    def dma(self, q, stream, out, in_, r=(), w=(), skip_same=False, **kw):
        E = self.E[q]
        r = flat(r)
        w = flat(w)
        self._wait(E, r, w, skip_key=stream.key if skip_same else None)
        ins = E.h.dma_start(out=out, in_=in_, **kw)
        self.ninst += 1
        stream.cnt += 16
        ins.then_inc(stream.sem, 16)
        tick = (stream.sem, stream.cnt, stream.key)
        if not skip_same:
            stream.gb = []
        for b in w:
            if b not in stream.gb:
                stream.gb.append(b)
        for b in r:
            b.r[stream.key] = tick
        for b in stream.gb:
            b.w = tick
            b.r = {}
        return ins

    def wait_all(self, eng, bufs):
        E = self.E[eng]
        self._wait(E, flat(bufs), flat(bufs))


class PT:
    def __init__(self, t, bufs):
        self.t = t
        self.bufs = bufs


class Tmp:
    def __init__(self, ap, bufs):
        self.ap = ap
        self.bufs = bufs


class Arena:
    PAGE = 512

    def __init__(self, nc, st, name, nelem_bf16):
        self.t = st.enter_context(nc.sbuf_tensor(name, [128, nelem_bf16], BF16))
        self.n = nelem_bf16
        self.pages = [Buf(f"{name}_p{i}") for i in range((nelem_bf16 + self.PAGE - 1) // self.PAGE)]
        self.off = 0
        self.hi = 0

    def reset(self, off=0):
        self.off = off

    def alloc(self, free_shape, dt, parts=128, align=False):
        if align:
            self.off = (self.off + self.PAGE - 1) // self.PAGE * self.PAGE
        n = int(np.prod(free_shape))
        nb = n * (2 if dt == F32 else 1)
        if dt == F32 and self.off % 2:
            self.off += 1
        o = self.off
        assert o + nb <= self.n, f"arena overflow {o + nb} > {self.n}"
        self.off += nb
        self.hi = max(self.hi, self.off)
        ap = self.t[0:parts, o:o + nb]
        if dt == F32:
            ap = ap.bitcast(F32)
        if len(free_shape) == 2:
            ap = ap.rearrange("p (a b) -> p a b", b=free_shape[1])
        elif len(free_shape) == 3:
            ap = ap.rearrange("p (a b c) -> p a b c", b=free_shape[1], c=free_shape[2])
        bufs = self.pages[o // self.PAGE:(o + nb - 1) // self.PAGE + 1]
        return Tmp(ap, bufs)


class _Stop(Exception):
    pass


def build(nseq=4, nlayers=2, dbg=False, stop=None):
    nc = bass.Bass("TRN2", target_bir_lowering=False)
    dr = {}

    def din(name, shape, dt=F32):
        dr[name] = nc.dram_tensor(name, list(shape), dt, kind="ExternalInput").ap()
        return dr[name]

    x_d = din("x", [nseq, SEQ, D])
    pos_d = din("positions", [nseq, SEQ], I32)
    nmw_d = din("norm_mix_w", [2, D])
    win_d = din("w_in", [2, D, NIN])
    hlb_d = din("hg_lower_bounds", [2, 512])
    hnw_d = din("hg_norm_w", [2, 512])
    rnw_d = din("ret_norm_w", [2, 1024])
    qnw_d = din("mla_q_norm_w", [2, 256])
    wuq_d = din("mla_w_uq", [2, 256, 768])
    kvnw_d = din("mla_kv_norm_w", [2, 128])
    wukv_d = din("mla_w_ukv", [2, 128, 1024])
    wba_d = din("w_br_a", [2, 512, D])
    wbb_d = din("w_br_b", [2, 1024, D])
    wbc_d = din("w_br_c", [2, 512, D])
    wout_d = din("w_out", [2, D, D])
    nfw_d = din("norm_ffn_w", [2, D])
    wfi_d = din("w_ffn_in", [2, D, 2 * DFF])
    wfo_d = din("w_ffn_out", [2, DFF, D])
    fnw_d = din("final_norm_w", [D])
    cident_d = din("c_ident", [128, 128], BF16)
    chgmask_d = din("c_hgmask", [128, 64], BF16)
    ccausal_d = din("c_causal", [128, 128], BF16)
    cones_d = din("c_ones", [128, 128], BF16)
    creset_d = din("c_reset", [128, 512])
    cdmask_d = din("c_dmask", [128, 4, 128])
    cqdec_d = din("c_qdec", [128, 4, 128])
    ckdec_d = din("c_kdec", [128, 4])
    cinvr_d = din("c_invr", [128, 64])
    cinvm_d = din("c_invm", [128, 32])
    out_d = nc.dram_tensor("out", [nseq, SEQ, D], F32, kind="ExternalOutput").ap()
    wscr = nc.dram_tensor("wscr", [2, NBLK, 128, SLOT], BF16, kind="Internal").ap()

    with ExitStack() as st:
        kk = K(nc, st)

        def sb(name, shape, dt, nb=1):
            t = st.enter_context(nc.sbuf_tensor(name, list(shape), dt))
            return PT(t, [Buf(f"{name}{i}") for i in range(nb)])

        def psb(name, shape, dt):
            t = st.enter_context(nc.psum_tensor(name, list(shape), dt))
            return PT(t, [Buf(name, psum=True)])

        xres = sb("xres", [128, NT, D], F32, NT)
        NSLOT = 3
        wsl = [sb(f"wsl{i}", [128, SLOT], BF16) for i in range(NSLOT)]
        wst = [Stream(kk, f"w{i}") for i in range(NSLOT)]
        hT = sb("hT", [128, 8, TT], BF16)
        kvnT = sb("kvnT", [128, SEQ], BF16, NT)
        kvtok = sb("kvtok", [128, NT, 128], BF16, NT)
        kpeT = sb("kpeT", [64, SEQ], BF16, NT)
        big = sb("big", [128, 24, TT], BF16, 4)
        yA, yB, yC, mTb = big.bufs
        ropeR = sb("ropeR", [128, NT, 2, 64], BF16)
        ropeM = sb("ropeM", [128, NT, 2, 32], BF16)
        shg_f = sb("shg_f", [128, 4, 128], F32, 4)
        shg_b = sb("shg_b", [128, 4, 128], BF16, 4)
        sr_f = sb("sr_f", [128, 4, 256], F32, 4)
        sr_b = sb("sr_b", [128, 4, 256], BF16, 4)
        ident = sb("ident", [128, 128], BF16)
        hgmask = sb("hgmask", [128, 64], BF16)
        causal = sb("causal", [128, 128], BF16)
        ones = sb("ones", [128, 128], BF16)
        resetc = sb("resetc", [128, 512], F32)
        dmask = sb("dmask", [128, 4, 128], F32)
        qdec = sb("qdec", [128, 4, 128], F32)
        kdecc = sb("kdecc", [128, 4], F32)
        invr = sb("invr", [128, 64], F32)
        invm = sb("invm", [128, 32], F32)
        gains = sb("gains", [128, 2, 32], F32)
        lbt = sb("lbt", [128, 2, 2, 4], F32)
        mhalf = sb("mhalf", [128, 8], F32)
        cols = sb("cols", [128, 64], F32)
        colbufs = [Buf(f"col{i}") for i in range(16)]
        colrr = [0]

        def newcol(n=4):
            i = colrr[0] % 16
            colrr[0] += 1
            return Tmp(cols.t[:, i * 4:i * 4 + n], [colbufs[i]])

        ar = Arena(nc, st, "arena", 22 * 1024)

        psT = [psb(f"psT{i}", [128, 1024], BF16) for i in range(2)]
        pmm = [psb(f"pmm{i}", [128, 512], F32) for i in range(3)]
        pacc = [psb(f"pacc{i}", [128, 512], F32) for i in range(2)]
        pS = psb("pS", [128, 512], F32)
        rr = {"T": 0, "mm": 0, "acc": 0, "wide": 0}
        pwide = pmm + pacc

        def nxt(kind):
            lst = {"T": psT, "mm": pmm, "acc": pacc, "wide": pwide}[kind]
            p = lst[rr[kind] % len(lst)]
            rr[kind] += 1
            return p

        def chk(name):
            if stop == name:
                if name in ("mla", "merge", "wout"):
                    kk.op("act", lambda h: h.activation(out=xres.t[:, 0:12, :].rearrange("p a b -> p (a b)"), in_=big.t[:].rearrange("p a b -> p (a b)"), func=AF.Copy), r=big.bufs, w=xres.bufs[0:12])
                if name == "rope":
                    kk.op("act", lambda h: h.activation(out=xres.t[:, 12:14, :].rearrange("p a b -> p (a b)"), in_=ropeR.t[:].rearrange("p a b c -> p (a b c)"), func=AF.Copy), r=ropeR, w=xres.bufs[12:14])
                    kk.op("act", lambda h: h.activation(out=xres.t[:, 14, :], in_=ropeM.t[:].rearrange("p a b c -> p (a b c)"), func=AF.Copy), r=ropeM, w=xres.bufs[14])
                raise _Stop()

        cst = Stream(kk, "const")
        xst = Stream(kk, "xload")
        osts = [Stream(kk, f"ostore{i}") for i in range(2)]
        pst_lds = [Stream(kk, f"pre_ld{i}") for i in range(2)]
        pst_sts = [Stream(kk, f"pre_st{i}") for i in range(2)]
        posst = Stream(kk, "posld")
        fnwst = Stream(kk, "fnwld")

        for t_, d_ in ((ident, cident_d), (hgmask, chgmask_d), (causal, ccausal_d), (ones, cones_d),
                       (resetc, creset_d), (dmask, cdmask_d), (qdec, cqdec_d), (kdecc, ckdec_d),
                       (invr, cinvr_d), (invm, cinvm_d)):
            kk.dma("sp", cst, t_.t[:], d_[:], w=t_, skip_same=(cst.cnt > 0))
        for l in range(2):
            for (src, n, c0) in ((nmw_d, 8, 0), (nfw_d, 8, 8), (hnw_d, 4, 16), (rnw_d, 8, 20),
                                 (qnw_d, 2, 28), (kvnw_d, 1, 30)):
                for k_ in range(n):
                    kk.dma("sp", cst, gains.t[:, l, c0 + k_:c0 + k_ + 1], src[l, k_ * 128:(k_ + 1) * 128].rearrange("(p o) -> p o", o=1),
                           w=gains, skip_same=True)
        kk.op("pool", lambda h: h.memset(mhalf.t[:], -0.5), w=mhalf)
        lbraw = ar.alloc([2, 4], F32)
        for l in range(2):
            for k_ in range(4):
                kk.dma("sp", cst, lbraw.ap[:, l, k_:k_ + 1], hlb_d[l, k_ * 128:(k_ + 1) * 128].rearrange("(p o) -> p o", o=1), w=lbraw,
                       skip_same=True)
        lbe = ar.alloc([2, 4], F32)
        lbs = ar.alloc([4], F32)
        lbp = ar.alloc([2, 4], F32)
        kk.op("act", lambda h: h.activation(out=lbe.ap, in_=lbraw.ap, func=AF.Exp), r=lbraw, w=lbe)
        kk.op("dve", lambda h: h.tensor_tensor(out=lbs.ap, in0=lbe.ap[:, 0, :], in1=lbe.ap[:, 1, :], op=ALU.add), r=lbe, w=lbs)
        kk.op("dve", lambda h: h.reciprocal(out=lbs.ap, in_=lbs.ap), r=lbs, w=lbs)
        for l in range(2):
            kk.op("dve", lambda h, l=l: h.tensor_tensor(out=lbp.ap[:, l, :], in0=lbe.ap[:, l, :], in1=lbs.ap, op=ALU.mult), r=[lbe, lbs], w=lbp)
        kk.op("dve", lambda h: h.tensor_tensor(out=lbt.t[:, 0, 0, :], in0=lbp.ap[:, 0, :], in1=lbp.ap[:, 0, :], op=ALU.subtract), r=lbp, w=lbt)
        cum1 = ar.alloc([4], F32)
        kk.op("dve", lambda h: h.tensor_tensor(out=cum1.ap, in0=lbp.ap[:, 0, :], in1=lbp.ap[:, 1, :], op=ALU.add), r=lbp, w=cum1)
        kk.op("dve", lambda h: h.tensor_tensor(out=lbt.t[:, 1, 0, :], in0=cum1.ap, in1=lbp.ap[:, 0, :], op=ALU.subtract), r=[cum1, lbp], w=lbt)
        for l in range(2):
            kk.op("dve", lambda h, l=l: h.tensor_scalar(out=lbt.t[:, l, 1, :], in0=lbt.t[:, l, 0, :], scalar1=-1.0, scalar2=1.0, op0=ALU.mult, op1=ALU.add), r=lbt, w=lbt)

        chk("consts")
        stg_f = [Tmp(xres.t[:, 5 * i:5 * i + 5, :].rearrange("p a b -> p (a b)"), xres.bufs[5 * i:5 * i + 5]) for i in range(2)]
        stg_b = [Tmp(xres.t[:, 10 + 3 * i:13 + 3 * i, :].rearrange("p a b -> p (a b)").bitcast(BF16)[:, 0:SLOT], xres.bufs[10 + 3 * i:13 + 3 * i]) for i in range(2)]
        cast_rr = [0]

        def cast(dst, src, gain):
            e = ("dve", "pool", "dve")[cast_rr[0] % 3]
            cast_rr[0] += 1
            if gain is None:
                if e == "act":
                    return ("act", lambda h: h.activation(out=dst, in_=src, func=AF.Copy))
                return (e, lambda h: h.tensor_copy(out=dst, in_=src))
            if e == "act":
                return ("act", lambda h: h.activation(out=dst, in_=src, func=AF.Copy, scale=gain))
            return (e, lambda h: h.tensor_scalar(out=dst, in0=src, scalar1=gain, scalar2=None, op0=ALU.mult))

        pre_i = [0]

        def prepass_block(l, name):
            i = pre_i[0] % 2
            pre_i[0] += 1
            sf, sbf = stg_f[i], stg_b[i]
            pst_ld, pst_st = pst_lds[i], pst_sts[i]
            pieces = []

            def g(c):
                return gains.t[:, l, c:c + 1]
            if name in WIN_C0:
                c0 = WIN_C0[name]
                ncol = 448 if name == "c" else 512
                kk.dma("sp", pst_ld, sf.ap[:, 0:8 * ncol].rearrange("p (k c) -> p k c", c=ncol),
                       win_d[l, :, c0:c0 + ncol].rearrange("(k p) c -> p k c", p=128), w=sf)
                for kc in range(8):
                    pieces.append((kc * ncol, ncol, kc * ncol, g(kc)))
            elif name == "mla":
                kk.dma("sp", pst_ld, sf.ap[:, 0:1536].rearrange("p (k c) -> p k c", c=768),
                       wuq_d[l].rearrange("(k p) c -> p k c", p=128), w=sf)
                kk.dma("sp", pst_ld, sf.ap[:, 1536:2560], wukv_d[l], w=sf, skip_same=True)
                for kc in range(2):
                    pieces.append((kc * 768, 768, kc * 768, g(28 + kc)))
                pieces.append((1536, 1024, 1536, g(30)))
            elif name.startswith("M"):
                j = int(name[1:])
                for b_ in range(3):
                    cg = 5568 + b_ * 1024 + j * 128
                    kk.dma("sp", pst_ld, sf.ap[:, 0:3072].rearrange("p (k b c) -> p k b c", b=3, c=128)[:, :, b_, :],
                           win_d[l, :, cg:cg + 128].rearrange("(k p) c -> p k c", p=128), w=sf, skip_same=(b_ > 0))
                kk.dma("sp", pst_ld, sf.ap[:, 3072:3584].rearrange("p (k c) -> p k c", c=128),
                       wba_d[l, :, j * 128:(j + 1) * 128].rearrange("(k p) c -> p k c", p=128), w=sf, skip_same=True)
                kk.dma("sp", pst_ld, sf.ap[:, 3584:4608].rearrange("p (k c) -> p k c", c=128),
                       wbb_d[l, :, j * 128:(j + 1) * 128].rearrange("(k p) c -> p k c", p=128), w=sf, skip_same=True)
                kk.dma("sp", pst_ld, sf.ap[:, 4608:5120].rearrange("p (k c) -> p k c", c=128),
                       wbc_d[l, :, j * 128:(j + 1) * 128].rearrange("(k p) c -> p k c", p=128), w=sf, skip_same=True)
                for kc in range(8):
                    pieces.append((kc * 384, 384, kc * 384, g(kc)))
                for kc in range(4):
                    pieces.append((3072 + kc * 128, 128, 3072 + kc * 128, g(16 + kc)))
                for kc in range(8):
                    pieces.append((3584 + kc * 128, 128, 3584 + kc * 128, g(20 + kc)))
                pieces.append((4608, 512, 4608, None))
            elif name.startswith("wout"):
                n = int(name[4:])
                kk.dma("sp", pst_ld, sf.ap[:, 0:4096].rearrange("p (k c) -> p k c", c=512),
                       wout_d[l, :, n * 512:(n + 1) * 512].rearrange("(k p) c -> p k c", p=128), w=sf)
                pieces.append((0, 4096, 0, None))
            elif name.startswith("ffin"):
                jb = int(name[4:])
                v = sf.ap[:, 0:4096].rearrange("p (k c) -> p k c", c=512)
                kk.dma("sp", pst_ld, v[:, :, 0:256],
                       wfi_d[l, :, jb * 256:(jb + 1) * 256].rearrange("(k p) c -> p k c", p=128), w=sf)
                kk.dma("sp", pst_ld, v[:, :, 256:512],
                       wfi_d[l, :, DFF + jb * 256:DFF + (jb + 1) * 256].rearrange("(k p) c -> p k c", p=128), w=sf, skip_same=True)
                for kc in range(8):
                    pieces.append((kc * 512, 512, kc * 512, g(8 + kc)))
            elif name.startswith("ffout"):
                n, kc0, nk = FFOUT_PARTS[int(name[5:])]
                kk.dma("sp", pst_ld, sf.ap[:, 0:nk * 512].rearrange("p (k c) -> p k c", c=512),
                       wfo_d[l, kc0 * 128:(kc0 + nk) * 128, n * 512:(n + 1) * 512].rearrange("(k p) c -> p k c", p=128), w=sf)
                pieces.append((0, nk * 512, 0, None))
            return (l, name, sf, sbf, pst_st, pieces)

        def prepass_finish(l, name, sf, sbf, pst_st, pieces):
            for (do, n, so, gn) in pieces:
                e, fn = cast(sbf.ap[:, do:do + n], sf.ap[:, so:so + n], gn)
                kk.op(e, fn, r=[sf, gains], w=sbf)
            if name == "mla":
                p = nxt("T")
                for hh in range(4):
                    kk.op("pe", lambda h, hh=hh: h.transpose(out=p.t[:, hh * 128:(hh + 1) * 128], in_=sbf.ap[:, 1536 + hh * 256:1536 + hh * 256 + 128], identity=ident.t[:]),
                          r=[sbf, ident], w=p, inc=(hh == 3))
                kk.op("dve", lambda h: h.tensor_copy(out=sbf.ap[:, 2560:3072], in_=p.t[:, 0:512]), r=p, w=sbf)
            sz = blk_size(name)
            kk.dma("act", pst_st, wscr[l, BIDX[name], :, 0:sz], sbf.ap[:, 0:sz], r=sbf)

        pend = None
        for l in range(nlayers):
            for name in BLK:
                cur = prepass_block(l, name)
                if pend is not None:
                    prepass_finish(*pend)
                pend = cur
        prepass_finish(*pend)
        for e in ("sp", "pool", "act"):
            kk.wait_all(e, [stg_f, stg_b])

        wq = []
        for s_ in range(nseq):
            for l in range(nlayers):
                for T in range(4):
                    for name in BLK:
                        wq.append((l, name))
        wstate = {"issued": 0, "used": 0}

        def w_issue(upto):
            while wstate["issued"] < min(upto, len(wq)):
                i = wstate["issued"]
                l, name = wq[i]
                sl = i % NSLOT
                sz = blk_size(name)
                kk.dma("sp", wst[sl], wsl[sl].t[:, 0:sz], wscr[l, BIDX[name], :, 0:sz], w=wsl[sl])
                wstate["issued"] += 1

        def wget(name, keep=0):
            i = wstate["used"]
            assert wq[i][1] == name, (wq[i], name)
            assert keep < NSLOT
            w_issue(i - keep + NSLOT)
            wstate["used"] += 1
            return wsl[i % NSLOT]

        def rstd_cols(ss, n, inv_d):
            t1 = newcol(n)
            kk.op("pool", lambda h: h.tensor_scalar(out=t1.ap, in0=ss.ap, scalar1=inv_d, scalar2=EPS, op0=ALU.mult, op1=ALU.add), r=ss, w=t1)
            t2 = newcol(n)
            kk.op("pool", lambda h: h.tensor_tensor(out=t2.ap, in0=t1.ap, in1=mhalf.t[:, 0:n], op=ALU.pow), r=[t1, mhalf], w=t2)
            return t2

        def norm_to_hT(T):
            ar.reset(0)
            junk = ar.alloc([D], BF16)
            hns = [ar.alloc([D], BF16) for _ in range(4)]
            ss = newcol(4)
            for j in range(4):
                t = 4 * T + j
                kk.op("act", lambda h, t=t, j=j: h.activation(out=junk.ap, in_=xres.t[:, t, :], func=AF.Square, accum_out=ss.ap[:, j:j + 1]), r=xres.bufs[t], w=[junk, ss])
            rs = rstd_cols(ss, 4, 1.0 / D)
            for j in range(4):
                t = 4 * T + j
                kk.op("dve", lambda h, t=t, j=j: h.tensor_scalar(out=hns[j].ap, in0=xres.t[:, t, :], scalar1=rs.ap[:, j:j + 1], scalar2=None, op0=ALU.mult), r=[xres.bufs[t], rs], w=hns[j])
            for j in range(4):
                p = nxt("T")
                for kc in range(8):
                    kk.op("pe", lambda h, kc=kc, j=j, p=p: h.transpose(out=p.t[:, kc * 128:(kc + 1) * 128], in_=hns[j].ap[:, kc * 128:(kc + 1) * 128], identity=ident.t[:]),
                          r=[hns[j], ident], w=p, inc=(kc == 7))
                kk.op("act", lambda h, j=j, p=p: h.activation(out=hT.t[:, :, j * 128:(j + 1) * 128], in_=p.t[:].rearrange("p (k c) -> p k c", c=128), func=AF.Copy), r=p, w=hT)

        def proj_tok(wap, ncol, j, p, rhsT=None):
            for kc in range(8):
                kk.op("pe", lambda h, kc=kc: h.matmul(p.t[:, 0:ncol], lhsT=hT.t[:, kc, j * 128:(j + 1) * 128], rhs=wap[:, kc, :], start=(kc == 0), stop=(kc == 7)),
                      r=[hT, curw[0]], w=p, inc=(kc == 7))

        def proj_feat(lhs_of_kc, p, nk=8, rhs_of_kc=None, rbufs=None):
            for kc in range(nk):
                rhs = hT.t[:, kc, :] if rhs_of_kc is None else rhs_of_kc(kc)
                kk.op("pe", lambda h, kc=kc, rhs=rhs: h.matmul(p.t[:, 0:TT], lhsT=lhs_of_kc(kc), rhs=rhs, start=(kc == 0), stop=(kc == nk - 1)),
                      r=[hT if rbufs is None else rbufs, curw[0]], w=p, inc=(kc == nk - 1))

        curw = [None]

        def transposes_to(src_of, n, dst_ap_of_group, rbufs, wbufs, evac="act", parts=128, width=128):
            i = 0
            while i < n:
                m = min(8, n - i)
                p = nxt("T")
                for q in range(m):
                    kk.op("pe", lambda h, q=q, i=i: h.transpose(out=p.t[0:width, q * 128:(q + 1) * 128], in_=src_of(i + q), identity=ident.t[:]),
                          r=[rbufs, ident], w=p, inc=(q == m - 1))
                dst_ap_of_group(i, m, p)
                i += m

        try:
          chk("prepass")
          for s_ in range(nseq):
              for q in range(4):
                  kk.dma("pool", xst, xres.t[:, 4 * q:4 * q + 4, :], x_d[s_, q * 512:(q + 1) * 512, :].rearrange("(t p) d -> p t d", p=128),
                         w=xres.bufs[4 * q:4 * q + 4], skip_same=(q > 0))
              ar.reset(0)
              chk("xload")
              posi = ar.alloc([NT], I32 if False else F32)
              posi_i = Tmp(posi.ap.bitcast(I32), posi.bufs)
              for t in range(NT):
                  kk.dma("pool", posst, posi_i.ap[:, t:t + 1], pos_d[s_, t * 128:(t + 1) * 128].rearrange("(p o) -> p o", o=1), w=posi, skip_same=(t > 0))
              posf = ar.alloc([NT], F32)
              kk.op("dve", lambda h: h.tensor_copy(out=posf.ap, in_=posi_i.ap), r=posi, w=posf)
              rope_base = ar.off
              for (tab, inv, half) in ((ropeR, invr, 64), (ropeM, invm, 32)):
                  ar.reset(rope_base)
                  ang = ar.alloc([NT, half], F32)
                  for t in range(NT):
                      kk.op("dve", lambda h, t=t: h.tensor_scalar(out=ang.ap[:, t, :], in0=inv.t[:, 0:half], scalar1=posf.ap[:, t:t + 1], scalar2=None, op0=ALU.mult), r=[inv, posf], w=ang)
                  a2 = ar.alloc([2, NT, half], F32)
                  inv2pi = float(1.0 / (2 * np.pi))
                  kk.op("dve", lambda h: h.tensor_scalar(out=a2.ap[:, 1], in0=ang.ap, scalar1=inv2pi, scalar2=None, op0=ALU.mult), r=ang, w=a2)
                  kk.op("dve", lambda h: h.tensor_scalar(out=a2.ap[:, 0], in0=ang.ap, scalar1=inv2pi, scalar2=0.25, op0=ALU.mult, op1=ALU.add), r=ang, w=a2)
                  kf = ar.alloc([2, NT, half], F32)
                  ki = Tmp(kf.ap.bitcast(I32), kf.bufs)
                  kk.op("dve", lambda h: h.tensor_copy(out=ki.ap, in_=a2.ap), r=a2, w=kf)
                  kf2 = ar.alloc([2, NT, half], F32)
                  kk.op("dve", lambda h: h.tensor_copy(out=kf2.ap, in_=ki.ap), r=kf, w=kf2)
                  kk.op("dve", lambda h: h.tensor_tensor(out=a2.ap, in0=a2.ap, in1=kf2.ap, op=ALU.subtract), r=[a2, kf2], w=a2)
                  kk.op("dve", lambda h: h.tensor_scalar(out=kf2.ap, in0=a2.ap, scalar1=0.5, scalar2=None, op0=ALU.is_gt), r=a2, w=kf2)
                  kk.op("dve", lambda h: h.tensor_tensor(out=a2.ap, in0=a2.ap, in1=kf2.ap, op=ALU.subtract), r=[a2, kf2], w=a2)
                  s2 = ar.alloc([2, NT, half], F32)
                  kk.op("act", lambda h: h.activation(out=s2.ap, in_=a2.ap, func=AF.Sin, scale=float(2 * np.pi * (1 - 1e-6))), r=a2, w=s2)
                  for c in range(2):
                      kk.op("dve", lambda h, c=c: h.tensor_copy(out=tab.t[:, :, c, :], in_=s2.ap[:, c]), r=s2, w=tab)

              chk("rope")
              for l in range(nlayers):
                  kk.op("pool", lambda h: h.memset(shg_f.t[:], 0.0), w=shg_f)
                  kk.op("pool", lambda h: h.memset(shg_b.t[:], 0.0), w=shg_b)
                  kk.op("pool", lambda h: h.memset(sr_f.t[:], 0.0), w=sr_f)
                  kk.op("pool", lambda h: h.memset(sr_b.t[:], 0.0), w=sr_b)
                  for T in range(4):
                      norm_to_hT(T)
                      chk("norm")
                      ar.reset(0)
                      v_hg = ar.alloc([4, 512], BF16)
                      gate_a = ar.alloc([4, 512], BF16)
                      y_a = ar.alloc([4, 512], BF16)
                      curw[0] = wget("hi")
                      wv = curw[0].t[:, 0:4096].rearrange("p (k c) -> p k c", c=512)
                      for j in range(4):
                          p = nxt("mm")
                          proj_tok(wv, 512, j, p)
                          kk.op("act", lambda h, j=j, p=p: h.activation(out=v_hg.ap[:, j, :], in_=p.t[:], func=AF.Copy), r=p, w=v_hg)
                      curw[0] = wget("hgate")
                      wv = curw[0].t[:, 0:4096].rearrange("p (k c) -> p k c", c=512)
                      for j in range(4):
                          p = nxt("mm")
                          proj_tok(wv, 512, j, p)
                          sg = ar.alloc([512], F32) if j == 0 else sg
                          kk.op("act", lambda h, p=p: h.activation(out=sg.ap, in_=p.t[:], func=AF.Sigmoid), r=p, w=sg)
                          kk.op("dve", lambda h, j=j, p=p: h.tensor_tensor(out=gate_a.ap[:, j, :], in0=p.t[:], in1=sg.ap, op=ALU.mult), r=[p, sg], w=gate_a)
                      wq_ = wget("hq")
                      wf_ = wget("hf", keep=1)
                      wqv = wq_.t[:, 0:4096].rearrange("p (k c) -> p k c", c=512)
                      wfv = wf_.t[:, 0:4096].rearrange("p (k c) -> p k c", c=512)
                      base_off = ar.off
                      t0 = ar.alloc([512], F32)
                      t1 = ar.alloc([512], F32)
                      t2 = ar.alloc([512], F32)
                      t3 = ar.alloc([512], F32)
                      q_f = ar.alloc([512], F32)
                      k_f = ar.alloc([512], F32)
                      ecs = [ar.alloc([512], F32) for _ in range(2)]
                      qes = [ar.alloc([512], BF16) for _ in range(2)]
                      kes = [ar.alloc([512], BF16) for _ in range(2)]
                      qcs = [ar.alloc([512], BF16) for _ in range(2)]
                      kdTs = [ar.alloc([512], BF16) for _ in range(2)]
                      kds = [ar.alloc([4, 128], BF16) for _ in range(2)]
                      ats = [ar.alloc([4, 64], BF16) for _ in range(2)]

                      def c3(ap):
                          return ap.rearrange("p (c t) -> p c t", t=64)
                      for hp in range(2):
                          pos_ = {}
                          for hh in range(2):
                              hd = 2 * hp + hh
                              ec, qe, ke, qc, kdT, kd, at = ecs[hh], qes[hh], kes[hh], qcs[hh], kdTs[hh], kds[hh], ats[hh]
                              curw[0] = wq_
                              p = nxt("mm")
                              proj_feat(lambda kc, hd=hd: wqv[:, kc, hd * 128:(hd + 1) * 128], p)
                              kk.op("act", lambda h, p=p: h.activation(out=t0.ap, in_=p.t[:], func=AF.Sigmoid), r=p, w=t0)
                              kk.op("dve", lambda h, p=p: h.tensor_tensor(out=q_f.ap, in0=p.t[:], in1=t0.ap, op=ALU.mult), r=[p, t0], w=q_f)
                              curw[0] = wf_
                              p = nxt("mm")
                              proj_feat(lambda kc, hd=hd: wfv[:, kc, hd * 128:(hd + 1) * 128], p)
                              kk.op("act", lambda h, p=p: h.activation(out=t0.ap, in_=p.t[:], func=AF.Sigmoid), r=p, w=t0)
                              kk.op("dve", lambda h, hd=hd: h.tensor_scalar(out=t1.ap, in0=t0.ap, scalar1=lbt.t[:, l, 1, hd:hd + 1], scalar2=lbt.t[:, l, 0, hd:hd + 1], op0=ALU.mult, op1=ALU.add), r=[t0, lbt], w=t1)
                              kk.op("act", lambda h: h.activation(out=t0.ap, in_=t1.ap, func=AF.Ln), r=t1, w=t0)
                              kk.op("dve", lambda h: h.tensor_scalar(out=k_f.ap, in0=t1.ap, scalar1=-1.0, scalar2=1.0, op0=ALU.mult, op1=ALU.add), r=t1, w=k_f)
                              kk.op("dve", lambda h: h.tensor_tensor_scan(out=t1.ap, data0=resetc.t[:], data1=t0.ap, initial=0.0, op0=ALU.mult, op1=ALU.add), r=[resetc, t0], w=t1)
                              kk.op("dve", lambda h: h.tensor_tensor(out=c3(t0.ap), in0=c3(t1.ap), in1=c3(t1.ap)[:, :, 31:32].broadcast_to([128, 8, 64]), op=ALU.subtract), r=t1, w=t0)
                              kk.op("dve", lambda h: h.tensor_tensor(out=c3(t2.ap), in0=c3(t1.ap), in1=c3(t1.ap)[:, :, 63:64].broadcast_to([128, 8, 64]), op=ALU.subtract), r=t1, w=t2)
                              kk.op("act", lambda h: h.activation(out=t3.ap, in_=t0.ap, func=AF.Exp), r=t0, w=t3)
                              kk.op("act", lambda h: h.activation(out=t0.ap, in_=t0.ap, func=AF.Exp, scale=-1.0), r=t0, w=t0)
                              kk.op("act", lambda h, ec=ec: h.activation(out=ec.ap, in_=t1.ap, func=AF.Exp), r=t1, w=ec)
                              kk.op("act", lambda h: h.activation(out=t2.ap, in_=t2.ap, func=AF.Exp, scale=-1.0), r=t2, w=t2)
                              kk.op("dve", lambda h, qe=qe: h.tensor_tensor(out=qe.ap, in0=q_f.ap, in1=t3.ap, op=ALU.mult), r=[q_f, t3], w=qe)
                              kk.op("dve", lambda h, ke=ke: h.tensor_tensor(out=ke.ap, in0=k_f.ap, in1=t0.ap, op=ALU.mult), r=[k_f, t0], w=ke)
                              kk.op("pool", lambda h, qc=qc, ec=ec: h.tensor_tensor(out=qc.ap, in0=q_f.ap, in1=ec.ap, op=ALU.mult), r=[q_f, ec], w=qc)
                              kk.op("dve", lambda h, kdT=kdT: h.tensor_tensor(out=kdT.ap, in0=k_f.ap, in1=t2.ap, op=ALU.mult), r=[k_f, t2], w=kdT)
                              pT_ = nxt("T")
                              for j in range(4):
                                  kk.op("pe", lambda h, j=j, kdT=kdT, pT_=pT_: h.transpose(out=pT_.t[:, j * 128:(j + 1) * 128], in_=kdT.ap[:, j * 128:(j + 1) * 128], identity=ident.t[:]),
                                        r=[kdT, ident], w=pT_, inc=(j == 3))
                              kk.op("act", lambda h, kd=kd, pT_=pT_: h.activation(out=kd.ap, in_=pT_.t[:, 0:512].rearrange("p (j c) -> p j c", c=128), func=AF.Copy), r=pT_, w=kd)
                              pa = nxt("mm")
                              for ci in range(8):
                                  j, hf_ = ci // 2, ci % 2
                                  kk.op("pe", lambda h, ci=ci, j=j, hf_=hf_, ke=ke, qe=qe, pa=pa: h.matmul(pa.t[hf_ * 64:(hf_ + 1) * 64, j * 64:(j + 1) * 64], lhsT=ke.ap[:, ci * 64:(ci + 1) * 64], rhs=qe.ap[:, ci * 64:(ci + 1) * 64], start=True, stop=True),
                                        r=[ke, qe], w=pa, inc=(ci == 7))
                              kk.op("dve", lambda h, at=at, pa=pa: h.tensor_tensor(out=at.ap, in0=pa.t[:, 0:256].rearrange("p (j t) -> p j t", t=64), in1=hgmask.t[:].unsqueeze(1).broadcast_to([128, 4, 64]), op=ALU.mult), r=[pa, hgmask], w=at)
                              pos_[hh] = nxt("acc")
                          for ci in range(8):
                              j, hf_ = ci // 2, ci % 2
                              P0, P1 = hf_ * 64, (hf_ + 1) * 64
                              for hh in range(2):
                                  hd = 2 * hp + hh
                                  ec, qc, kd, at, po = ecs[hh], qcs[hh], kds[hh], ats[hh], pos_[hh]
                                  kk.op("pe", lambda h, po=po, at=at, j=j, hd=hd, P0=P0, P1=P1: h.matmul(po.t[P0:P1, j * 128:(j + 1) * 128], lhsT=at.ap[P0:P1, j, :], rhs=v_hg.ap[P0:P1, j, hd * 128:(hd + 1) * 128], start=True, stop=False),
                                        r=[at, v_hg], w=po, inc=False)
                                  kk.op("pe", lambda h, po=po, qc=qc, ci=ci, j=j, hd=hd, P0=P0, P1=P1: h.matmul(po.t[P0:P1, j * 128:(j + 1) * 128], lhsT=qc.ap[:, ci * 64:(ci + 1) * 64], rhs=shg_b.t[:, hd, :], start=False, stop=True),
                                        r=[qc, shg_b.bufs[hd]], w=po, inc=True)
                                  pSx = nxt("mm")
                                  kk.op("pe", lambda h, kd=kd, j=j, hd=hd, P0=P0, P1=P1, pSx=pSx: h.matmul(pSx.t[:, 0:128], lhsT=kd.ap[P0:P1, j, :], rhs=v_hg.ap[P0:P1, j, hd * 128:(hd + 1) * 128], start=True, stop=True),
                                        r=[kd, v_hg], w=pSx)
                                  kk.op("dve", lambda h, ec=ec, ci=ci, hd=hd, pSx=pSx: h.scalar_tensor_tensor(out=shg_f.t[:, hd, :], in0=shg_f.t[:, hd, :], scalar=ec.ap[:, ci * 64 + 63:ci * 64 + 64], in1=pSx.t[:, 0:128], op0=ALU.mult, op1=ALU.add),
                                        r=[ec, pSx, shg_f.bufs[hd]], w=shg_f.bufs[hd])
                                  kk.op("act", lambda h, hd=hd: h.activation(out=shg_b.t[:, hd, :], in_=shg_f.t[:, hd, :], func=AF.Copy), r=shg_f.bufs[hd], w=shg_b.bufs[hd])
                          for hh in range(2):
                              hd = 2 * hp + hh
                              po = pos_[hh]
                              ss = newcol(4)
                              for j in range(4):
                                  kk.op("act", lambda h, j=j, po=po, ss=ss: h.activation(out=t3.ap[:, 0:128], in_=po.t[:, j * 128:(j + 1) * 128], func=AF.Square, accum_out=ss.ap[:, j:j + 1]), r=po, w=[t3, ss])
                              rs = rstd_cols(ss, 4, 1.0 / 128)
                              for j in range(4):
                                  kk.op("dve", lambda h, j=j, po=po, rs=rs, hd=hd: h.scalar_tensor_tensor(out=y_a.ap[:, j, hd * 128:(hd + 1) * 128], in0=po.t[:, j * 128:(j + 1) * 128], scalar=rs.ap[:, j:j + 1], in1=gate_a.ap[:, j, hd * 128:(hd + 1) * 128], op0=ALU.mult, op1=ALU.mult),
                                        r=[po, rs, gate_a], w=y_a)
                      for j in range(4):
                          p = nxt("T")
                          for kc in range(4):
                              kk.op("pe", lambda h, j=j, kc=kc, p=p: h.transpose(out=p.t[:, kc * 128:(kc + 1) * 128], in_=y_a.ap[:, j, kc * 128:(kc + 1) * 128], identity=ident.t[:]),
                                    r=[y_a, ident], w=p, inc=(kc == 3))
                          kk.op("act", lambda h, j=j, p=p: h.activation(out=big.t[:, 0:4, j * 128:(j + 1) * 128], in_=p.t[:, 0:512].rearrange("p (k c) -> p k c", c=128), func=AF.Copy), r=p, w=yA)

                      chk("hg")
                      wrq, wrk = wget("rq"), wget("rk", keep=1)
                      wrv0, wrv1, wrg0, wrg1 = None, None, None, None
                      ar.reset(0)
                      qT = ar.alloc([4, 4, 128], BF16)
                      kT = ar.alloc([4, 4, 128], BF16)
                      qdT = ar.alloc([4, 4, 128], BF16)
                      kdr = ar.alloc([4, 4, 128], BF16)
                      v_r = ar.alloc([4, 1024], BF16)
                      gate_b = ar.alloc([4, 1024], BF16)
                      tA = ar.alloc([512], F32)
                      tB = ar.alloc([512], F32)
                      qk_r = [ar.alloc([512], BF16) for _ in range(2)]

                      def v4(ap):
                          return ap.rearrange("p (h two d) -> p h two d", two=2, d=64)
                      for j in range(4):
                          t = 4 * T + j
                          cosb = ropeR.t[:, t, 0, :].unsqueeze(1).unsqueeze(1).broadcast_to([128, 4, 2, 64])
                          sinb = ropeR.t[:, t, 1, :].unsqueeze(1).broadcast_to([128, 4, 64])
                          for qi, wblk in enumerate((wrq, wrk)):
                              curw[0] = wblk
                              p = nxt("mm")
                              proj_tok(wblk.t[:, 0:4096].rearrange("p (k c) -> p k c", c=512), 512, j, p)
                              dst = qk_r[qi]
                              kk.op("dve", lambda h, p=p: h.tensor_tensor(out=v4(tA.ap), in0=v4(p.t[:]), in1=cosb, op=ALU.mult), r=[p, ropeR], w=tA)
                              kk.op("dve", lambda h, p=p: h.tensor_tensor(out=v4(tB.ap)[:, :, 0, :], in0=v4(p.t[:])[:, :, 1, :], in1=sinb, op=ALU.mult), r=[p, ropeR], w=tB)
                              kk.op("dve", lambda h, p=p: h.tensor_tensor(out=v4(tB.ap)[:, :, 1, :], in0=v4(p.t[:])[:, :, 0, :], in1=sinb, op=ALU.mult), r=[p, ropeR], w=tB)
                              kk.op("dve", lambda h, dst=dst: h.tensor_tensor(out=v4(dst.ap)[:, :, 0, :], in0=v4(tA.ap)[:, :, 0, :], in1=v4(tB.ap)[:, :, 0, :], op=ALU.subtract), r=[tA, tB], w=dst)
                              kk.op("dve", lambda h, dst=dst: h.tensor_tensor(out=v4(dst.ap)[:, :, 1, :], in0=v4(tA.ap)[:, :, 1, :], in1=v4(tB.ap)[:, :, 1, :], op=ALU.add), r=[tA, tB], w=dst)
                          chk("ret1")
                          q_r, k_r = qk_r
                          kk.op("dve", lambda h, j=j: h.tensor_tensor(out=kdr.ap[:, j], in0=k_r.ap.rearrange("p (h d) -> p h d", d=128), in1=kdecc.t[:].unsqueeze(2).broadcast_to([128, 4, 128]), op=ALU.mult), r=[k_r, kdecc], w=kdr)
                          p = nxt("T")
                          for hd in range(4):
                              kk.op("pe", lambda h, hd=hd, p=p: h.transpose(out=p.t[:, hd * 128:(hd + 1) * 128], in_=q_r.ap[:, hd * 128:(hd + 1) * 128], identity=ident.t[:]), r=[q_r, ident], w=p, inc=False)
                          for hd in range(4):
                              kk.op("pe", lambda h, hd=hd, p=p: h.transpose(out=p.t[:, 512 + hd * 128:512 + (hd + 1) * 128], in_=k_r.ap[:, hd * 128:(hd + 1) * 128], identity=ident.t[:]), r=[k_r, ident], w=p, inc=(hd == 3))
                          chk("ret1b")
                          kk.op("act", lambda h, j=j, p=p: h.activation(out=qT.ap[:, j], in_=p.t[:, 0:512].rearrange("p (h t) -> p h t", t=128), func=AF.Copy), r=p, w=qT)
                          kk.op("act", lambda h, j=j, p=p: h.activation(out=kT.ap[:, j], in_=p.t[:, 512:1024].rearrange("p (h t) -> p h t", t=128), func=AF.Copy), r=p, w=kT)
                          chk("ret1c")
                          kk.op("dve", lambda h, j=j, p=p: h.tensor_tensor(out=qdT.ap[:, j], in0=p.t[:, 0:512].rearrange("p (h t) -> p h t", t=128), in1=qdec.t[:], op=ALU.mult), r=[p, qdec], w=qdT)
                          chk("ret1d")
                          if j == 1:
                              chk("ret1e")
                      chk("ret2")
                      wrv0, wrv1 = wget("rv0"), wget("rv1", keep=1)
                      for j in range(4):
                          for n_, wblk in enumerate((wrv0, wrv1)):
                              curw[0] = wblk
                              p = nxt("mm")
                              proj_tok(wblk.t[:, 0:4096].rearrange("p (k c) -> p k c", c=512), 512, j, p)
                              kk.op("act", lambda h, j=j, n_=n_, p=p: h.activation(out=v_r.ap[:, j, n_ * 512:(n_ + 1) * 512], in_=p.t[:], func=AF.Copy), r=p, w=v_r)
                      wrg0, wrg1 = wget("rg0"), wget("rg1", keep=1)
                      for j in range(4):
                          for n_, wblk in enumerate((wrg0, wrg1)):
                              curw[0] = wblk
                              p = nxt("mm")
                              proj_tok(wblk.t[:, 0:4096].rearrange("p (k c) -> p k c", c=512), 512, j, p)
                              kk.op("act", lambda h, p=p: h.activation(out=tA.ap, in_=p.t[:], func=AF.Sigmoid), r=p, w=tA)
                              kk.op("dve", lambda h, j=j, n_=n_, p=p: h.tensor_tensor(out=gate_b.ap[:, j, n_ * 512:(n_ + 1) * 512], in0=p.t[:], in1=tA.ap, op=ALU.mult), r=[p, tA], w=gate_b)
                      chk("ret3")
                      at_r = ar.alloc([4, 128], BF16)
                      y_bs = [ar.alloc([1024], BF16) for _ in range(2)]
                      junk = ar.alloc([256], F32)
                      pend_yT = [None]

                      def emit_yT(j, y_b):
                          p = nxt("T")
                          for kc in range(8):
                              kk.op("pe", lambda h, kc=kc, p=p: h.transpose(out=p.t[:, kc * 128:(kc + 1) * 128], in_=y_b.ap[:, kc * 128:(kc + 1) * 128], identity=ident.t[:]), r=[y_b, ident], w=p, inc=(kc == 7))
                          kk.op("act", lambda h, j=j, p=p: h.activation(out=big.t[:, 4:12, j * 128:(j + 1) * 128], in_=p.t[:].rearrange("p (k c) -> p k c", c=128), func=AF.Copy), r=p, w=yB)
                      g128 = [float(np.exp(128.0 * LOG_GAMMA[h_])) for h_ in range(4)]
                      for j in range(4):
                          pa = nxt("mm")
                          for hd in range(4):
                              kk.op("pe", lambda h, j=j, hd=hd, pa=pa: h.matmul(pa.t[:, hd * 128:(hd + 1) * 128], lhsT=kT.ap[:, j, hd, :], rhs=qT.ap[:, j, hd, :], start=True, stop=True),
                                    r=[kT, qT], w=pa, inc=(hd == 3))
                          kk.op("dve", lambda h, pa=pa: h.tensor_tensor(out=at_r.ap, in0=pa.t[:].rearrange("p (h t) -> p h t", t=128), in1=dmask.t[:], op=ALU.mult), r=[pa, dmask], w=at_r)
                          pos2 = [nxt("acc"), nxt("acc")]
                          ss = newcol(4)
                          for hd in range(4):
                              po = pos2[hd // 2]
                              oc = (hd % 2) * 256
                              kk.op("pe", lambda h, j=j, hd=hd, po=po, oc=oc: h.matmul(po.t[:, oc:oc + 256], lhsT=at_r.ap[:, hd, :], rhs=v_r.ap[:, j, hd * 256:(hd + 1) * 256], start=True, stop=False),
                                    r=[at_r, v_r], w=po, inc=False)
                              kk.op("pe", lambda h, j=j, hd=hd, po=po, oc=oc: h.matmul(po.t[:, oc:oc + 256], lhsT=qdT.ap[:, j, hd, :], rhs=sr_b.t[:, hd, :], start=False, stop=True),
                                    r=[qdT, sr_b.bufs[hd]], w=po, inc=True)
                              pSx = nxt("mm") if hd % 2 == 0 else pS
                              kk.op("pe", lambda h, j=j, hd=hd, pSx=pSx: h.matmul(pSx.t[:, 0:256], lhsT=kdr.ap[:, j, hd, :], rhs=v_r.ap[:, j, hd * 256:(hd + 1) * 256], start=True, stop=True),
                                    r=[kdr, v_r], w=pSx)
                              kk.op("dve", lambda h, hd=hd, pSx=pSx: h.scalar_tensor_tensor(out=sr_f.t[:, hd, :], in0=sr_f.t[:, hd, :], scalar=g128[hd], in1=pSx.t[:, 0:256], op0=ALU.mult, op1=ALU.add),
                                    r=[pSx, sr_f.bufs[hd]], w=sr_f.bufs[hd])
                              kk.op("act", lambda h, hd=hd: h.activation(out=sr_b.t[:, hd, :], in_=sr_f.t[:, hd, :], func=AF.Copy), r=sr_f.bufs[hd], w=sr_b.bufs[hd])
                              kk.op("act", lambda h, hd=hd, po=po, oc=oc, ss=ss: h.activation(out=junk.ap, in_=po.t[:, oc:oc + 256], func=AF.Square, accum_out=ss.ap[:, hd:hd + 1]), r=po, w=[junk, ss])
                          chk("ret4")
                          if pend_yT[0] is not None:
                              emit_yT(*pend_yT[0])
                          y_b = y_bs[j % 2]
                          rs = rstd_cols(ss, 4, 1.0 / 256)
                          for hd in range(4):
                              po = pos2[hd // 2]
                              oc = (hd % 2) * 256
                              kk.op("dve", lambda h, j=j, hd=hd, po=po, oc=oc, rs=rs, y_b=y_b: h.scalar_tensor_tensor(out=y_b.ap[:, hd * 256:(hd + 1) * 256], in0=po.t[:, oc:oc + 256], scalar=rs.ap[:, hd:hd + 1], in1=gate_b.ap[:, j, hd * 256:(hd + 1) * 256], op0=ALU.mult, op1=ALU.mult),
                                    r=[po, rs, gate_b], w=y_b)
                          pend_yT[0] = (j, y_b)
                      emit_yT(*pend_yT[0])

                      chk("ret")
                      wc = wget("c")
                      wm = wget("mla", keep=1)
                      ar.reset(0)
                      qnT = ar.alloc([2, 512], BF16)
                      qabsT = ar.alloc([4, 512], BF16)
                      qpeT = ar.alloc([4, 512], BF16, parts=64)
                      qn = ar.alloc([256], BF16)
                      kpe = ar.alloc([64], BF16)
                      junk = ar.alloc([256], F32)
                      tA = ar.alloc([256], F32)
                      tB = ar.alloc([256], F32)
                      qpe = ar.alloc([256], BF16)
                      qnopeT = ar.alloc([512], BF16)
                      wcv = wc.t[:, 0:8 * 448].rearrange("p (k c) -> p k c", c=448)
                      wuq = wm.t[:, 0:1536].rearrange("p (k c) -> p k c", c=768)
                      wukv = wm.t[:, 1536:2560]
                      wukT = wm.t[:, 2560:3072].rearrange("p (h c) -> p h c", c=128)

                      def rope_small(dst, src_ap, nh, t, rb, wb):
                          def v(ap):
                              return ap.rearrange("p (h two d) -> p h two d", two=2, d=32)
                          cosb = ropeM.t[:, t, 0, :].unsqueeze(1).unsqueeze(1).broadcast_to([128, nh, 2, 32])
                          sinb = ropeM.t[:, t, 1, :].unsqueeze(1).broadcast_to([128, nh, 32])
                          n = nh * 64
                          kk.op("dve", lambda h: h.tensor_tensor(out=v(tA.ap[:, 0:n]), in0=v(src_ap), in1=cosb, op=ALU.mult), r=[rb, ropeM], w=tA)
                          kk.op("dve", lambda h: h.tensor_tensor(out=v(tB.ap[:, 0:n])[:, :, 0, :], in0=v(src_ap)[:, :, 1, :], in1=sinb, op=ALU.mult), r=[rb, ropeM], w=tB)
                          kk.op("dve", lambda h: h.tensor_tensor(out=v(tB.ap[:, 0:n])[:, :, 1, :], in0=v(src_ap)[:, :, 0, :], in1=sinb, op=ALU.mult), r=[rb, ropeM], w=tB)
                          kk.op("dve", lambda h: h.tensor_tensor(out=v(dst)[:, :, 0, :], in0=v(tA.ap[:, 0:n])[:, :, 0, :], in1=v(tB.ap[:, 0:n])[:, :, 0, :], op=ALU.subtract), r=[tA, tB], w=wb)
                          kk.op("dve", lambda h: h.tensor_tensor(out=v(dst)[:, :, 1, :], in0=v(tA.ap[:, 0:n])[:, :, 1, :], in1=v(tB.ap[:, 0:n])[:, :, 1, :], op=ALU.add), r=[tA, tB], w=wb)

                      qns = [ar.alloc([256], BF16) for _ in range(4)]
                      kpes = [ar.alloc([64], BF16) for _ in range(4)]
                      curw[0] = wc
                      ps_c = [nxt("wide") for _ in range(4)]
                      for j in range(4):
                          proj_tok(wcv, 448, j, ps_c[j])
                      ssq, sskv = newcol(4), newcol(4)
                      for j in range(4):
                          p = ps_c[j]
                          kk.op("act", lambda h, p=p, j=j: h.activation(out=junk.ap, in_=p.t[:, 0:256], func=AF.Square, accum_out=ssq.ap[:, j:j + 1]), r=p, w=[junk, ssq])
                          kk.op("act", lambda h, p=p, j=j: h.activation(out=junk.ap[:, 0:128], in_=p.t[:, 256:384], func=AF.Square, accum_out=sskv.ap[:, j:j + 1]), r=p, w=[junk, sskv])
                      rsq = rstd_cols(ssq, 4, 1.0 / 256)
                      rskv = rstd_cols(sskv, 4, 1.0 / 128)
                      for j in range(4):
                          t = 4 * T + j
                          p = ps_c[j]
                          kk.op("dve", lambda h, p=p, j=j: h.tensor_scalar(out=qns[j].ap, in0=p.t[:, 0:256], scalar1=rsq.ap[:, j:j + 1], scalar2=None, op0=ALU.mult), r=[p, rsq], w=qns[j])
                          kk.op("dve", lambda h, p=p, j=j, t=t: h.tensor_scalar(out=kvtok.t[:, t, :], in0=p.t[:, 256:384], scalar1=rskv.ap[:, j:j + 1], scalar2=None, op0=ALU.mult), r=[p, rskv], w=kvtok.bufs[t])
                          rope_small(kpes[j].ap, p.t[:, 384:448], 1, t, p, kpes[j])
                      for j in range(4):
                          t = 4 * T + j
                          qn_, kpe_ = qns[j], kpes[j]
                          pt_ = nxt("T")
                          kk.op("pe", lambda h, pt_=pt_, qn_=qn_: h.transpose(out=pt_.t[:, 0:128], in_=qn_.ap[:, 0:128], identity=ident.t[:]), r=[qn_, ident], w=pt_, inc=False)
                          kk.op("pe", lambda h, pt_=pt_, qn_=qn_: h.transpose(out=pt_.t[:, 128:256], in_=qn_.ap[:, 128:256], identity=ident.t[:]), r=[qn_, ident], w=pt_, inc=False)
                          kk.op("pe", lambda h, pt_=pt_, t=t: h.transpose(out=pt_.t[:, 256:384], in_=kvtok.t[:, t, :], identity=ident.t[:]), r=[kvtok.bufs[t], ident], w=pt_, inc=False)
                          kk.op("pe", lambda h, pt_=pt_, kpe_=kpe_: h.transpose(out=pt_.t[0:64, 384:512], in_=kpe_.ap, identity=ident.t[:]), r=[kpe_, ident], w=pt_, inc=True)
                          kk.op("act", lambda h, j=j, pt_=pt_: h.activation(out=qnT.ap[:, :, j * 128:(j + 1) * 128], in_=pt_.t[:, 0:256].rearrange("p (k c) -> p k c", c=128), func=AF.Copy), r=pt_, w=qnT)
                          kk.op("act", lambda h, t=t, pt_=pt_: h.activation(out=kvnT.t[:, t * 128:(t + 1) * 128], in_=pt_.t[:, 256:384], func=AF.Copy), r=pt_, w=kvnT.bufs[t])
                          kk.op("act", lambda h, t=t, pt_=pt_: h.activation(out=kpeT.t[:, t * 128:(t + 1) * 128], in_=pt_.t[0:64, 384:512], func=AF.Copy), r=pt_, w=kpeT.bufs[t])
                      curw[0] = wm
                      for hd in range(4):
                          p = nxt("mm")
                          proj_feat(lambda kc, hd=hd: wuq[:, kc, hd * 192:hd * 192 + 128], p, nk=2, rhs_of_kc=lambda kc: qnT.ap[:, kc, :], rbufs=qnT)
                          kk.op("act", lambda h, p=p: h.activation(out=qnopeT.ap, in_=p.t[:], func=AF.Copy, scale=S192), r=p, w=qnopeT)
                          p2 = nxt("mm")
                          kk.op("pe", lambda h, hd=hd, p2=p2: h.matmul(p2.t[:], lhsT=wukT[:, hd, :], rhs=qnopeT.ap, start=True, stop=True), r=[wm, qnopeT], w=p2)
                          kk.op("act", lambda h, hd=hd, p2=p2: h.activation(out=qabsT.ap[:, hd, :], in_=p2.t[:], func=AF.Copy), r=p2, w=qabsT)
                      wuq_pe = wuq.rearrange("p k (h d) -> p k h d", d=192)[:, :, :, 128:192]
                      qpes = [ar.alloc([256], BF16) for _ in range(4)]
                      ps_q = [nxt("wide") for _ in range(4)]
                      for j in range(4):
                          p = ps_q[j]
                          for kc in range(2):
                              kk.op("pe", lambda h, kc=kc, j=j, p=p: h.matmul(p.t[:, 0:256].rearrange("p (h d) -> p h d", d=64), lhsT=qnT.ap[:, kc, j * 128:(j + 1) * 128], rhs=wuq_pe[:, kc], start=(kc == 0), stop=(kc == 1)),
                                    r=[qnT, wm], w=p, inc=(kc == 1))
                      for j in range(4):
                          rope_small(qpes[j].ap, ps_q[j].t[:, 0:256], 4, 4 * T + j, ps_q[j], qpes[j])
                      for j in range(4):
                          qpe_ = qpes[j]
                          pt_ = nxt("T")
                          for hd in range(4):
                              kk.op("pe", lambda h, hd=hd, pt_=pt_, qpe_=qpe_: h.transpose(out=pt_.t[0:64, hd * 128:(hd + 1) * 128], in_=qpe_.ap[:, hd * 64:(hd + 1) * 64], identity=ident.t[:]), r=[qpe_, ident], w=pt_, inc=(hd == 3))
                          kk.op("act", lambda h, j=j, pt_=pt_: h.activation(out=qpeT.ap[:, :, j * 128:(j + 1) * 128], in_=pt_.t[0:64, 0:512].rearrange("p (h c) -> p h c", c=128), func=AF.Copy, scale=S192), r=pt_, w=qpeT)
                      pTs = [ar.alloc([512], BF16, align=True) for _ in range(3)]
                      ar.alloc([0], BF16, align=True)
                      olT = ar.alloc([512], BF16)
                      rinv = ar.alloc([512], F32)
                      prr = 0
                      for hd in range(4):
                          po_l, po_s = nxt("acc"), nxt("acc")
                          njb = 4 * T + 4
                          def emit_st(jb, hd=hd):
                              c0 = max(0, jb - 4 * T) * 128
                              p = nxt("mm")
                              kk.op("pe", lambda h, jb=jb, hd=hd, c0=c0, p=p: h.matmul(p.t[:, c0:512], lhsT=kvnT.t[:, jb * 128:(jb + 1) * 128], rhs=qabsT.ap[:, hd, c0:512], start=True, stop=False),
                                    r=[kvnT.bufs[jb], qabsT], w=p, inc=False)
                              kk.op("pe", lambda h, jb=jb, hd=hd, c0=c0, p=p: h.matmul(p.t[:, c0:512], lhsT=kpeT.t[:, jb * 128:(jb + 1) * 128], rhs=qpeT.ap[:, hd, c0:512], start=False, stop=True),
                                    r=[kpeT.bufs[jb], qpeT], w=p, inc=True)
                              return p
                          stq = [emit_st(0)]
                          for jb in range(njb):
                              c0 = max(0, jb - 4 * T) * 128
                              if jb + 1 < njb:
                                  stq.append(emit_st(jb + 1))
                              p = stq.pop(0)
                              pT_ = pTs[prr % 3]
                              prr += 1
                              kk.op("act", lambda h, c0=c0, p=p, pT_=pT_: h.activation(out=pT_.ap[:, c0:512], in_=p.t[:, c0:512], func=AF.Exp), r=p, w=pT_)
                              if jb >= 4 * T:
                                  kk.op("pool", lambda h, c0=c0, pT_=pT_: h.tensor_tensor(out=pT_.ap[:, c0:c0 + 128], in0=pT_.ap[:, c0:c0 + 128], in1=causal.t[:], op=ALU.mult), r=[pT_, causal], w=pT_)
                              kk.op("pe", lambda h, jb=jb, c0=c0, pT_=pT_, po_l=po_l: h.matmul(po_l.t[:, c0:512], lhsT=kvtok.t[:, jb, :], rhs=pT_.ap[:, c0:512], start=(jb == 0), stop=(jb == njb - 1)),
                                    r=[kvtok.bufs[jb], pT_], w=po_l, inc=(jb == njb - 1))
                              kk.op("pe", lambda h, jb=jb, c0=c0, pT_=pT_, po_s=po_s: h.matmul(po_s.t[:, c0:512], lhsT=ones.t[:], rhs=pT_.ap[:, c0:512], start=(jb == 0), stop=(jb == njb - 1)),
                                    r=[ones, pT_], w=po_s, inc=True)
                          kk.op("act", lambda h, po_l=po_l: h.activation(out=olT.ap, in_=po_l.t[:], func=AF.Copy), r=po_l, w=olT)
                          kk.op("dve", lambda h, po_s=po_s: h.reciprocal(out=rinv.ap, in_=po_s.t[:]), r=po_s, w=rinv)
                          p = nxt("mm")
                          kk.op("pe", lambda h, hd=hd, p=p: h.matmul(p.t[:], lhsT=wukv[:, hd * 256 + 128:hd * 256 + 256], rhs=olT.ap, start=True, stop=True), r=[wm, olT], w=p)
                          kk.op("dve", lambda h, hd=hd, p=p: h.tensor_tensor(out=big.t[:, 12 + hd, :], in0=p.t[:], in1=rinv.ap, op=ALU.mult), r=[p, rinv], w=yC)

                      chk("mla")
                      ar.reset(0)
                      gs = [ar.alloc([512], F32) for _ in range(3)]
                      ts = [ar.alloc([512], F32) for _ in range(3)]
                      m1 = ar.alloc([512], F32)
                      for jc in range(8):
                          wM = wget(f"M{jc}")
                          curw[0] = wM
                          wg = wM.t[:, 0:3072].rearrange("p (k b c) -> p k b c", b=3, c=128)
                          wb = wM.t[:, 3072:5120].rearrange("p (k c) -> p k c", c=128)
                          for b_, (k0, nk, yb) in enumerate(((0, 4, yA), (4, 8, yB), (12, 4, yC))):
                              pg = nxt("wide")
                              proj_feat(lambda kc, b_=b_: wg[:, kc, b_, :], pg)
                              kk.op("act", lambda h, b_=b_, pg=pg: h.activation(out=gs[b_].ap, in_=pg.t[:], func=AF.Sigmoid), r=pg, w=gs[b_])
                              pp = nxt("wide")
                              proj_feat(lambda kc, k0=k0: wb[:, k0 + kc, :], pp, nk=nk, rhs_of_kc=lambda kc, k0=k0: big.t[:, k0 + kc, :], rbufs=yb)
                              kk.op("dve", lambda h, b_=b_, pp=pp: h.tensor_tensor(out=ts[b_].ap, in0=pp.t[:], in1=gs[b_].ap, op=ALU.mult), r=[pp, gs[b_]], w=ts[b_])
                          kk.op("pool", lambda h: h.tensor_tensor(out=m1.ap, in0=ts[0].ap, in1=ts[1].ap, op=ALU.add), r=[ts[0], ts[1]], w=m1)
                          kk.op("pool", lambda h, jc=jc: h.tensor_tensor(out=big.t[:, 16 + jc, :], in0=m1.ap, in1=ts[2].ap, op=ALU.add), r=[m1, ts[2]], w=mTb)
                      chk("merge")
                      for n_ in range(2):
                          wo = wget(f"wout{n_}")
                          wov = wo.t[:, 0:4096].rearrange("p (k c) -> p k c", c=512)
                          for j in range(4):
                              t = 4 * T + j
                              p = nxt("wide")
                              for kc in range(8):
                                  kk.op("pe", lambda h, kc=kc, j=j, p=p: h.matmul(p.t[:], lhsT=big.t[:, 16 + kc, j * 128:(j + 1) * 128], rhs=wov[:, kc, :], start=(kc == 0), stop=(kc == 7)),
                                        r=[mTb, wo], w=p, inc=(kc == 7))
                              kk.op("dve", lambda h, t=t, n_=n_, p=p: h.tensor_tensor(out=xres.t[:, t, n_ * 512:(n_ + 1) * 512], in0=xres.t[:, t, n_ * 512:(n_ + 1) * 512], in1=p.t[:], op=ALU.add),
                                    r=[p, xres.bufs[t]], w=xres.bufs[t])
                      chk("wout")
                      norm_to_hT(T)
                      ar.reset(5120)
                      sgs = [ar.alloc([512], F32) for _ in range(2)]
                      t1s = [ar.alloc([512], F32) for _ in range(2)]
                      for jb in range(11):
                          wf = wget(f"ffin{jb}")
                          curw[0] = wf
                          wfv = wf.t[:, 0:4096].rearrange("p (k c) -> p k c", c=512)
                          for fc in range(2):
                              ff = jb * 2 + fc
                              pg = nxt("wide")
                              proj_feat(lambda kc, fc=fc: wfv[:, kc, fc * 128:(fc + 1) * 128], pg)
                              pu = nxt("wide")
                              proj_feat(lambda kc, fc=fc: wfv[:, kc, 256 + fc * 128:256 + (fc + 1) * 128], pu)
                              sg, t1_ = sgs[ff % 2], t1s[ff % 2]
                              kk.op("act", lambda h, pg=pg, sg=sg: h.activation(out=sg.ap, in_=pg.t[:], func=AF.Sigmoid), r=pg, w=sg)
                              kk.op("dve", lambda h, pg=pg, sg=sg, t1_=t1_: h.tensor_tensor(out=t1_.ap, in0=pg.t[:], in1=sg.ap, op=ALU.mult), r=[pg, sg], w=t1_)
                              kk.op("dve", lambda h, pu=pu, t1_=t1_, ff=ff: h.tensor_tensor(out=big.t[:, ff, :], in0=pu.t[:], in1=t1_.ap, op=ALU.mult), r=[pu, t1_], w=big.bufs)
                      for n_ in range(2):
                          wparts = [wget(f"ffout{n_ * 3 + i}", keep=i) for i in range(2)]
                          for j in range(4):
                              pass
                          pj = [nxt("mm"), nxt("mm"), nxt("mm"), pS]
                          kcs = [(0, 8), (8, 8), (16, 6)]
                          for pi in range(3):
                              wpart = wparts[pi] if pi < 2 else wget(f"ffout{n_ * 3 + 2}", keep=2)
                              kc0, nk = kcs[pi]
                              wpv = wpart.t[:, 0:nk * 512].rearrange("p (k c) -> p k c", c=512)
                              for j in range(4):
                                  for kc in range(nk):
                                      g = kc0 + kc
                                      kk.op("pe", lambda h, g=g, kc=kc, j=j, wpv=wpv: h.matmul(pj[j].t[:], lhsT=big.t[:, g, j * 128:(j + 1) * 128], rhs=wpv[:, kc, :], start=(g == 0), stop=(g == 21)),
                                            r=[big.bufs, wpart], w=pj[j], inc=(kc == nk - 1))
                          for j in range(4):
                              t = 4 * T + j
                              kk.op("dve", lambda h, t=t, n_=n_, j=j: h.tensor_tensor(out=xres.t[:, t, n_ * 512:(n_ + 1) * 512], in0=xres.t[:, t, n_ * 512:(n_ + 1) * 512], in1=pj[j].t[:], op=ALU.add),
                                    r=[pj[j], xres.bufs[t]], w=xres.bufs[t])
              ar.reset(0)
              obuf = [ar.alloc([D], F32) for _ in range(2)]
              junk = ar.alloc([D], BF16)
              fnw = ar.alloc([D], F32)
              kk.dma("pool", fnwst, fnw.ap, fnw_d.partition_broadcast(128), w=fnw)
              for t in range(NT):
                  xb = xres.bufs[t]
                  ss = newcol(1)
                  kk.op("act", lambda h, t=t, ss=ss: h.activation(out=junk.ap, in_=xres.t[:, t, :], func=AF.Square, accum_out=ss.ap), r=xb, w=[junk, ss])
                  rs = rstd_cols(ss, 1, 1.0 / D)
                  ob = obuf[t % 2]
                  kk.op("dve", lambda h, t=t, rs=rs, ob=ob: h.scalar_tensor_tensor(out=ob.ap, in0=xres.t[:, t, :], scalar=rs.ap, in1=fnw.ap, op0=ALU.mult, op1=ALU.mult), r=[xb, rs, fnw], w=ob)
                  kk.dma("pool", osts[t % 2], out_d[s_, t * 128:(t + 1) * 128, :], ob.ap, r=ob)
        except _Stop:
            for t in range(NT):
                kk.dma("sp", osts[t % 2], out_d[0, t * 128:(t + 1) * 128, :], xres.t[:, t, :], r=xres.bufs[t])
        for ost in osts:
            kk.E["pool"].h.wait_ge(ost.sem, ost.cnt)
            kk.E["sp"].h.wait_ge(ost.sem, ost.cnt)
        build.info = dict(ninst=kk.ninst, nsem=kk.nsem, arena_hi=ar.hi, sbuf_left=nc.sbuf_bytes_remaining)
    return nc


def host_consts():
    bf = ml_dtypes.bfloat16
    c = {}
    c["c_ident"] = np.eye(128, dtype=np.float32).astype(bf)
    s = np.arange(128)[:, None] % 64
    t = np.arange(64)[None, :]
    c["c_hgmask"] = (s <= t).astype(np.float32).astype(bf)
    s = np.arange(128)[:, None]
    t = np.arange(128)[None, :]
    c["c_causal"] = (s <= t).astype(np.float32).astype(bf)
    c["c_ones"] = np.ones((128, 128), np.float32).astype(bf)
    r = np.ones((128, 512), np.float32)
    r[:, ::64] = 0.0
    c["c_reset"] = r
    lg = np.array(LOG_GAMMA, np.float64)
    dm = np.zeros((128, 4, 128), np.float64)
    for h in range(4):
        dm[:, h, :] = np.where(s <= t, np.exp((t - s) * lg[h]), 0.0) * (128.0 ** -0.5)
    c["c_dmask"] = dm.astype(np.float32)
    qd = np.zeros((128, 4, 128), np.float64)
    for h in range(4):
        qd[:, h, :] = np.exp((np.arange(128)[None, :] + 1.0) * lg[h])
    c["c_qdec"] = qd.astype(np.float32)
    kd = np.zeros((128, 4), np.float64)
    for h in range(4):
        kd[:, h] = np.exp((127.0 - np.arange(128)) * lg[h]) * (128.0 ** -0.5)
    c["c_kdec"] = kd.astype(np.float32)
    c["c_invr"] = np.broadcast_to((10000.0 ** (-np.arange(64, dtype=np.float32) / 64)).astype(np.float32), (128, 64)).copy()
    c["c_invm"] = np.broadcast_to((10000.0 ** (-np.arange(32, dtype=np.float32) / 32)).astype(np.float32), (128, 32)).copy()
    return c


_NC_CACHE = {}


def kernel(**inputs):
    ncores = 8
    B = inputs["x"].shape[0]
    nseq = B // ncores
    key = (nseq, 2)
    if key not in _NC_CACHE:
        _NC_CACHE[key] = build(nseq=nseq, nlayers=2)
    nc = _NC_CACHE[key]
    consts = host_consts()
    in_maps = []
    for c in range(ncores):
        m = {}
        for k, v in inputs.items():
            v = np.asarray(v)
            if k in ("x", "positions"):
                m[k] = np.ascontiguousarray(v[c * nseq:(c + 1) * nseq])
            else:
                m[k] = np.ascontiguousarray(v)
        m.update(consts)
        in_maps.append(m)
    res = run_bass_kernel_spmd(nc, in_maps, core_ids=list(range(ncores)))
    out = np.concatenate([np.asarray(r["out"]) for r in res.results], axis=0)
    return out.astype(np.float32)
```

```python
import numpy as np
import ml_dtypes
from contextlib import ExitStack
import concourse.bass as bass
import concourse.mybir as mybir
from concourse.bass_utils import run_bass_kernel_spmd

F32, BF16, I32 = mybir.dt.float32, mybir.dt.bfloat16, mybir.dt.int32
AF = mybir.ActivationFunctionType
ALU = mybir.AluOpType

D = 1024
SEQ = 2048
NT = 16
TT = 512
NIN = 8640
DFF = 2816
EPS = 1e-6
S192 = 192.0 ** -0.5
LOG_GAMMA = [float(np.log1p(-(2.0 ** (-5.0 - h)))) for h in range(4)]
SLOT = 5120

BLK = ["hi", "hgate", "hq", "hf", "rq", "rk", "rv0", "rv1", "rg0", "rg1", "c", "mla"] + \
      [f"M{j}" for j in range(8)] + ["wout0", "wout1"] + [f"ffin{j}" for j in range(11)] + \
      [f"ffout{i}" for i in range(6)]
BIDX = {n: i for i, n in enumerate(BLK)}
NBLK = len(BLK)
WIN_C0 = {"hq": 0, "hf": 512, "hi": 1024, "hgate": 1536, "rq": 2048, "rk": 2560, "rv0": 3072,
          "rv1": 3584, "rg0": 4096, "rg1": 4608, "c": 5120}
FFOUT_PARTS = [(n, kc0, nk) for n in range(2) for (kc0, nk) in ((0, 8), (8, 8), (16, 6))]


def blk_size(name):
    if name == "c":
        return 8 * 448
    if name == "mla":
        return 3072
    if name.startswith("M"):
        return 5120
    if name.startswith("ffout"):
        return FFOUT_PARTS[int(name[5:])][2] * 512
    return 4096


class Buf:
    __slots__ = ("name", "w", "r", "psum")

    def __init__(self, name, psum=False):
        self.name = name
        self.w = None
        self.r = {}
        self.psum = psum


def flat(x):
    out = []

    def rec(y):
        if y is None:
            return
        if isinstance(y, Buf):
            out.append(y)
        elif hasattr(y, "bufs"):
            out.extend(y.bufs)
        else:
            for z in y:
                rec(z)
    rec(x)
    return out


class Eng:
    def __init__(self, h, key, is_pe=False):
        self.h = h
        self.key = key
        self.is_pe = is_pe
        self.sem = None
        self.cnt = 0
        self.seen = {}
        self.pend = False


class Stream:
    def __init__(self, kk, name):
        self.sem = kk.newsem(name)
        self.cnt = 0
        self.key = ("dma", name)
        self.gb = []


class K:
    SEM_LIMIT = 24000

    def __init__(self, nc, st):
        self.nc = nc
        self.st = st
        self.nsem = 0
        self.E = {}
        for key, h in (("pe", nc.tensor), ("act", nc.scalar), ("dve", nc.vector),
                       ("pool", nc.gpsimd), ("sp", nc.sync)):
            e = Eng(h, key, key == "pe")
            e.sem = self.newsem(key)
            self.E[key] = e
        self.ninst = 0

    def newsem(self, name):
        self.nsem += 1
        return self.st.enter_context(self.nc.semaphore(f"s{self.nsem}_{name}"))

    def _wait(self, E, r, w, skip_key=None):
        deps = {}

        def add(t):
            if t is None:
                return
            sem, val, key = t
            if key == E.key and E.is_pe:
                return
            if skip_key is not None and key == skip_key:
                return
            k = id(sem)
            if k not in deps or deps[k][1] < val:
                deps[k] = (sem, val)
        for b in r:
            add(b.w)
        for b in w:
            add(b.w)
            for k, t in b.r.items():
                add(t)
        for k, (sem, val) in deps.items():
            if E.seen.get(k, 0) < val:
                E.h.wait_ge(sem, val)
                E.seen[k] = val

    def op(self, eng, fn, r=(), w=(), inc=True):
        E = self.E[eng]
        r = flat(r)
        w = flat(w)
        w = w + [b for b in r if b.psum and b not in w]
        r = [b for b in r if not b.psum]
        self._wait(E, r, w)
        ins = fn(E.h)
        self.ninst += 1
        if inc:
            if E.cnt >= self.SEM_LIMIT and not E.pend:
                E.sem = self.newsem(E.key)
                E.cnt = 0
            E.cnt += 1
            ins.then_inc(E.sem, 1)
            tick = (E.sem, E.cnt, E.key)
            E.pend = False
        else:
            assert E.is_pe
            tick = (E.sem, E.cnt + 1, E.key)
            E.pend = True
        for b in r:
            b.r[E.key] = tick
        for b in w:
            b.w = tick
            b.r = {}
        return ins

    def dma(self, q, stream, out, in_, r=(), w=(), skip_same=False, **kw):
        E = self.E[q]
        r = flat(r)
        w = flat(w)
        self._wait(E, r, w, skip_key=stream.key if skip_same else None)
        ins = E.h.dma_start(out=out, in_=in_, **kw)
        self.ninst += 1
        stream.cnt += 16
        ins.then_inc(stream.sem, 16)
        tick = (stream.sem, stream.cnt, stream.key)
        if not skip_same:
            stream.gb = []
        for b in w:
            if b not in stream.gb:
                stream.gb.append(b)
        for b in r:
            b.r[stream.key] = tick
        for b in stream.gb:
            b.w = tick
            b.r = {}
        return ins

    def wait_all(self, eng, bufs):
        E = self.E[eng]
        self._wait(E, flat(bufs), flat(bufs))


class PT:
    def __init__(self, t, bufs):
        self.t = t
        self.bufs = bufs


class Tmp:
    def __init__(self, ap, bufs):
        self.ap = ap
        self.bufs = bufs


class Arena:
    PAGE = 512

    def __init__(self, nc, st, name, nelem_bf16):
        self.t = st.enter_context(nc.sbuf_tensor(name, [128, nelem_bf16], BF16))
        self.n = nelem_bf16
        self.pages = [Buf(f"{name}_p{i}") for i in range((nelem_bf16 + self.PAGE - 1) // self.PAGE)]
        self.off = 0
        self.hi = 0

    def reset(self, off=0):
        self.off = off

    def alloc(self, free_shape, dt, parts=128, align=False):
        if align:
            self.off = (self.off + self.PAGE - 1) // self.PAGE * self.PAGE
        n = int(np.prod(free_shape))
        nb = n * (2 if dt == F32 else 1)
        if dt == F32 and self.off % 2:
            self.off += 1
        o = self.off
        assert o + nb <= self.n, f"arena overflow {o + nb} > {self.n}"
        self.off += nb
        self.hi = max(self.hi, self.off)
        ap = self.t[0:parts, o:o + nb]
        if dt == F32:
            ap = ap.bitcast(F32)
        if len(free_shape) == 2:
            ap = ap.rearrange("p (a b) -> p a b", b=free_shape[1])
        elif len(free_shape) == 3:
            ap = ap.rearrange("p (a b c) -> p a b c", b=free_shape[1], c=free_shape[2])
        bufs = self.pages[o // self.PAGE:(o + nb - 1) // self.PAGE + 1]
        return Tmp(ap, bufs)


class _Stop(Exception):
    pass


def build(nseq=4, nlayers=2, dbg=False, stop=None):
    nc = bass.Bass("TRN2", target_bir_lowering=False)
    dr = {}

    def din(name, shape, dt=F32):
        dr[name] = nc.dram_tensor(name, list(shape), dt, kind="ExternalInput").ap()
        return dr[name]

    x_d = din("x", [nseq, SEQ, D])
    pos_d = din("positions", [nseq, SEQ], I32)
    nmw_d = din("norm_mix_w", [2, D])
    win_d = din("w_in", [2, D, NIN])
    hlb_d = din("hg_lower_bounds", [2, 512])
    hnw_d = din("hg_norm_w", [2, 512])
    rnw_d = din("ret_norm_w", [2, 1024])
    qnw_d = din("mla_q_norm_w", [2, 256])
    wuq_d = din("mla_w_uq", [2, 256, 768])
    kvnw_d = din("mla_kv_norm_w", [2, 128])
    wukv_d = din("mla_w_ukv", [2, 128, 1024])
    wba_d = din("w_br_a", [2, 512, D])
    wbb_d = din("w_br_b", [2, 1024, D])
    wbc_d = din("w_br_c", [2, 512, D])
    wout_d = din("w_out", [2, D, D])
    nfw_d = din("norm_ffn_w", [2, D])
    wfi_d = din("w_ffn_in", [2, D, 2 * DFF])
    wfo_d = din("w_ffn_out", [2, DFF, D])
    fnw_d = din("final_norm_w", [D])
    cident_d = din("c_ident", [128, 128], BF16)
    chgmask_d = din("c_hgmask", [128, 64], BF16)
    ccausal_d = din("c_causal", [128, 128], BF16)
    cones_d = din("c_ones", [128, 128], BF16)
    creset_d = din("c_reset", [128, 512])
    cdmask_d = din("c_dmask", [128, 4, 128])
    cqdec_d = din("c_qdec", [128, 4, 128])
    ckdec_d = din("c_kdec", [128, 4])
    cinvr_d = din("c_invr", [128, 64])
    cinvm_d = din("c_invm", [128, 32])
    out_d = nc.dram_tensor("out", [nseq, SEQ, D], F32, kind="ExternalOutput").ap()
    wscr = nc.dram_tensor("wscr", [2, NBLK, 128, SLOT], BF16, kind="Internal").ap()

    with ExitStack() as st:
        kk = K(nc, st)

        def sb(name, shape, dt, nb=1):
            t = st.enter_context(nc.sbuf_tensor(name, list(shape), dt))
            return PT(t, [Buf(f"{name}{i}") for i in range(nb)])

        def psb(name, shape, dt):
            t = st.enter_context(nc.psum_tensor(name, list(shape), dt))
            return PT(t, [Buf(name, psum=True)])

        xres = sb("xres", [128, NT, D], F32, NT)
        NSLOT = 3
        wsl = [sb(f"wsl{i}", [128, SLOT], BF16) for i in range(NSLOT)]
        wst = [Stream(kk, f"w{i}") for i in range(NSLOT)]
        hT = sb("hT", [128, 8, TT], BF16)
        kvnT = sb("kvnT", [128, SEQ], BF16, NT)
        kvtok = sb("kvtok", [128, NT, 128], BF16, NT)
        kpeT = sb("kpeT", [64, SEQ], BF16, NT)
        big = sb("big", [128, 24, TT], BF16, 4)
        yA, yB, yC, mTb = big.bufs
        ropeR = sb("ropeR", [128, NT, 2, 64], BF16)
        ropeM = sb("ropeM", [128, NT, 2, 32], BF16)
        shg_f = sb("shg_f", [128, 4, 128], F32, 4)
        shg_b = sb("shg_b", [128, 4, 128], BF16, 4)
        sr_f = sb("sr_f", [128, 4, 256], F32, 4)
        sr_b = sb("sr_b", [128, 4, 256], BF16, 4)
        ident = sb("ident", [128, 128], BF16)
        hgmask = sb("hgmask", [128, 64], BF16)
        causal = sb("causal", [128, 128], BF16)
        ones = sb("ones", [128, 128], BF16)
        resetc = sb("resetc", [128, 512], F32)
        dmask = sb("dmask", [128, 4, 128], F32)
        qdec = sb("qdec", [128, 4, 128], F32)
        kdecc = sb("kdecc", [128, 4], F32)
        invr = sb("invr", [128, 64], F32)
        invm = sb("invm", [128, 32], F32)
        gains = sb("gains", [128, 2, 32], F32)
        lbt = sb("lbt", [128, 2, 2, 4], F32)
        mhalf = sb("mhalf", [128, 8], F32)
        cols = sb("cols", [128, 64], F32)
        colbufs = [Buf(f"col{i}") for i in range(16)]
        colrr = [0]

        def newcol(n=4):
            i = colrr[0] % 16
            colrr[0] += 1
            return Tmp(cols.t[:, i * 4:i * 4 + n], [colbufs[i]])

        ar = Arena(nc, st, "arena", 22 * 1024)

        psT = [psb(f"psT{i}", [128, 1024], BF16) for i in range(2)]
        pmm = [psb(f"pmm{i}", [128, 512], F32) for i in range(3)]
        pacc = [psb(f"pacc{i}", [128, 512], F32) for i in range(2)]
        pS = psb("pS", [128, 512], F32)
        rr = {"T": 0, "mm": 0, "acc": 0, "wide": 0}
        pwide = pmm + pacc

        def nxt(kind):
            lst = {"T": psT, "mm": pmm, "acc": pacc, "wide": pwide}[kind]
            p = lst[rr[kind] % len(lst)]
            rr[kind] += 1
            return p

        def chk(name):
            if stop == name:
                if name in ("mla", "merge", "wout"):
                    kk.op("act", lambda h: h.activation(out=xres.t[:, 0:12, :].rearrange("p a b -> p (a b)"), in_=big.t[:].rearrange("p a b -> p (a b)"), func=AF.Copy), r=big.bufs, w=xres.bufs[0:12])
                if name == "rope":
                    kk.op("act", lambda h: h.activation(out=xres.t[:, 12:14, :].rearrange("p a b -> p (a b)"), in_=ropeR.t[:].rearrange("p a b c -> p (a b c)"), func=AF.Copy), r=ropeR, w=xres.bufs[12:14])
                    kk.op("act", lambda h: h.activation(out=xres.t[:, 14, :], in_=ropeM.t[:].rearrange("p a b c -> p (a b c)"), func=AF.Copy), r=ropeM, w=xres.bufs[14])
                raise _Stop()

        cst = Stream(kk, "const")
        xst = Stream(kk, "xload")
        osts = [Stream(kk, f"ostore{i}") for i in range(2)]
        pst_lds = [Stream(kk, f"pre_ld{i}") for i in range(2)]
        pst_sts = [Stream(kk, f"pre_st{i}") for i in range(2)]
        posst = Stream(kk, "posld")
        fnwst = Stream(kk, "fnwld")

        for t_, d_ in ((ident, cident_d), (hgmask, chgmask_d), (causal, ccausal_d), (ones, cones_d),
                       (resetc, creset_d), (dmask, cdmask_d), (qdec, cqdec_d), (kdecc, ckdec_d),
                       (invr, cinvr_d), (invm, cinvm_d)):
            kk.dma("sp", cst, t_.t[:], d_[:], w=t_, skip_same=(cst.cnt > 0))
        for l in range(2):
            for (src, n, c0) in ((nmw_d, 8, 0), (nfw_d, 8, 8), (hnw_d, 4, 16), (rnw_d, 8, 20),
                                 (qnw_d, 2, 28), (kvnw_d, 1, 30)):
                for k_ in range(n):
                    kk.dma("sp", cst, gains.t[:, l, c0 + k_:c0 + k_ + 1], src[l, k_ * 128:(k_ + 1) * 128].rearrange("(p o) -> p o", o=1),
                           w=gains, skip_same=True)
        kk.op("pool", lambda h: h.memset(mhalf.t[:], -0.5), w=mhalf)
        lbraw = ar.alloc([2, 4], F32)
        for l in range(2):
            for k_ in range(4):
                kk.dma("sp", cst, lbraw.ap[:, l, k_:k_ + 1], hlb_d[l, k_ * 128:(k_ + 1) * 128].rearrange("(p o) -> p o", o=1), w=lbraw,
                       skip_same=True)
        lbe = ar.alloc([2, 4], F32)
        lbs = ar.alloc([4], F32)
        lbp = ar.alloc([2, 4], F32)
        kk.op("act", lambda h: h.activation(out=lbe.ap, in_=lbraw.ap, func=AF.Exp), r=lbraw, w=lbe)
        kk.op("dve", lambda h: h.tensor_tensor(out=lbs.ap, in0=lbe.ap[:, 0, :], in1=lbe.ap[:, 1, :], op=ALU.add), r=lbe, w=lbs)
        kk.op("dve", lambda h: h.reciprocal(out=lbs.ap, in_=lbs.ap), r=lbs, w=lbs)
        for l in range(2):
            kk.op("dve", lambda h, l=l: h.tensor_tensor(out=lbp.ap[:, l, :], in0=lbe.ap[:, l, :], in1=lbs.ap, op=ALU.mult), r=[lbe, lbs], w=lbp)
        kk.op("dve", lambda h: h.tensor_tensor(out=lbt.t[:, 0, 0, :], in0=lbp.ap[:, 0, :], in1=lbp.ap[:, 0, :], op=ALU.subtract), r=lbp, w=lbt)
        cum1 = ar.alloc([4], F32)
        kk.op("dve", lambda h: h.tensor_tensor(out=cum1.ap, in0=lbp.ap[:, 0, :], in1=lbp.ap[:, 1, :], op=ALU.add), r=lbp, w=cum1)
        kk.op("dve", lambda h: h.tensor_tensor(out=lbt.t[:, 1, 0, :], in0=cum1.ap, in1=lbp.ap[:, 0, :], op=ALU.subtract), r=[cum1, lbp], w=lbt)
        for l in range(2):
            kk.op("dve", lambda h, l=l: h.tensor_scalar(out=lbt.t[:, l, 1, :], in0=lbt.t[:, l, 0, :], scalar1=-1.0, scalar2=1.0, op0=ALU.mult, op1=ALU.add), r=lbt, w=lbt)

        chk("consts")
        stg_f = [Tmp(xres.t[:, 5 * i:5 * i + 5, :].rearrange("p a b -> p (a b)"), xres.bufs[5 * i:5 * i + 5]) for i in range(2)]
        stg_b = [Tmp(xres.t[:, 10 + 3 * i:13 + 3 * i, :].rearrange("p a b -> p (a b)").bitcast(BF16)[:, 0:SLOT], xres.bufs[10 + 3 * i:13 + 3 * i]) for i in range(2)]
        cast_rr = [0]

        def cast(dst, src, gain):
            e = ("dve", "act")[cast_rr[0] % 2]
            cast_rr[0] += 1
            if gain is None:
                if e == "act":
                    return ("act", lambda h: h.activation(out=dst, in_=src, func=AF.Copy))
                return (e, lambda h: h.tensor_copy(out=dst, in_=src))
            if e == "act":
                return ("act", lambda h: h.activation(out=dst, in_=src, func=AF.Copy, scale=gain))
            return (e, lambda h: h.tensor_scalar(out=dst, in0=src, scalar1=gain, scalar2=None, op0=ALU.mult))

        pre_i = [0]

        def prepass_block(l, name):
            i = pre_i[0] % 2
            pre_i[0] += 1
            sf, sbf = stg_f[i], stg_b[i]
            pst_ld, pst_st = pst_lds[i], pst_sts[i]
            pieces = []

            def g(c):
                return gains.t[:, l, c:c + 1]
            if name in WIN_C0:
                c0 = WIN_C0[name]
                ncol = 448 if name == "c" else 512
                kk.dma("sp", pst_ld, sf.ap[:, 0:8 * ncol].rearrange("p (k c) -> p k c", c=ncol),
                       win_d[l, :, c0:c0 + ncol].rearrange("(k p) c -> p k c", p=128), w=sf)
                for kc in range(8):
                    pieces.append((kc * ncol, ncol, kc * ncol, g(kc)))
            elif name == "mla":
                kk.dma("sp", pst_ld, sf.ap[:, 0:1536].rearrange("p (k c) -> p k c", c=768),
                       wuq_d[l].rearrange("(k p) c -> p k c", p=128), w=sf)
                kk.dma("sp", pst_ld, sf.ap[:, 1536:2560], wukv_d[l], w=sf, skip_same=True)
                for kc in range(2):
                    pieces.append((kc * 768, 768, kc * 768, g(28 + kc)))
                pieces.append((1536, 1024, 1536, g(30)))
            elif name.startswith("M"):
                j = int(name[1:])
                for b_ in range(3):
                    cg = 5568 + b_ * 1024 + j * 128
                    kk.dma("sp", pst_ld, sf.ap[:, 0:3072].rearrange("p (k b c) -> p k b c", b=3, c=128)[:, :, b_, :],
                           win_d[l, :, cg:cg + 128].rearrange("(k p) c -> p k c", p=128), w=sf, skip_same=(b_ > 0))
                kk.dma("sp", pst_ld, sf.ap[:, 3072:3584].rearrange("p (k c) -> p k c", c=128),
                       wba_d[l, :, j * 128:(j + 1) * 128].rearrange("(k p) c -> p k c", p=128), w=sf, skip_same=True)
                kk.dma("sp", pst_ld, sf.ap[:, 3584:4608].rearrange("p (k c) -> p k c", c=128),
                       wbb_d[l, :, j * 128:(j + 1) * 128].rearrange("(k p) c -> p k c", p=128), w=sf, skip_same=True)
                kk.dma("sp", pst_ld, sf.ap[:, 4608:5120].rearrange("p (k c) -> p k c", c=128),
                       wbc_d[l, :, j * 128:(j + 1) * 128].rearrange("(k p) c -> p k c", p=128), w=sf, skip_same=True)
                for kc in range(8):
                    pieces.append((kc * 384, 384, kc * 384, g(kc)))
                for kc in range(4):
                    pieces.append((3072 + kc * 128, 128, 3072 + kc * 128, g(16 + kc)))
                for kc in range(8):
                    pieces.append((3584 + kc * 128, 128, 3584 + kc * 128, g(20 + kc)))
                pieces.append((4608, 512, 4608, None))
            elif name.startswith("wout"):
                n = int(name[4:])
                kk.dma("sp", pst_ld, sf.ap[:, 0:4096].rearrange("p (k c) -> p k c", c=512),
                       wout_d[l, :, n * 512:(n + 1) * 512].rearrange("(k p) c -> p k c", p=128), w=sf)
                pieces.append((0, 4096, 0, None))
            elif name.startswith("ffin"):
                jb = int(name[4:])
                v = sf.ap[:, 0:4096].rearrange("p (k c) -> p k c", c=512)
                kk.dma("sp", pst_ld, v[:, :, 0:256],
                       wfi_d[l, :, jb * 256:(jb + 1) * 256].rearrange("(k p) c -> p k c", p=128), w=sf)
                kk.dma("sp", pst_ld, v[:, :, 256:512],
                       wfi_d[l, :, DFF + jb * 256:DFF + (jb + 1) * 256].rearrange("(k p) c -> p k c", p=128), w=sf, skip_same=True)
                for kc in range(8):
                    pieces.append((kc * 512, 512, kc * 512, g(8 + kc)))
            elif name.startswith("ffout"):
                n, kc0, nk = FFOUT_PARTS[int(name[5:])]
                kk.dma("sp", pst_ld, sf.ap[:, 0:nk * 512].rearrange("p (k c) -> p k c", c=512),
                       wfo_d[l, kc0 * 128:(kc0 + nk) * 128, n * 512:(n + 1) * 512].rearrange("(k p) c -> p k c", p=128), w=sf)
                pieces.append((0, nk * 512, 0, None))
            return (l, name, sf, sbf, pst_st, pieces)

        def prepass_finish(l, name, sf, sbf, pst_st, pieces):
            for (do, n, so, gn) in pieces:
                e, fn = cast(sbf.ap[:, do:do + n], sf.ap[:, so:so + n], gn)
                kk.op(e, fn, r=[sf, gains], w=sbf)
            if name == "mla":
                p = nxt("T")
                for hh in range(4):
                    kk.op("pe", lambda h, hh=hh: h.transpose(out=p.t[:, hh * 128:(hh + 1) * 128], in_=sbf.ap[:, 1536 + hh * 256:1536 + hh * 256 + 128], identity=ident.t[:]),
                          r=[sbf, ident], w=p, inc=(hh == 3))
                kk.op("dve", lambda h: h.tensor_copy(out=sbf.ap[:, 2560:3072], in_=p.t[:, 0:512]), r=p, w=sbf)
            sz = blk_size(name)
            kk.dma("act", pst_st, wscr[l, BIDX[name], :, 0:sz], sbf.ap[:, 0:sz], r=sbf)

        pend = None
        for l in range(nlayers):
            for name in BLK:
                cur = prepass_block(l, name)
                if pend is not None:
                    prepass_finish(*pend)
                pend = cur
        prepass_finish(*pend)
        for e in ("sp", "pool", "act"):
            kk.wait_all(e, [stg_f, stg_b])

        wq = []
        for s_ in range(nseq):
            for l in range(nlayers):
                for T in range(4):
                    for name in BLK:
                        wq.append((l, name))
        wstate = {"issued": 0, "used": 0}

        def w_issue(upto):
            while wstate["issued"] < min(upto, len(wq)):
                i = wstate["issued"]
                l, name = wq[i]
                sl = i % NSLOT
                sz = blk_size(name)
                kk.dma("sp", wst[sl], wsl[sl].t[:, 0:sz], wscr[l, BIDX[name], :, 0:sz], w=wsl[sl])
                wstate["issued"] += 1

        def wget(name, keep=0):
            i = wstate["used"]
            assert wq[i][1] == name, (wq[i], name)
            assert keep < NSLOT
            w_issue(i - keep + NSLOT)
            wstate["used"] += 1
            return wsl[i % NSLOT]

        def rstd_cols(ss, n, inv_d):
            t1 = newcol(n)
            kk.op("pool", lambda h: h.tensor_scalar(out=t1.ap, in0=ss.ap, scalar1=inv_d, scalar2=EPS, op0=ALU.mult, op1=ALU.add), r=ss, w=t1)
            t2 = newcol(n)
            kk.op("pool", lambda h: h.tensor_tensor(out=t2.ap, in0=t1.ap, in1=mhalf.t[:, 0:n], op=ALU.pow), r=[t1, mhalf], w=t2)
            return t2

        def norm_to_hT(T):
            ar.reset(0)
            junk = ar.alloc([D], BF16)
            hns = [ar.alloc([D], BF16) for _ in range(4)]
            ss = newcol(4)
            for j in range(4):
                t = 4 * T + j
                kk.op("act", lambda h, t=t, j=j: h.activation(out=junk.ap, in_=xres.t[:, t, :], func=AF.Square, accum_out=ss.ap[:, j:j + 1]), r=xres.bufs[t], w=[junk, ss])
            rs = rstd_cols(ss, 4, 1.0 / D)
            for j in range(4):
                t = 4 * T + j
                kk.op("dve", lambda h, t=t, j=j: h.tensor_scalar(out=hns[j].ap, in0=xres.t[:, t, :], scalar1=rs.ap[:, j:j + 1], scalar2=None, op0=ALU.mult), r=[xres.bufs[t], rs], w=hns[j])
            for j in range(4):
                p = nxt("T")
                for kc in range(8):
                    kk.op("pe", lambda h, kc=kc, j=j, p=p: h.transpose(out=p.t[:, kc * 128:(kc + 1) * 128], in_=hns[j].ap[:, kc * 128:(kc + 1) * 128], identity=ident.t[:]),
                          r=[hns[j], ident], w=p, inc=(kc == 7))
                kk.op("act", lambda h, j=j, p=p: h.activation(out=hT.t[:, :, j * 128:(j + 1) * 128], in_=p.t[:].rearrange("p (k c) -> p k c", c=128), func=AF.Copy), r=p, w=hT)

        def proj_tok(wap, ncol, j, p, rhsT=None):
            for kc in range(8):
                kk.op("pe", lambda h, kc=kc: h.matmul(p.t[:, 0:ncol], lhsT=hT.t[:, kc, j * 128:(j + 1) * 128], rhs=wap[:, kc, :], start=(kc == 0), stop=(kc == 7)),
                      r=[hT, curw[0]], w=p, inc=(kc == 7))

        def proj_feat(lhs_of_kc, p, nk=8, rhs_of_kc=None, rbufs=None):
            for kc in range(nk):
                rhs = hT.t[:, kc, :] if rhs_of_kc is None else rhs_of_kc(kc)
                kk.op("pe", lambda h, kc=kc, rhs=rhs: h.matmul(p.t[:, 0:TT], lhsT=lhs_of_kc(kc), rhs=rhs, start=(kc == 0), stop=(kc == nk - 1)),
                      r=[hT if rbufs is None else rbufs, curw[0]], w=p, inc=(kc == nk - 1))

        curw = [None]

        def transposes_to(src_of, n, dst_ap_of_group, rbufs, wbufs, evac="act", parts=128, width=128):
            i = 0
            while i < n:
                m = min(8, n - i)
                p = nxt("T")
                for q in range(m):
                    kk.op("pe", lambda h, q=q, i=i: h.transpose(out=p.t[0:width, q * 128:(q + 1) * 128], in_=src_of(i + q), identity=ident.t[:]),
                          r=[rbufs, ident], w=p, inc=(q == m - 1))
                dst_ap_of_group(i, m, p)
                i += m

        try:
          chk("prepass")
          for s_ in range(nseq):
              for q in range(4):
                  kk.dma("pool", xst, xres.t[:, 4 * q:4 * q + 4, :], x_d[s_, q * 512:(q + 1) * 512, :].rearrange("(t p) d -> p t d", p=128),
                         w=xres.bufs[4 * q:4 * q + 4], skip_same=(q > 0))
              ar.reset(0)
              chk("xload")
              posi = ar.alloc([NT], I32 if False else F32)
              posi_i = Tmp(posi.ap.bitcast(I32), posi.bufs)
              for t in range(NT):
                  kk.dma("pool", posst, posi_i.ap[:, t:t + 1], pos_d[s_, t * 128:(t + 1) * 128].rearrange("(p o) -> p o", o=1), w=posi, skip_same=(t > 0))
              posf = ar.alloc([NT], F32)
              kk.op("dve", lambda h: h.tensor_copy(out=posf.ap, in_=posi_i.ap), r=posi, w=posf)
              rope_base = ar.off
              for (tab, inv, half) in ((ropeR, invr, 64), (ropeM, invm, 32)):
                  ar.reset(rope_base)
                  ang = ar.alloc([NT, half], F32)
                  for t in range(NT):
                      kk.op("dve", lambda h, t=t: h.tensor_scalar(out=ang.ap[:, t, :], in0=inv.t[:, 0:half], scalar1=posf.ap[:, t:t + 1], scalar2=None, op0=ALU.mult), r=[inv, posf], w=ang)
                  a2 = ar.alloc([2, NT, half], F32)
                  inv2pi = float(1.0 / (2 * np.pi))
                  kk.op("dve", lambda h: h.tensor_scalar(out=a2.ap[:, 1], in0=ang.ap, scalar1=inv2pi, scalar2=None, op0=ALU.mult), r=ang, w=a2)
                  kk.op("dve", lambda h: h.tensor_scalar(out=a2.ap[:, 0], in0=ang.ap, scalar1=inv2pi, scalar2=0.25, op0=ALU.mult, op1=ALU.add), r=ang, w=a2)
                  kf = ar.alloc([2, NT, half], F32)
                  ki = Tmp(kf.ap.bitcast(I32), kf.bufs)
                  kk.op("dve", lambda h: h.tensor_copy(out=ki.ap, in_=a2.ap), r=a2, w=kf)
                  kf2 = ar.alloc([2, NT, half], F32)
                  kk.op("dve", lambda h: h.tensor_copy(out=kf2.ap, in_=ki.ap), r=kf, w=kf2)
                  kk.op("dve", lambda h: h.tensor_tensor(out=a2.ap, in0=a2.ap, in1=kf2.ap, op=ALU.subtract), r=[a2, kf2], w=a2)
                  kk.op("dve", lambda h: h.tensor_scalar(out=kf2.ap, in0=a2.ap, scalar1=0.5, scalar2=None, op0=ALU.is_gt), r=a2, w=kf2)
                  kk.op("dve", lambda h: h.tensor_tensor(out=a2.ap, in0=a2.ap, in1=kf2.ap, op=ALU.subtract), r=[a2, kf2], w=a2)
                  s2 = ar.alloc([2, NT, half], F32)
                  kk.op("act", lambda h: h.activation(out=s2.ap, in_=a2.ap, func=AF.Sin, scale=float(2 * np.pi * (1 - 1e-6))), r=a2, w=s2)
                  for c in range(2):
                      kk.op("dve", lambda h, c=c: h.tensor_copy(out=tab.t[:, :, c, :], in_=s2.ap[:, c]), r=s2, w=tab)

              chk("rope")
              for l in range(nlayers):
                  kk.op("pool", lambda h: h.memset(shg_f.t[:], 0.0), w=shg_f)
                  kk.op("pool", lambda h: h.memset(shg_b.t[:], 0.0), w=shg_b)
                  kk.op("pool", lambda h: h.memset(sr_f.t[:], 0.0), w=sr_f)
                  kk.op("pool", lambda h: h.memset(sr_b.t[:], 0.0), w=sr_b)
                  for T in range(4):
                      norm_to_hT(T)
                      chk("norm")
                      ar.reset(0)
                      v_hg = ar.alloc([4, 512], BF16)
                      gate_a = ar.alloc([4, 512], BF16)
                      y_a = ar.alloc([4, 512], BF16)
                      curw[0] = wget("hi")
                      wv = curw[0].t[:, 0:4096].rearrange("p (k c) -> p k c", c=512)
                      for j in range(4):
                          p = nxt("mm")
                          proj_tok(wv, 512, j, p)
                          kk.op("act", lambda h, j=j, p=p: h.activation(out=v_hg.ap[:, j, :], in_=p.t[:], func=AF.Copy), r=p, w=v_hg)
                      curw[0] = wget("hgate")
                      wv = curw[0].t[:, 0:4096].rearrange("p (k c) -> p k c", c=512)
                      for j in range(4):
                          p = nxt("mm")
                          proj_tok(wv, 512, j, p)
                          sg = ar.alloc([512], F32) if j == 0 else sg
                          kk.op("act", lambda h, p=p: h.activation(out=sg.ap, in_=p.t[:], func=AF.Sigmoid), r=p, w=sg)
                          kk.op("dve", lambda h, j=j, p=p: h.tensor_tensor(out=gate_a.ap[:, j, :], in0=p.t[:], in1=sg.ap, op=ALU.mult), r=[p, sg], w=gate_a)
                      wq_ = wget("hq")
                      wf_ = wget("hf", keep=1)
                      wqv = wq_.t[:, 0:4096].rearrange("p (k c) -> p k c", c=512)
                      wfv = wf_.t[:, 0:4096].rearrange("p (k c) -> p k c", c=512)
                      base_off = ar.off
                      t0 = ar.alloc([512], F32)
                      t1 = ar.alloc([512], F32)
                      t2 = ar.alloc([512], F32)
                      t3 = ar.alloc([512], F32)
                      q_f = ar.alloc([512], F32)
                      k_f = ar.alloc([512], F32)
                      ecs = [ar.alloc([512], F32) for _ in range(2)]
                      qes = [ar.alloc([512], BF16) for _ in range(2)]
                      kes = [ar.alloc([512], BF16) for _ in range(2)]
                      qcs = [ar.alloc([512], BF16) for _ in range(2)]
                      kdTs = [ar.alloc([512], BF16) for _ in range(2)]
                      kds = [ar.alloc([4, 128], BF16) for _ in range(2)]
                      ats = [ar.alloc([4, 64], BF16) for _ in range(2)]

                      def c3(ap):
                          return ap.rearrange("p (c t) -> p c t", t=64)
                      for hp in range(2):
                          pos_ = {}
                          for hh in range(2):
                              hd = 2 * hp + hh
                              ec, qe, ke, qc, kdT, kd, at = ecs[hh], qes[hh], kes[hh], qcs[hh], kdTs[hh], kds[hh], ats[hh]
                              curw[0] = wq_
                              p = nxt("mm")
                              proj_feat(lambda kc, hd=hd: wqv[:, kc, hd * 128:(hd + 1) * 128], p)
                              kk.op("act", lambda h, p=p: h.activation(out=t0.ap, in_=p.t[:], func=AF.Sigmoid), r=p, w=t0)
                              kk.op("dve", lambda h, p=p: h.tensor_tensor(out=q_f.ap, in0=p.t[:], in1=t0.ap, op=ALU.mult), r=[p, t0], w=q_f)
                              curw[0] = wf_
                              p = nxt("mm")
                              proj_feat(lambda kc, hd=hd: wfv[:, kc, hd * 128:(hd + 1) * 128], p)
                              kk.op("act", lambda h, p=p: h.activation(out=t0.ap, in_=p.t[:], func=AF.Sigmoid), r=p, w=t0)
                              kk.op("dve", lambda h, hd=hd: h.tensor_scalar(out=t1.ap, in0=t0.ap, scalar1=lbt.t[:, l, 1, hd:hd + 1], scalar2=lbt.t[:, l, 0, hd:hd + 1], op0=ALU.mult, op1=ALU.add), r=[t0, lbt], w=t1)
                              kk.op("act", lambda h: h.activation(out=t0.ap, in_=t1.ap, func=AF.Ln), r=t1, w=t0)
                              kk.op("dve", lambda h: h.tensor_scalar(out=k_f.ap, in0=t1.ap, scalar1=-1.0, scalar2=1.0, op0=ALU.mult, op1=ALU.add), r=t1, w=k_f)
                              kk.op("dve", lambda h: h.tensor_tensor_scan(out=t1.ap, data0=resetc.t[:], data1=t0.ap, initial=0.0, op0=ALU.mult, op1=ALU.add), r=[resetc, t0], w=t1)
                              kk.op("dve", lambda h: h.tensor_tensor(out=c3(t0.ap), in0=c3(t1.ap), in1=c3(t1.ap)[:, :, 31:32].broadcast_to([128, 8, 64]), op=ALU.subtract), r=t1, w=t0)
                              kk.op("dve", lambda h: h.tensor_tensor(out=c3(t2.ap), in0=c3(t1.ap), in1=c3(t1.ap)[:, :, 63:64].broadcast_to([128, 8, 64]), op=ALU.subtract), r=t1, w=t2)
                              kk.op("act", lambda h: h.activation(out=t3.ap, in_=t0.ap, func=AF.Exp), r=t0, w=t3)
                              kk.op("act", lambda h: h.activation(out=t0.ap, in_=t0.ap, func=AF.Exp, scale=-1.0), r=t0, w=t0)
                              kk.op("act", lambda h, ec=ec: h.activation(out=ec.ap, in_=t1.ap, func=AF.Exp), r=t1, w=ec)
                              kk.op("act", lambda h: h.activation(out=t2.ap, in_=t2.ap, func=AF.Exp, scale=-1.0), r=t2, w=t2)
                              kk.op("dve", lambda h, qe=qe: h.tensor_tensor(out=qe.ap, in0=q_f.ap, in1=t3.ap, op=ALU.mult), r=[q_f, t3], w=qe)
                              kk.op("dve", lambda h, ke=ke: h.tensor_tensor(out=ke.ap, in0=k_f.ap, in1=t0.ap, op=ALU.mult), r=[k_f, t0], w=ke)
                              kk.op("pool", lambda h, qc=qc, ec=ec: h.tensor_tensor(out=qc.ap, in0=q_f.ap, in1=ec.ap, op=ALU.mult), r=[q_f, ec], w=qc)
                              kk.op("dve", lambda h, kdT=kdT: h.tensor_tensor(out=kdT.ap, in0=k_f.ap, in1=t2.ap, op=ALU.mult), r=[k_f, t2], w=kdT)
                              pT_ = nxt("T")
                              for j in range(4):
                                  kk.op("pe", lambda h, j=j, kdT=kdT, pT_=pT_: h.transpose(out=pT_.t[:, j * 128:(j + 1) * 128], in_=kdT.ap[:, j * 128:(j + 1) * 128], identity=ident.t[:]),
                                        r=[kdT, ident], w=pT_, inc=(j == 3))
                              kk.op("act", lambda h, kd=kd, pT_=pT_: h.activation(out=kd.ap, in_=pT_.t[:, 0:512].rearrange("p (j c) -> p j c", c=128), func=AF.Copy), r=pT_, w=kd)
                              pa = nxt("mm")
                              for ci in range(8):
                                  j, hf_ = ci // 2, ci % 2
                                  kk.op("pe", lambda h, ci=ci, j=j, hf_=hf_, ke=ke, qe=qe, pa=pa: h.matmul(pa.t[hf_ * 64:(hf_ + 1) * 64, j * 64:(j + 1) * 64], lhsT=ke.ap[:, ci * 64:(ci + 1) * 64], rhs=qe.ap[:, ci * 64:(ci + 1) * 64], start=True, stop=True),
                                        r=[ke, qe], w=pa, inc=(ci == 7))
                              kk.op("dve", lambda h, at=at, pa=pa: h.tensor_tensor(out=at.ap, in0=pa.t[:, 0:256].rearrange("p (j t) -> p j t", t=64), in1=hgmask.t[:].unsqueeze(1).broadcast_to([128, 4, 64]), op=ALU.mult), r=[pa, hgmask], w=at)
                              pos_[hh] = nxt("acc")
                          for ci in range(8):
                              j, hf_ = ci // 2, ci % 2
                              P0, P1 = hf_ * 64, (hf_ + 1) * 64
                              for hh in range(2):
                                  hd = 2 * hp + hh
                                  ec, qc, kd, at, po = ecs[hh], qcs[hh], kds[hh], ats[hh], pos_[hh]
                                  kk.op("pe", lambda h, po=po, at=at, j=j, hd=hd, P0=P0, P1=P1: h.matmul(po.t[P0:P1, j * 128:(j + 1) * 128], lhsT=at.ap[P0:P1, j, :], rhs=v_hg.ap[P0:P1, j, hd * 128:(hd + 1) * 128], start=True, stop=False),
                                        r=[at, v_hg], w=po, inc=False)
                                  kk.op("pe", lambda h, po=po, qc=qc, ci=ci, j=j, hd=hd, P0=P0, P1=P1: h.matmul(po.t[P0:P1, j * 128:(j + 1) * 128], lhsT=qc.ap[:, ci * 64:(ci + 1) * 64], rhs=shg_b.t[:, hd, :], start=False, stop=True),
                                        r=[qc, shg_b.bufs[hd]], w=po, inc=True)
                                  pSx = nxt("mm")
                                  kk.op("pe", lambda h, kd=kd, j=j, hd=hd, P0=P0, P1=P1, pSx=pSx: h.matmul(pSx.t[:, 0:128], lhsT=kd.ap[P0:P1, j, :], rhs=v_hg.ap[P0:P1, j, hd * 128:(hd + 1) * 128], start=True, stop=True),
                                        r=[kd, v_hg], w=pSx)
                                  kk.op("dve", lambda h, ec=ec, ci=ci, hd=hd, pSx=pSx: h.scalar_tensor_tensor(out=shg_f.t[:, hd, :], in0=shg_f.t[:, hd, :], scalar=ec.ap[:, ci * 64 + 63:ci * 64 + 64], in1=pSx.t[:, 0:128], op0=ALU.mult, op1=ALU.add),
                                        r=[ec, pSx, shg_f.bufs[hd]], w=shg_f.bufs[hd])
                                  kk.op("act", lambda h, hd=hd: h.activation(out=shg_b.t[:, hd, :], in_=shg_f.t[:, hd, :], func=AF.Copy), r=shg_f.bufs[hd], w=shg_b.bufs[hd])
                          for hh in range(2):
                              hd = 2 * hp + hh
                              po = pos_[hh]
                              ss = newcol(4)
                              for j in range(4):
                                  kk.op("act", lambda h, j=j, po=po, ss=ss: h.activation(out=t3.ap[:, 0:128], in_=po.t[:, j * 128:(j + 1) * 128], func=AF.Square, accum_out=ss.ap[:, j:j + 1]), r=po, w=[t3, ss])
                              rs = rstd_cols(ss, 4, 1.0 / 128)
                              for j in range(4):
                                  kk.op("dve", lambda h, j=j, po=po, rs=rs, hd=hd: h.scalar_tensor_tensor(out=y_a.ap[:, j, hd * 128:(hd + 1) * 128], in0=po.t[:, j * 128:(j + 1) * 128], scalar=rs.ap[:, j:j + 1], in1=gate_a.ap[:, j, hd * 128:(hd + 1) * 128], op0=ALU.mult, op1=ALU.mult),
                                        r=[po, rs, gate_a], w=y_a)
                      for j in range(4):
                          p = nxt("T")
                          for kc in range(4):
                              kk.op("pe", lambda h, j=j, kc=kc, p=p: h.transpose(out=p.t[:, kc * 128:(kc + 1) * 128], in_=y_a.ap[:, j, kc * 128:(kc + 1) * 128], identity=ident.t[:]),
                                    r=[y_a, ident], w=p, inc=(kc == 3))
                          kk.op("act", lambda h, j=j, p=p: h.activation(out=big.t[:, 0:4, j * 128:(j + 1) * 128], in_=p.t[:, 0:512].rearrange("p (k c) -> p k c", c=128), func=AF.Copy), r=p, w=yA)

                      chk("hg")
                      wrq, wrk = wget("rq"), wget("rk", keep=1)
                      wrv0, wrv1, wrg0, wrg1 = None, None, None, None
                      ar.reset(0)
                      qT = ar.alloc([4, 4, 128], BF16)
                      kT = ar.alloc([4, 4, 128], BF16)
                      qdT = ar.alloc([4, 4, 128], BF16)
                      kdr = ar.alloc([4, 4, 128], BF16)
                      v_r = ar.alloc([4, 1024], BF16)
                      gate_b = ar.alloc([4, 1024], BF16)
                      tA = ar.alloc([512], F32)
                      tB = ar.alloc([512], F32)
                      qk_r = [ar.alloc([512], BF16) for _ in range(2)]

                      def v4(ap):
                          return ap.rearrange("p (h two d) -> p h two d", two=2, d=64)
                      for j in range(4):
                          t = 4 * T + j
                          cosb = ropeR.t[:, t, 0, :].unsqueeze(1).unsqueeze(1).broadcast_to([128, 4, 2, 64])
                          sinb = ropeR.t[:, t, 1, :].unsqueeze(1).broadcast_to([128, 4, 64])
                          for qi, wblk in enumerate((wrq, wrk)):
                              curw[0] = wblk
                              p = nxt("mm")
                              proj_tok(wblk.t[:, 0:4096].rearrange("p (k c) -> p k c", c=512), 512, j, p)
                              dst = qk_r[qi]
                              kk.op("dve", lambda h, p=p: h.tensor_tensor(out=v4(tA.ap), in0=v4(p.t[:]), in1=cosb, op=ALU.mult), r=[p, ropeR], w=tA)
                              kk.op("dve", lambda h, p=p: h.tensor_tensor(out=v4(tB.ap)[:, :, 0, :], in0=v4(p.t[:])[:, :, 1, :], in1=sinb, op=ALU.mult), r=[p, ropeR], w=tB)
                              kk.op("dve", lambda h, p=p: h.tensor_tensor(out=v4(tB.ap)[:, :, 1, :], in0=v4(p.t[:])[:, :, 0, :], in1=sinb, op=ALU.mult), r=[p, ropeR], w=tB)
                              kk.op("dve", lambda h, dst=dst: h.tensor_tensor(out=v4(dst.ap)[:, :, 0, :], in0=v4(tA.ap)[:, :, 0, :], in1=v4(tB.ap)[:, :, 0, :], op=ALU.subtract), r=[tA, tB], w=dst)
                              kk.op("dve", lambda h, dst=dst: h.tensor_tensor(out=v4(dst.ap)[:, :, 1, :], in0=v4(tA.ap)[:, :, 1, :], in1=v4(tB.ap)[:, :, 1, :], op=ALU.add), r=[tA, tB], w=dst)
                          chk("ret1")
                          q_r, k_r = qk_r
                          kk.op("dve", lambda h, j=j: h.tensor_tensor(out=kdr.ap[:, j], in0=k_r.ap.rearrange("p (h d) -> p h d", d=128), in1=kdecc.t[:].unsqueeze(2).broadcast_to([128, 4, 128]), op=ALU.mult), r=[k_r, kdecc], w=kdr)
                          p = nxt("T")
                          for hd in range(4):
                              kk.op("pe", lambda h, hd=hd, p=p: h.transpose(out=p.t[:, hd * 128:(hd + 1) * 128], in_=q_r.ap[:, hd * 128:(hd + 1) * 128], identity=ident.t[:]), r=[q_r, ident], w=p, inc=False)
                          for hd in range(4):
                              kk.op("pe", lambda h, hd=hd, p=p: h.transpose(out=p.t[:, 512 + hd * 128:512 + (hd + 1) * 128], in_=k_r.ap[:, hd * 128:(hd + 1) * 128], identity=ident.t[:]), r=[k_r, ident], w=p, inc=(hd == 3))
                          chk("ret1b")
                          kk.op("act", lambda h, j=j, p=p: h.activation(out=qT.ap[:, j], in_=p.t[:, 0:512].rearrange("p (h t) -> p h t", t=128), func=AF.Copy), r=p, w=qT)
                          kk.op("act", lambda h, j=j, p=p: h.activation(out=kT.ap[:, j], in_=p.t[:, 512:1024].rearrange("p (h t) -> p h t", t=128), func=AF.Copy), r=p, w=kT)
                          chk("ret1c")
                          kk.op("dve", lambda h, j=j, p=p: h.tensor_tensor(out=qdT.ap[:, j], in0=p.t[:, 0:512].rearrange("p (h t) -> p h t", t=128), in1=qdec.t[:], op=ALU.mult), r=[p, qdec], w=qdT)
                          chk("ret1d")
                          if j == 1:
                              chk("ret1e")
                      chk("ret2")
                      wrv0, wrv1 = wget("rv0"), wget("rv1", keep=1)
                      for j in range(4):
                          for n_, wblk in enumerate((wrv0, wrv1)):
                              curw[0] = wblk
                              p = nxt("mm")
                              proj_tok(wblk.t[:, 0:4096].rearrange("p (k c) -> p k c", c=512), 512, j, p)
                              kk.op("act", lambda h, j=j, n_=n_, p=p: h.activation(out=v_r.ap[:, j, n_ * 512:(n_ + 1) * 512], in_=p.t[:], func=AF.Copy), r=p, w=v_r)
                      wrg0, wrg1 = wget("rg0"), wget("rg1", keep=1)
                      for j in range(4):
                          for n_, wblk in enumerate((wrg0, wrg1)):
                              curw[0] = wblk
                              p = nxt("mm")
                              proj_tok(wblk.t[:, 0:4096].rearrange("p (k c) -> p k c", c=512), 512, j, p)
                              kk.op("act", lambda h, p=p: h.activation(out=tA.ap, in_=p.t[:], func=AF.Sigmoid), r=p, w=tA)
                              kk.op("dve", lambda h, j=j, n_=n_, p=p: h.tensor_tensor(out=gate_b.ap[:, j, n_ * 512:(n_ + 1) * 512], in0=p.t[:], in1=tA.ap, op=ALU.mult), r=[p, tA], w=gate_b)
                      chk("ret3")
                      at_r = ar.alloc([4, 128], BF16)
                      y_bs = [ar.alloc([1024], BF16) for _ in range(2)]
                      junk = ar.alloc([256], F32)
                      pend_yT = [None]

                      def emit_yT(j, y_b):
                          p = nxt("T")
                          for kc in range(8):
                              kk.op("pe", lambda h, kc=kc, p=p: h.transpose(out=p.t[:, kc * 128:(kc + 1) * 128], in_=y_b.ap[:, kc * 128:(kc + 1) * 128], identity=ident.t[:]), r=[y_b, ident], w=p, inc=(kc == 7))
                          kk.op("act", lambda h, j=j, p=p: h.activation(out=big.t[:, 4:12, j * 128:(j + 1) * 128], in_=p.t[:].rearrange("p (k c) -> p k c", c=128), func=AF.Copy), r=p, w=yB)
                      g128 = [float(np.exp(128.0 * LOG_GAMMA[h_])) for h_ in range(4)]
                      for j in range(4):
                          pa = nxt("mm")
                          for hd in range(4):
                              kk.op("pe", lambda h, j=j, hd=hd, pa=pa: h.matmul(pa.t[:, hd * 128:(hd + 1) * 128], lhsT=kT.ap[:, j, hd, :], rhs=qT.ap[:, j, hd, :], start=True, stop=True),
                                    r=[kT, qT], w=pa, inc=(hd == 3))
                          kk.op("dve", lambda h, pa=pa: h.tensor_tensor(out=at_r.ap, in0=pa.t[:].rearrange("p (h t) -> p h t", t=128), in1=dmask.t[:], op=ALU.mult), r=[pa, dmask], w=at_r)
                          pos2 = [nxt("acc"), nxt("acc")]
                          ss = newcol(4)
                          for hd in range(4):
                              po = pos2[hd // 2]
                              oc = (hd % 2) * 256
                              kk.op("pe", lambda h, j=j, hd=hd, po=po, oc=oc: h.matmul(po.t[:, oc:oc + 256], lhsT=at_r.ap[:, hd, :], rhs=v_r.ap[:, j, hd * 256:(hd + 1) * 256], start=True, stop=False),
                                    r=[at_r, v_r], w=po, inc=False)
                              kk.op("pe", lambda h, j=j, hd=hd, po=po, oc=oc: h.matmul(po.t[:, oc:oc + 256], lhsT=qdT.ap[:, j, hd, :], rhs=sr_b.t[:, hd, :], start=False, stop=True),
                                    r=[qdT, sr_b.bufs[hd]], w=po, inc=True)
                              pSx = nxt("mm") if hd % 2 == 0 else pS
                              kk.op("pe", lambda h, j=j, hd=hd, pSx=pSx: h.matmul(pSx.t[:, 0:256], lhsT=kdr.ap[:, j, hd, :], rhs=v_r.ap[:, j, hd * 256:(hd + 1) * 256], start=True, stop=True),
                                    r=[kdr, v_r], w=pSx)
                              kk.op("dve", lambda h, hd=hd, pSx=pSx: h.scalar_tensor_tensor(out=sr_f.t[:, hd, :], in0=sr_f.t[:, hd, :], scalar=g128[hd], in1=pSx.t[:, 0:256], op0=ALU.mult, op1=ALU.add),
                                    r=[pSx, sr_f.bufs[hd]], w=sr_f.bufs[hd])
                              kk.op("act", lambda h, hd=hd: h.activation(out=sr_b.t[:, hd, :], in_=sr_f.t[:, hd, :], func=AF.Copy), r=sr_f.bufs[hd], w=sr_b.bufs[hd])
                              kk.op("act", lambda h, hd=hd, po=po, oc=oc, ss=ss: h.activation(out=junk.ap, in_=po.t[:, oc:oc + 256], func=AF.Square, accum_out=ss.ap[:, hd:hd + 1]), r=po, w=[junk, ss])
                          chk("ret4")
                          if pend_yT[0] is not None:
                              emit_yT(*pend_yT[0])
                          y_b = y_bs[j % 2]
                          rs = rstd_cols(ss, 4, 1.0 / 256)
                          for hd in range(4):
                              po = pos2[hd // 2]
                              oc = (hd % 2) * 256
                              kk.op("dve", lambda h, j=j, hd=hd, po=po, oc=oc, rs=rs, y_b=y_b: h.scalar_tensor_tensor(out=y_b.ap[:, hd * 256:(hd + 1) * 256], in0=po.t[:, oc:oc + 256], scalar=rs.ap[:, hd:hd + 1], in1=gate_b.ap[:, j, hd * 256:(hd + 1) * 256], op0=ALU.mult, op1=ALU.mult),
                                    r=[po, rs, gate_b], w=y_b)
                          pend_yT[0] = (j, y_b)
                      emit_yT(*pend_yT[0])

                      chk("ret")
                      wc = wget("c")
                      wm = wget("mla", keep=1)
                      ar.reset(0)
                      qnT = ar.alloc([2, 512], BF16)
                      qabsT = ar.alloc([4, 512], BF16)
                      qpeT = ar.alloc([4, 512], BF16, parts=64)
                      qn = ar.alloc([256], BF16)
                      kpe = ar.alloc([64], BF16)
                      junk = ar.alloc([256], F32)
                      tA = ar.alloc([256], F32)
                      tB = ar.alloc([256], F32)
                      qpe = ar.alloc([256], BF16)
                      qnopeT = ar.alloc([512], BF16)
                      wcv = wc.t[:, 0:8 * 448].rearrange("p (k c) -> p k c", c=448)
                      wuq = wm.t[:, 0:1536].rearrange("p (k c) -> p k c", c=768)
                      wukv = wm.t[:, 1536:2560]
                      wukT = wm.t[:, 2560:3072].rearrange("p (h c) -> p h c", c=128)

                      def rope_small(dst, src_ap, nh, t, rb, wb):
                          def v(ap):
                              return ap.rearrange("p (h two d) -> p h two d", two=2, d=32)
                          cosb = ropeM.t[:, t, 0, :].unsqueeze(1).unsqueeze(1).broadcast_to([128, nh, 2, 32])
                          sinb = ropeM.t[:, t, 1, :].unsqueeze(1).broadcast_to([128, nh, 32])
                          n = nh * 64
                          kk.op("dve", lambda h: h.tensor_tensor(out=v(tA.ap[:, 0:n]), in0=v(src_ap), in1=cosb, op=ALU.mult), r=[rb, ropeM], w=tA)
                          kk.op("dve", lambda h: h.tensor_tensor(out=v(tB.ap[:, 0:n])[:, :, 0, :], in0=v(src_ap)[:, :, 1, :], in1=sinb, op=ALU.mult), r=[rb, ropeM], w=tB)
                          kk.op("dve", lambda h: h.tensor_tensor(out=v(tB.ap[:, 0:n])[:, :, 1, :], in0=v(src_ap)[:, :, 0, :], in1=sinb, op=ALU.mult), r=[rb, ropeM], w=tB)
                          kk.op("dve", lambda h: h.tensor_tensor(out=v(dst)[:, :, 0, :], in0=v(tA.ap[:, 0:n])[:, :, 0, :], in1=v(tB.ap[:, 0:n])[:, :, 0, :], op=ALU.subtract), r=[tA, tB], w=wb)
                          kk.op("dve", lambda h: h.tensor_tensor(out=v(dst)[:, :, 1, :], in0=v(tA.ap[:, 0:n])[:, :, 1, :], in1=v(tB.ap[:, 0:n])[:, :, 1, :], op=ALU.add), r=[tA, tB], w=wb)

                      qns = [ar.alloc([256], BF16) for _ in range(4)]
                      kpes = [ar.alloc([64], BF16) for _ in range(4)]
                      curw[0] = wc
                      ps_c = [nxt("wide") for _ in range(4)]
                      for j in range(4):
                          proj_tok(wcv, 448, j, ps_c[j])
                      ssq, sskv = newcol(4), newcol(4)
                      for j in range(4):
                          p = ps_c[j]
                          kk.op("act", lambda h, p=p, j=j: h.activation(out=junk.ap, in_=p.t[:, 0:256], func=AF.Square, accum_out=ssq.ap[:, j:j + 1]), r=p, w=[junk, ssq])
                          kk.op("act", lambda h, p=p, j=j: h.activation(out=junk.ap[:, 0:128], in_=p.t[:, 256:384], func=AF.Square, accum_out=sskv.ap[:, j:j + 1]), r=p, w=[junk, sskv])
                      rsq = rstd_cols(ssq, 4, 1.0 / 256)
                      rskv = rstd_cols(sskv, 4, 1.0 / 128)
                      for j in range(4):
                          t = 4 * T + j
                          p = ps_c[j]
                          kk.op("dve", lambda h, p=p, j=j: h.tensor_scalar(out=qns[j].ap, in0=p.t[:, 0:256], scalar1=rsq.ap[:, j:j + 1], scalar2=None, op0=ALU.mult), r=[p, rsq], w=qns[j])
                          kk.op("dve", lambda h, p=p, j=j, t=t: h.tensor_scalar(out=kvtok.t[:, t, :], in0=p.t[:, 256:384], scalar1=rskv.ap[:, j:j + 1], scalar2=None, op0=ALU.mult), r=[p, rskv], w=kvtok.bufs[t])
                          rope_small(kpes[j].ap, p.t[:, 384:448], 1, t, p, kpes[j])
                      for j in range(4):
                          t = 4 * T + j
                          qn_, kpe_ = qns[j], kpes[j]
                          pt_ = nxt("T")
                          kk.op("pe", lambda h, pt_=pt_, qn_=qn_: h.transpose(out=pt_.t[:, 0:128], in_=qn_.ap[:, 0:128], identity=ident.t[:]), r=[qn_, ident], w=pt_, inc=False)
                          kk.op("pe", lambda h, pt_=pt_, qn_=qn_: h.transpose(out=pt_.t[:, 128:256], in_=qn_.ap[:, 128:256], identity=ident.t[:]), r=[qn_, ident], w=pt_, inc=False)
                          kk.op("pe", lambda h, pt_=pt_, t=t: h.transpose(out=pt_.t[:, 256:384], in_=kvtok.t[:, t, :], identity=ident.t[:]), r=[kvtok.bufs[t], ident], w=pt_, inc=False)
                          kk.op("pe", lambda h, pt_=pt_, kpe_=kpe_: h.transpose(out=pt_.t[0:64, 384:512], in_=kpe_.ap, identity=ident.t[:]), r=[kpe_, ident], w=pt_, inc=True)
                          kk.op("act", lambda h, j=j, pt_=pt_: h.activation(out=qnT.ap[:, :, j * 128:(j + 1) * 128], in_=pt_.t[:, 0:256].rearrange("p (k c) -> p k c", c=128), func=AF.Copy), r=pt_, w=qnT)
                          kk.op("act", lambda h, t=t, pt_=pt_: h.activation(out=kvnT.t[:, t * 128:(t + 1) * 128], in_=pt_.t[:, 256:384], func=AF.Copy), r=pt_, w=kvnT.bufs[t])
                          kk.op("act", lambda h, t=t, pt_=pt_: h.activation(out=kpeT.t[:, t * 128:(t + 1) * 128], in_=pt_.t[0:64, 384:512], func=AF.Copy), r=pt_, w=kpeT.bufs[t])
                      curw[0] = wm
                      for hd in range(4):
                          p = nxt("mm")
                          proj_feat(lambda kc, hd=hd: wuq[:, kc, hd * 192:hd * 192 + 128], p, nk=2, rhs_of_kc=lambda kc: qnT.ap[:, kc, :], rbufs=qnT)
                          kk.op("act", lambda h, p=p: h.activation(out=qnopeT.ap, in_=p.t[:], func=AF.Copy, scale=S192), r=p, w=qnopeT)
                          p2 = nxt("mm")
                          kk.op("pe", lambda h, hd=hd, p2=p2: h.matmul(p2.t[:], lhsT=wukT[:, hd, :], rhs=qnopeT.ap, start=True, stop=True), r=[wm, qnopeT], w=p2)
                          kk.op("act", lambda h, hd=hd, p2=p2: h.activation(out=qabsT.ap[:, hd, :], in_=p2.t[:], func=AF.Copy), r=p2, w=qabsT)
                      wuq_pe = wuq.rearrange("p k (h d) -> p k h d", d=192)[:, :, :, 128:192]
                      qpes = [ar.alloc([256], BF16) for _ in range(4)]
                      ps_q = [nxt("wide") for _ in range(4)]
                      for j in range(4):
                          p = ps_q[j]
                          for kc in range(2):
                              kk.op("pe", lambda h, kc=kc, j=j, p=p: h.matmul(p.t[:, 0:256].rearrange("p (h d) -> p h d", d=64), lhsT=qnT.ap[:, kc, j * 128:(j + 1) * 128], rhs=wuq_pe[:, kc], start=(kc == 0), stop=(kc == 1)),
                                    r=[qnT, wm], w=p, inc=(kc == 1))
                      for j in range(4):
                          rope_small(qpes[j].ap, ps_q[j].t[:, 0:256], 4, 4 * T + j, ps_q[j], qpes[j])
                      for j in range(4):
                          qpe_ = qpes[j]
                          pt_ = nxt("T")
                          for hd in range(4):
                              kk.op("pe", lambda h, hd=hd, pt_=pt_, qpe_=qpe_: h.transpose(out=pt_.t[0:64, hd * 128:(hd + 1) * 128], in_=qpe_.ap[:, hd * 64:(hd + 1) * 64], identity=ident.t[:]), r=[qpe_, ident], w=pt_, inc=(hd == 3))
                          kk.op("act", lambda h, j=j, pt_=pt_: h.activation(out=qpeT.ap[:, :, j * 128:(j + 1) * 128], in_=pt_.t[0:64, 0:512].rearrange("p (h c) -> p h c", c=128), func=AF.Copy, scale=S192), r=pt_, w=qpeT)
                      pTs = [ar.alloc([512], BF16, align=True) for _ in range(3)]
                      ar.alloc([0], BF16, align=True)
                      olT = ar.alloc([512], BF16)
                      rinv = ar.alloc([512], F32)
                      prr = 0
                      for hd in range(4):
                          po_l, po_s = nxt("acc"), nxt("acc")
                          njb = 4 * T + 4
                          def emit_st(jb, hd=hd):
                              c0 = max(0, jb - 4 * T) * 128
                              p = nxt("mm")
                              kk.op("pe", lambda h, jb=jb, hd=hd, c0=c0, p=p: h.matmul(p.t[:, c0:512], lhsT=kvnT.t[:, jb * 128:(jb + 1) * 128], rhs=qabsT.ap[:, hd, c0:512], start=True, stop=False),
                                    r=[kvnT.bufs[jb], qabsT], w=p, inc=False)
                              kk.op("pe", lambda h, jb=jb, hd=hd, c0=c0, p=p: h.matmul(p.t[:, c0:512], lhsT=kpeT.t[:, jb * 128:(jb + 1) * 128], rhs=qpeT.ap[:, hd, c0:512], start=False, stop=True),
                                    r=[kpeT.bufs[jb], qpeT], w=p, inc=True)
                              return p
                          stq = [emit_st(0)]
                          for jb in range(njb):
                              c0 = max(0, jb - 4 * T) * 128
                              if jb + 1 < njb:
                                  stq.append(emit_st(jb + 1))
                              p = stq.pop(0)
                              pT_ = pTs[prr % 3]
                              prr += 1
                              kk.op("act", lambda h, c0=c0, p=p, pT_=pT_: h.activation(out=pT_.ap[:, c0:512], in_=p.t[:, c0:512], func=AF.Exp), r=p, w=pT_)
                              if jb >= 4 * T:
                                  kk.op("pool", lambda h, c0=c0, pT_=pT_: h.tensor_tensor(out=pT_.ap[:, c0:c0 + 128], in0=pT_.ap[:, c0:c0 + 128], in1=causal.t[:], op=ALU.mult), r=[pT_, causal], w=pT_)
                              kk.op("pe", lambda h, jb=jb, c0=c0, pT_=pT_, po_l=po_l: h.matmul(po_l.t[:, c0:512], lhsT=kvtok.t[:, jb, :], rhs=pT_.ap[:, c0:512], start=(jb == 0), stop=(jb == njb - 1)),
                                    r=[kvtok.bufs[jb], pT_], w=po_l, inc=(jb == njb - 1))
                              kk.op("pe", lambda h, jb=jb, c0=c0, pT_=pT_, po_s=po_s: h.matmul(po_s.t[:, c0:512], lhsT=ones.t[:], rhs=pT_.ap[:, c0:512], start=(jb == 0), stop=(jb == njb - 1)),
                                    r=[ones, pT_], w=po_s, inc=True)
                          kk.op("act", lambda h, po_l=po_l: h.activation(out=olT.ap, in_=po_l.t[:], func=AF.Copy), r=po_l, w=olT)
                          kk.op("act", lambda h, po_s=po_s: h.activation(out=rinv.ap, in_=po_s.t[:], func=AF.Ln), r=po_s, w=rinv)
                          kk.op("act", lambda h: h.activation(out=rinv.ap, in_=rinv.ap, func=AF.Exp, scale=-1.0), r=rinv, w=rinv)
                          p = nxt("mm")
                          kk.op("pe", lambda h, hd=hd, p=p: h.matmul(p.t[:], lhsT=wukv[:, hd * 256 + 128:hd * 256 + 256], rhs=olT.ap, start=True, stop=True), r=[wm, olT], w=p)
                          kk.op("dve", lambda h, hd=hd, p=p: h.tensor_tensor(out=big.t[:, 12 + hd, :], in0=p.t[:], in1=rinv.ap, op=ALU.mult), r=[p, rinv], w=yC)

                      chk("mla")
                      ar.reset(0)
                      gs = [ar.alloc([512], F32) for _ in range(3)]
                      ts = [ar.alloc([512], F32) for _ in range(3)]
                      m1 = ar.alloc([512], F32)
                      for jc in range(8):
                          wM = wget(f"M{jc}")
                          curw[0] = wM
                          wg = wM.t[:, 0:3072].rearrange("p (k b c) -> p k b c", b=3, c=128)
                          wb = wM.t[:, 3072:5120].rearrange("p (k c) -> p k c", c=128)
                          for b_, (k0, nk, yb) in enumerate(((0, 4, yA), (4, 8, yB), (12, 4, yC))):
                              pg = nxt("wide")
                              proj_feat(lambda kc, b_=b_: wg[:, kc, b_, :], pg)
                              kk.op("act", lambda h, b_=b_, pg=pg: h.activation(out=gs[b_].ap, in_=pg.t[:], func=AF.Sigmoid), r=pg, w=gs[b_])
                              pp = nxt("wide")
                              proj_feat(lambda kc, k0=k0: wb[:, k0 + kc, :], pp, nk=nk, rhs_of_kc=lambda kc, k0=k0: big.t[:, k0 + kc, :], rbufs=yb)
                              kk.op("dve", lambda h, b_=b_, pp=pp: h.tensor_tensor(out=ts[b_].ap, in0=pp.t[:], in1=gs[b_].ap, op=ALU.mult), r=[pp, gs[b_]], w=ts[b_])
                          kk.op("pool", lambda h: h.tensor_tensor(out=m1.ap, in0=ts[0].ap, in1=ts[1].ap, op=ALU.add), r=[ts[0], ts[1]], w=m1)
                          kk.op("pool", lambda h, jc=jc: h.tensor_tensor(out=big.t[:, 16 + jc, :], in0=m1.ap, in1=ts[2].ap, op=ALU.add), r=[m1, ts[2]], w=mTb)
                      chk("merge")
                      for n_ in range(2):
                          wo = wget(f"wout{n_}")
                          wov = wo.t[:, 0:4096].rearrange("p (k c) -> p k c", c=512)
                          for j in range(4):
                              t = 4 * T + j
                              p = nxt("wide")
                              for kc in range(8):
                                  kk.op("pe", lambda h, kc=kc, j=j, p=p: h.matmul(p.t[:], lhsT=big.t[:, 16 + kc, j * 128:(j + 1) * 128], rhs=wov[:, kc, :], start=(kc == 0), stop=(kc == 7)),
                                        r=[mTb, wo], w=p, inc=(kc == 7))
                              kk.op("dve", lambda h, t=t, n_=n_, p=p: h.tensor_tensor(out=xres.t[:, t, n_ * 512:(n_ + 1) * 512], in0=xres.t[:, t, n_ * 512:(n_ + 1) * 512], in1=p.t[:], op=ALU.add),
                                    r=[p, xres.bufs[t]], w=xres.bufs[t])
                      chk("wout")
                      norm_to_hT(T)
                      ar.reset(5120)
                      sgs = [ar.alloc([512], F32) for _ in range(2)]
                      t1s = [ar.alloc([512], F32) for _ in range(2)]
                      for jb in range(11):
                          wf = wget(f"ffin{jb}")
                          curw[0] = wf
                          wfv = wf.t[:, 0:4096].rearrange("p (k c) -> p k c", c=512)
                          for fc in range(2):
                              ff = jb * 2 + fc
                              pg = nxt("wide")
                              proj_feat(lambda kc, fc=fc: wfv[:, kc, fc * 128:(fc + 1) * 128], pg)
                              pu = nxt("wide")
                              proj_feat(lambda kc, fc=fc: wfv[:, kc, 256 + fc * 128:256 + (fc + 1) * 128], pu)
                              sg, t1_ = sgs[ff % 2], t1s[ff % 2]
                              kk.op("act", lambda h, pg=pg, sg=sg: h.activation(out=sg.ap, in_=pg.t[:], func=AF.Sigmoid), r=pg, w=sg)
                              kk.op("dve", lambda h, pg=pg, sg=sg, t1_=t1_: h.tensor_tensor(out=t1_.ap, in0=pg.t[:], in1=sg.ap, op=ALU.mult), r=[pg, sg], w=t1_)
                              kk.op("dve", lambda h, pu=pu, t1_=t1_, ff=ff: h.tensor_tensor(out=big.t[:, ff, :], in0=pu.t[:], in1=t1_.ap, op=ALU.mult), r=[pu, t1_], w=big.bufs)
                      for n_ in range(2):
                          wparts = [wget(f"ffout{n_ * 3 + i}", keep=i) for i in range(2)]
                          for j in range(4):
                              pass
                          pj = [nxt("mm"), nxt("mm"), nxt("mm"), pS]
                          kcs = [(0, 8), (8, 8), (16, 6)]
                          for pi in range(3):
                              wpart = wparts[pi] if pi < 2 else wget(f"ffout{n_ * 3 + 2}", keep=2)
                              kc0, nk = kcs[pi]
                              wpv = wpart.t[:, 0:nk * 512].rearrange("p (k c) -> p k c", c=512)
                              for j in range(4):
                                  for kc in range(nk):
                                      g = kc0 + kc
                                      kk.op("pe", lambda h, g=g, kc=kc, j=j, wpv=wpv: h.matmul(pj[j].t[:], lhsT=big.t[:, g, j * 128:(j + 1) * 128], rhs=wpv[:, kc, :], start=(g == 0), stop=(g == 21)),
                                            r=[big.bufs, wpart], w=pj[j], inc=(kc == nk - 1))
                          for j in range(4):
                              t = 4 * T + j
                              kk.op("dve", lambda h, t=t, n_=n_, j=j: h.tensor_tensor(out=xres.t[:, t, n_ * 512:(n_ + 1) * 512], in0=xres.t[:, t, n_ * 512:(n_ + 1) * 512], in1=pj[j].t[:], op=ALU.add),
                                    r=[pj[j], xres.bufs[t]], w=xres.bufs[t])
              ar.reset(0)
              obuf = [ar.alloc([D], F32) for _ in range(2)]
              junk = ar.alloc([D], BF16)
              fnw = ar.alloc([D], F32)
              kk.dma("pool", fnwst, fnw.ap, fnw_d.partition_broadcast(128), w=fnw)
              for t in range(NT):
                  xb = xres.bufs[t]
                  ss = newcol(1)
                  kk.op("act", lambda h, t=t, ss=ss: h.activation(out=junk.ap, in_=xres.t[:, t, :], func=AF.Square, accum_out=ss.ap), r=xb, w=[junk, ss])
                  rs = rstd_cols(ss, 1, 1.0 / D)
                  ob = obuf[t % 2]
                  kk.op("dve", lambda h, t=t, rs=rs, ob=ob: h.scalar_tensor_tensor(out=ob.ap, in0=xres.t[:, t, :], scalar=rs.ap, in1=fnw.ap, op0=ALU.mult, op1=ALU.mult), r=[xb, rs, fnw], w=ob)
                  kk.dma("pool", osts[t % 2], out_d[s_, t * 128:(t + 1) * 128, :], ob.ap, r=ob)
        except _Stop:
            for t in range(NT):
                kk.dma("sp", osts[t % 2], out_d[0, t * 128:(t + 1) * 128, :], xres.t[:, t, :], r=xres.bufs[t])
        for ost in osts:
            kk.E["pool"].h.wait_ge(ost.sem, ost.cnt)
            kk.E["sp"].h.wait_ge(ost.sem, ost.cnt)
        build.info = dict(ninst=kk.ninst, nsem=kk.nsem, arena_hi=ar.hi, sbuf_left=nc.sbuf_bytes_remaining)
    return nc


def host_consts():
    bf = ml_dtypes.bfloat16
    c = {}
    c["c_ident"] = np.eye(128, dtype=np.float32).astype(bf)
    s = np.arange(128)[:, None] % 64
    t = np.arange(64)[None, :]
    c["c_hgmask"] = (s <= t).astype(np.float32).astype(bf)
    s = np.arange(128)[:, None]
    t = np.arange(128)[None, :]
    c["c_causal"] = (s <= t).astype(np.float32).astype(bf)
    c["c_ones"] = np.ones((128, 128), np.float32).astype(bf)
    r = np.ones((128, 512), np.float32)
    r[:, ::64] = 0.0
    c["c_reset"] = r
    lg = np.array(LOG_GAMMA, np.float64)
    dm = np.zeros((128, 4, 128), np.float64)
    for h in range(4):
        dm[:, h, :] = np.where(s <= t, np.exp((t - s) * lg[h]), 0.0) * (128.0 ** -0.5)
    c["c_dmask"] = dm.astype(np.float32)
    qd = np.zeros((128, 4, 128), np.float64)
    for h in range(4):
        qd[:, h, :] = np.exp((np.arange(128)[None, :] + 1.0) * lg[h])
    c["c_qdec"] = qd.astype(np.float32)
    kd = np.zeros((128, 4), np.float64)
    for h in range(4):
        kd[:, h] = np.exp((127.0 - np.arange(128)) * lg[h]) * (128.0 ** -0.5)
    c["c_kdec"] = kd.astype(np.float32)
    c["c_invr"] = np.broadcast_to((10000.0 ** (-np.arange(64, dtype=np.float32) / 64)).astype(np.float32), (128, 64)).copy()
    c["c_invm"] = np.broadcast_to((10000.0 ** (-np.arange(32, dtype=np.float32) / 32)).astype(np.float32), (128, 32)).copy()
    return c


_NC_CACHE = {}


def kernel(**inputs):
    ncores = 8
    B = inputs["x"].shape[0]
    nseq = B // ncores
    key = (nseq, 2)
    if key not in _NC_CACHE:
        _NC_CACHE[key] = build(nseq=nseq, nlayers=2)
    nc = _NC_CACHE[key]
    consts = host_consts()
    in_maps = []
    for c in range(ncores):
        m = {}
        for k, v in inputs.items():
            v = np.asarray(v)
            if k in ("x", "positions"):
                m[k] = np.ascontiguousarray(v[c * nseq:(c + 1) * nseq])
            else:
                m[k] = np.ascontiguousarray(v)
        m.update(consts)
        in_maps.append(m)
    res = run_bass_kernel_spmd(nc, in_maps, core_ids=list(range(ncores)))
    out = np.concatenate([np.asarray(r["out"]) for r in res.results], axis=0)
    return out.astype(np.float32)
```

```python
import numpy as np
import ml_dtypes
from contextlib import ExitStack
import concourse.bass as bass
import concourse.mybir as mybir
from concourse.bass_utils import run_bass_kernel_spmd

F32, BF16, I32 = mybir.dt.float32, mybir.dt.bfloat16, mybir.dt.int32
AF = mybir.ActivationFunctionType
ALU = mybir.AluOpType

D = 1024
SEQ = 2048
NT = 16
TT = 512
NIN = 8640
DFF = 2816
EPS = 1e-6
S192 = 192.0 ** -0.5
LOG_GAMMA = [float(np.log1p(-(2.0 ** (-5.0 - h)))) for h in range(4)]
SLOT = 5120

BLK = ["hqf0", "hi", "hqf1", "hgate", "rq", "rk", "rv0", "rv1", "rg0", "rg1", "c", "mla"] + \
      [f"M{j}" for j in range(8)] + ["wout0", "wout1"] + [f"ffin{j}" for j in range(11)] + \
      [f"ffout{i}" for i in range(6)]
BIDX = {n: i for i, n in enumerate(BLK)}
NBLK = len(BLK)
WIN_C0 = {"hi": 1024, "hgate": 1536, "rq": 2048, "rk": 2560, "rv0": 3072,
          "rv1": 3584, "rg0": 4096, "rg1": 4608, "c": 5120}
FFOUT_PARTS = [(n, kc0, nk) for n in range(2) for (kc0, nk) in ((0, 8), (8, 8), (16, 6))]


def blk_size(name):
    if name == "c":
        return 8 * 448
    if name == "mla":
        return 3072
    if name.startswith("M"):
        return 5120
    if name.startswith("ffout"):
        return FFOUT_PARTS[int(name[5:])][2] * 512
    return 4096


class Buf:
    __slots__ = ("name", "w", "r", "psum")

    def __init__(self, name, psum=False):
        self.name = name
        self.w = None
        self.r = {}
        self.psum = psum


def flat(x):
    out = []

    def rec(y):
        if y is None:
            return
        if isinstance(y, Buf):
            out.append(y)
        elif hasattr(y, "bufs"):
            out.extend(y.bufs)
        else:
            for z in y:
                rec(z)
    rec(x)
    return out


class Eng:
    def __init__(self, h, key, is_pe=False):
        self.h = h
        self.key = key
        self.is_pe = is_pe
        self.sem = None
        self.cnt = 0
        self.seen = {}
        self.pend = False


class Stream:
    def __init__(self, kk, name):
        self.sem = kk.newsem(name)
        self.cnt = 0
        self.key = ("dma", name)
        self.gb = []


class K:
    SEM_LIMIT = 24000

    def __init__(self, nc, st):
        self.nc = nc
        self.st = st
        self.nsem = 0
        self.E = {}
        for key, h in (("pe", nc.tensor), ("act", nc.scalar), ("dve", nc.vector),
                       ("pool", nc.gpsimd), ("sp", nc.sync)):
            e = Eng(h, key, key == "pe")
            e.sem = self.newsem(key)
            self.E[key] = e
        self.ninst = 0

    def newsem(self, name):
        self.nsem += 1
        return self.st.enter_context(self.nc.semaphore(f"s{self.nsem}_{name}"))

    def _wait(self, E, r, w, skip_key=None):
        deps = {}

        def add(t):
            if t is None:
                return
            sem, val, key = t
            if key == E.key and E.is_pe:
                return
            if skip_key is not None and key == skip_key:
                return
            k = id(sem)
            if k not in deps or deps[k][1] < val:
                deps[k] = (sem, val)
        for b in r:
            add(b.w)
        for b in w:
            add(b.w)
            for k, t in b.r.items():
                add(t)
        for k, (sem, val) in deps.items():
            if E.seen.get(k, 0) < val:
                E.h.wait_ge(sem, val)
                E.seen[k] = val

    def op(self, eng, fn, r=(), w=(), inc=True):
        E = self.E[eng]
        r = flat(r)
        w = flat(w)
        w = w + [b for b in r if b.psum and b not in w]
        r = [b for b in r if not b.psum]
        self._wait(E, r, w)
        ins = fn(E.h)
        self.ninst += 1
        if inc:
            if E.cnt >= self.SEM_LIMIT and not E.pend:
                E.sem = self.newsem(E.key)
                E.cnt = 0
            E.cnt += 1
            ins.then_inc(E.sem, 1)
            tick = (E.sem, E.cnt, E.key)
            E.pend = False
        else:
            assert E.is_pe
            tick = (E.sem, E.cnt + 1, E.key)
            E.pend = True
        for b in r:
            b.r[E.key] = tick
        for b in w:
            b.w = tick
            b.r = {}
        return ins

    def dma(self, q, stream, out, in_, r=(), w=(), skip_same=False, **kw):
        E = self.E[q]
        r = flat(r)
        w = flat(w)
        self._wait(E, r, w, skip_key=stream.key if skip_same else None)
        ins = E.h.dma_start(out=out, in_=in_, **kw)
        self.ninst += 1
        stream.cnt += 16
        ins.then_inc(stream.sem, 16)
        tick = (stream.sem, stream.cnt, stream.key)
        if not skip_same:
            stream.gb = []
        for b in w:
            if b not in stream.gb:
                stream.gb.append(b)
        for b in r:
            b.r[stream.key] = tick
        for b in stream.gb:
            b.w = tick
            b.r = {}
        return ins

    def wait_all(self, eng, bufs):
        E = self.E[eng]
        self._wait(E, flat(bufs), flat(bufs))


class PT:
    def __init__(self, t, bufs):
        self.t = t
        self.bufs = bufs


class Tmp:
    def __init__(self, ap, bufs):
        self.ap = ap
        self.bufs = bufs


class Arena:
    PAGE = 512

    def __init__(self, nc, st, name, nelem_bf16):
        self.t = st.enter_context(nc.sbuf_tensor(name, [128, nelem_bf16], BF16))
        self.n = nelem_bf16
        self.pages = [Buf(f"{name}_p{i}") for i in range((nelem_bf16 + self.PAGE - 1) // self.PAGE)]
        self.off = 0
        self.hi = 0

    def reset(self, off=0):
        self.off = off

    def alloc(self, free_shape, dt, parts=128, align=False):
        if align:
            self.off = (self.off + self.PAGE - 1) // self.PAGE * self.PAGE
        n = int(np.prod(free_shape))
        nb = n * (2 if dt == F32 else 1)
        if dt == F32 and self.off % 2:
            self.off += 1
        o = self.off
        assert o + nb <= self.n, f"arena overflow {o + nb} > {self.n}"
        self.off += nb
        self.hi = max(self.hi, self.off)
        ap = self.t[0:parts, o:o + nb]
        if dt == F32:
            ap = ap.bitcast(F32)
        if len(free_shape) == 2:
            ap = ap.rearrange("p (a b) -> p a b", b=free_shape[1])
        elif len(free_shape) == 3:
            ap = ap.rearrange("p (a b c) -> p a b c", b=free_shape[1], c=free_shape[2])
        bufs = self.pages[o // self.PAGE:(o + nb - 1) // self.PAGE + 1]
        return Tmp(ap, bufs)


class _Stop(Exception):
    pass


def build(nseq=4, nlayers=2, dbg=False, stop=None):
    nc = bass.Bass("TRN2", target_bir_lowering=False)
    dr = {}

    def din(name, shape, dt=F32):
        dr[name] = nc.dram_tensor(name, list(shape), dt, kind="ExternalInput").ap()
        return dr[name]

    x_d = din("x", [nseq, SEQ, D])
    pos_d = din("positions", [nseq, SEQ], I32)
    nmw_d = din("norm_mix_w", [2, D])
    win_d = din("w_in", [2, D, NIN])
    hlb_d = din("hg_lower_bounds", [2, 512])
    hnw_d = din("hg_norm_w", [2, 512])
    rnw_d = din("ret_norm_w", [2, 1024])
    qnw_d = din("mla_q_norm_w", [2, 256])
    wuq_d = din("mla_w_uq", [2, 256, 768])
    kvnw_d = din("mla_kv_norm_w", [2, 128])
    wukv_d = din("mla_w_ukv", [2, 128, 1024])
    wba_d = din("w_br_a", [2, 512, D])
    wbb_d = din("w_br_b", [2, 1024, D])
    wbc_d = din("w_br_c", [2, 512, D])
    wout_d = din("w_out", [2, D, D])
    nfw_d = din("norm_ffn_w", [2, D])
    wfi_d = din("w_ffn_in", [2, D, 2 * DFF])
    wfo_d = din("w_ffn_out", [2, DFF, D])
    fnw_d = din("final_norm_w", [D])
    cident_d = din("c_ident", [128, 128], BF16)
    chgmask_d = din("c_hgmask", [128, 64], BF16)
    ccausal_d = din("c_causal", [128, 128], BF16)
    cones_d = din("c_ones", [128, 128], BF16)
    creset_d = din("c_reset", [128, 512])
    cdmask_d = din("c_dmask", [128, 4, 128])
    cqdec_d = din("c_qdec", [128, 4, 128])
    ckdec_d = din("c_kdec", [128, 4])
    cinvr_d = din("c_invr", [128, 64])
    cinvm_d = din("c_invm", [128, 32])
    out_d = nc.dram_tensor("out", [nseq, SEQ, D], F32, kind="ExternalOutput").ap()
    wscr = nc.dram_tensor("wscr", [2, NBLK, 128, SLOT], BF16, kind="Internal").ap()

    with ExitStack() as st:
        kk = K(nc, st)

        def sb(name, shape, dt, nb=1):
            t = st.enter_context(nc.sbuf_tensor(name, list(shape), dt))
            return PT(t, [Buf(f"{name}{i}") for i in range(nb)])

        def psb(name, shape, dt):
            t = st.enter_context(nc.psum_tensor(name, list(shape), dt))
            return PT(t, [Buf(name, psum=True)])

        xres = sb("xres", [128, NT, D], F32, NT)
        NSLOT = 3
        wsl = [sb(f"wsl{i}", [128, SLOT], BF16) for i in range(NSLOT)]
        wst = [Stream(kk, f"w{i}") for i in range(NSLOT)]
        hT = sb("hT", [128, 8, TT], BF16)
        kvnT = sb("kvnT", [128, SEQ], BF16, NT)
        kvtok = sb("kvtok", [128, NT, 128], BF16, NT)
        kpeT = sb("kpeT", [64, SEQ], BF16, NT)
        big = sb("big", [128, 24, TT], BF16, 4)
        yA, yB, yC, mTb = big.bufs
        ropeR = sb("ropeR", [128, NT, 2, 64], BF16)
        ropeM = sb("ropeM", [128, NT, 2, 32], BF16)
        shg_f = sb("shg_f", [128, 4, 128], F32, 4)
        shg_b = sb("shg_b", [128, 4, 128], BF16, 4)
        sr_f = sb("sr_f", [128, 4, 256], F32, 4)
        sr_b = sb("sr_b", [128, 4, 256], BF16, 4)
        ident = sb("ident", [128, 128], BF16)
        hgmask = sb("hgmask", [128, 64], BF16)
        causal = sb("causal", [128, 128], BF16)
        ones = sb("ones", [128, 128], BF16)
        resetc = sb("resetc", [128, 512], F32)
        dmask = sb("dmask", [128, 4, 128], F32)
        qdec = sb("qdec", [128, 4, 128], F32)
        kdecc = sb("kdecc", [128, 4], F32)
        invr = sb("invr", [128, 64], F32)
        invm = sb("invm", [128, 32], F32)
        gains = sb("gains", [128, 2, 32], F32)
        lbt = sb("lbt", [128, 2, 2, 4], F32)
        mhalf = sb("mhalf", [128, 8], F32)
        cols = sb("cols", [128, 64], F32)
        colbufs = [Buf(f"col{i}") for i in range(16)]
        colrr = [0]

        def newcol(n=4):
            i = colrr[0] % 16
            colrr[0] += 1
            return Tmp(cols.t[:, i * 4:i * 4 + n], [colbufs[i]])

        ar = Arena(nc, st, "arena", 22 * 1024)

        psT = [psb(f"psT{i}", [128, 1024], BF16) for i in range(2)]
        pmm = [psb(f"pmm{i}", [128, 512], F32) for i in range(3)]
        pacc = [psb(f"pacc{i}", [128, 512], F32) for i in range(2)]
        pS = psb("pS", [128, 512], F32)
        rr = {"T": 0, "mm": 0, "acc": 0, "wide": 0}
        pwide = pmm + pacc

        def nxt(kind):
            lst = {"T": psT, "mm": pmm, "acc": pacc, "wide": pwide}[kind]
            p = lst[rr[kind] % len(lst)]
            rr[kind] += 1
            return p

        def chk(name):
            if stop == name:
                if name in ("mla", "merge", "wout"):
                    kk.op("act", lambda h: h.activation(out=xres.t[:, 0:12, :].rearrange("p a b -> p (a b)"), in_=big.t[:].rearrange("p a b -> p (a b)"), func=AF.Copy), r=big.bufs, w=xres.bufs[0:12])
                if name == "rope":
                    kk.op("act", lambda h: h.activation(out=xres.t[:, 12:14, :].rearrange("p a b -> p (a b)"), in_=ropeR.t[:].rearrange("p a b c -> p (a b c)"), func=AF.Copy), r=ropeR, w=xres.bufs[12:14])
                    kk.op("act", lambda h: h.activation(out=xres.t[:, 14, :], in_=ropeM.t[:].rearrange("p a b c -> p (a b c)"), func=AF.Copy), r=ropeM, w=xres.bufs[14])
                raise _Stop()

        cst = Stream(kk, "const")
        xst = Stream(kk, "xload")
        osts = [Stream(kk, f"ostore{i}") for i in range(2)]
        pst_lds = [Stream(kk, f"pre_ld{i}") for i in range(2)]
        pst_sts = [Stream(kk, f"pre_st{i}") for i in range(2)]
        posst = Stream(kk, "posld")
        fnwst = Stream(kk, "fnwld")

        for t_, d_ in ((ident, cident_d), (hgmask, chgmask_d), (causal, ccausal_d), (ones, cones_d),
                       (resetc, creset_d), (dmask, cdmask_d), (qdec, cqdec_d), (kdecc, ckdec_d),
                       (invr, cinvr_d), (invm, cinvm_d)):
            kk.dma("sp", cst, t_.t[:], d_[:], w=t_, skip_same=(cst.cnt > 0))
        for l in range(2):
            for (src, n, c0) in ((nmw_d, 8, 0), (nfw_d, 8, 8), (hnw_d, 4, 16), (rnw_d, 8, 20),
                                 (qnw_d, 2, 28), (kvnw_d, 1, 30)):
                for k_ in range(n):
                    kk.dma("sp", cst, gains.t[:, l, c0 + k_:c0 + k_ + 1], src[l, k_ * 128:(k_ + 1) * 128].rearrange("(p o) -> p o", o=1),
                           w=gains, skip_same=True)
        kk.op("pool", lambda h: h.memset(mhalf.t[:], -0.5), w=mhalf)
        lbraw = ar.alloc([2, 4], F32)
        for l in range(2):
            for k_ in range(4):
                kk.dma("sp", cst, lbraw.ap[:, l, k_:k_ + 1], hlb_d[l, k_ * 128:(k_ + 1) * 128].rearrange("(p o) -> p o", o=1), w=lbraw,
                       skip_same=True)
        lbe = ar.alloc([2, 4], F32)
        lbs = ar.alloc([4], F32)
        lbp = ar.alloc([2, 4], F32)
        kk.op("act", lambda h: h.activation(out=lbe.ap, in_=lbraw.ap, func=AF.Exp), r=lbraw, w=lbe)
        kk.op("dve", lambda h: h.tensor_tensor(out=lbs.ap, in0=lbe.ap[:, 0, :], in1=lbe.ap[:, 1, :], op=ALU.add), r=lbe, w=lbs)
        kk.op("dve", lambda h: h.reciprocal(out=lbs.ap, in_=lbs.ap), r=lbs, w=lbs)
        for l in range(2):
            kk.op("dve", lambda h, l=l: h.tensor_tensor(out=lbp.ap[:, l, :], in0=lbe.ap[:, l, :], in1=lbs.ap, op=ALU.mult), r=[lbe, lbs], w=lbp)
        kk.op("dve", lambda h: h.tensor_tensor(out=lbt.t[:, 0, 0, :], in0=lbp.ap[:, 0, :], in1=lbp.ap[:, 0, :], op=ALU.subtract), r=lbp, w=lbt)
        cum1 = ar.alloc([4], F32)
        kk.op("dve", lambda h: h.tensor_tensor(out=cum1.ap, in0=lbp.ap[:, 0, :], in1=lbp.ap[:, 1, :], op=ALU.add), r=lbp, w=cum1)
        kk.op("dve", lambda h: h.tensor_tensor(out=lbt.t[:, 1, 0, :], in0=cum1.ap, in1=lbp.ap[:, 0, :], op=ALU.subtract), r=[cum1, lbp], w=lbt)
        for l in range(2):
            kk.op("dve", lambda h, l=l: h.tensor_scalar(out=lbt.t[:, l, 1, :], in0=lbt.t[:, l, 0, :], scalar1=-1.0, scalar2=1.0, op0=ALU.mult, op1=ALU.add), r=lbt, w=lbt)

        chk("consts")
        stg_f = [Tmp(xres.t[:, 5 * i:5 * i + 5, :].rearrange("p a b -> p (a b)"), xres.bufs[5 * i:5 * i + 5]) for i in range(2)]
        stg_b = [Tmp(xres.t[:, 10 + 3 * i:13 + 3 * i, :].rearrange("p a b -> p (a b)").bitcast(BF16)[:, 0:SLOT], xres.bufs[10 + 3 * i:13 + 3 * i]) for i in range(2)]
        cast_rr = [0]

        def cast(dst, src, gain):
            e = ("dve", "act")[cast_rr[0] % 2]
            cast_rr[0] += 1
            if gain is None:
                if e == "act":
                    return ("act", lambda h: h.activation(out=dst, in_=src, func=AF.Copy))
                return (e, lambda h: h.tensor_copy(out=dst, in_=src))
            if e == "act":
                return ("act", lambda h: h.activation(out=dst, in_=src, func=AF.Copy, scale=gain))
            return (e, lambda h: h.tensor_scalar(out=dst, in0=src, scalar1=gain, scalar2=None, op0=ALU.mult))

        pre_i = [0]

        def prepass_block(l, name):
            i = pre_i[0] % 2
            pre_i[0] += 1
            sf, sbf = stg_f[i], stg_b[i]
            pst_ld, pst_st = pst_lds[i], pst_sts[i]
            pieces = []

            def g(c):
                return gains.t[:, l, c:c + 1]
            if name in WIN_C0:
                c0 = WIN_C0[name]
                ncol = 448 if name == "c" else 512
                kk.dma("sp", pst_ld, sf.ap[:, 0:8 * ncol].rearrange("p (k c) -> p k c", c=ncol),
                       win_d[l, :, c0:c0 + ncol].rearrange("(k p) c -> p k c", p=128), w=sf)
                for kc in range(8):
                    pieces.append((kc * ncol, ncol, kc * ncol, g(kc)))
            elif name == "mla":
                kk.dma("sp", pst_ld, sf.ap[:, 0:1536].rearrange("p (k c) -> p k c", c=768),
                       wuq_d[l].rearrange("(k p) c -> p k c", p=128), w=sf)
                kk.dma("sp", pst_ld, sf.ap[:, 1536:2560], wukv_d[l], w=sf, skip_same=True)
                for kc in range(2):
                    pieces.append((kc * 768, 768, kc * 768, g(28 + kc)))
                pieces.append((1536, 1024, 1536, g(30)))
            elif name.startswith("M"):
                j = int(name[1:])
                for b_ in range(3):
                    cg = 5568 + b_ * 1024 + j * 128
                    kk.dma("sp", pst_ld, sf.ap[:, 0:3072].rearrange("p (k b c) -> p k b c", b=3, c=128)[:, :, b_, :],
                           win_d[l, :, cg:cg + 128].rearrange("(k p) c -> p k c", p=128), w=sf, skip_same=(b_ > 0))
                kk.dma("sp", pst_ld, sf.ap[:, 3072:3584].rearrange("p (k c) -> p k c", c=128),
                       wba_d[l, :, j * 128:(j + 1) * 128].rearrange("(k p) c -> p k c", p=128), w=sf, skip_same=True)
                kk.dma("sp", pst_ld, sf.ap[:, 3584:4608].rearrange("p (k c) -> p k c", c=128),
                       wbb_d[l, :, j * 128:(j + 1) * 128].rearrange("(k p) c -> p k c", p=128), w=sf, skip_same=True)
                kk.dma("sp", pst_ld, sf.ap[:, 4608:5120].rearrange("p (k c) -> p k c", c=128),
                       wbc_d[l, :, j * 128:(j + 1) * 128].rearrange("(k p) c -> p k c", p=128), w=sf, skip_same=True)
                for kc in range(8):
                    pieces.append((kc * 384, 384, kc * 384, g(kc)))
                for kc in range(4):
                    pieces.append((3072 + kc * 128, 128, 3072 + kc * 128, g(16 + kc)))
                for kc in range(8):
                    pieces.append((3584 + kc * 128, 128, 3584 + kc * 128, g(20 + kc)))
                pieces.append((4608, 512, 4608, None))
            elif name.startswith("wout"):
                n = int(name[4:])
                kk.dma("sp", pst_ld, sf.ap[:, 0:4096].rearrange("p (k c) -> p k c", c=512),
                       wout_d[l, :, n * 512:(n + 1) * 512].rearrange("(k p) c -> p k c", p=128), w=sf)
                pieces.append((0, 4096, 0, None))
            elif name.startswith("hqf"):
                pp = int(name[3:])
                v = sf.ap[:, 0:4096].rearrange("p (k c) -> p k c", c=512)
                kk.dma("sp", pst_ld, v[:, :, 0:256],
                       win_d[l, :, pp * 256:(pp + 1) * 256].rearrange("(k p) c -> p k c", p=128), w=sf)
                kk.dma("sp", pst_ld, v[:, :, 256:512],
                       win_d[l, :, 512 + pp * 256:512 + (pp + 1) * 256].rearrange("(k p) c -> p k c", p=128), w=sf, skip_same=True)
                for kc in range(8):
                    pieces.append((kc * 512, 512, kc * 512, g(kc)))
            elif name.startswith("ffin"):
                jb = int(name[4:])
                v = sf.ap[:, 0:4096].rearrange("p (k c) -> p k c", c=512)
                kk.dma("sp", pst_ld, v[:, :, 0:256],
                       wfi_d[l, :, jb * 256:(jb + 1) * 256].rearrange("(k p) c -> p k c", p=128), w=sf)
                kk.dma("sp", pst_ld, v[:, :, 256:512],
                       wfi_d[l, :, DFF + jb * 256:DFF + (jb + 1) * 256].rearrange("(k p) c -> p k c", p=128), w=sf, skip_same=True)
                for kc in range(8):
                    pieces.append((kc * 512, 512, kc * 512, g(8 + kc)))
            elif name.startswith("ffout"):
                n, kc0, nk = FFOUT_PARTS[int(name[5:])]
                kk.dma("sp", pst_ld, sf.ap[:, 0:nk * 512].rearrange("p (k c) -> p k c", c=512),
                       wfo_d[l, kc0 * 128:(kc0 + nk) * 128, n * 512:(n + 1) * 512].rearrange("(k p) c -> p k c", p=128), w=sf)
                pieces.append((0, nk * 512, 0, None))
            return (l, name, sf, sbf, pst_st, pieces)

        def prepass_finish(l, name, sf, sbf, pst_st, pieces):
            for (do, n, so, gn) in pieces:
                e, fn = cast(sbf.ap[:, do:do + n], sf.ap[:, so:so + n], gn)
                kk.op(e, fn, r=[sf, gains], w=sbf)
            if name == "mla":
                p = nxt("T")
                for hh in range(4):
                    kk.op("pe", lambda h, hh=hh: h.transpose(out=p.t[:, hh * 128:(hh + 1) * 128], in_=sbf.ap[:, 1536 + hh * 256:1536 + hh * 256 + 128], identity=ident.t[:]),
                          r=[sbf, ident], w=p, inc=(hh == 3))
                kk.op("dve", lambda h: h.tensor_copy(out=sbf.ap[:, 2560:3072], in_=p.t[:, 0:512]), r=p, w=sbf)
            sz = blk_size(name)
            kk.dma("act", pst_st, wscr[l, BIDX[name], :, 0:sz], sbf.ap[:, 0:sz], r=sbf)

        pend = None
        for l in range(nlayers):
            for name in BLK:
                cur = prepass_block(l, name)
                if pend is not None:
                    prepass_finish(*pend)
                pend = cur
        prepass_finish(*pend)
        for e in ("sp", "pool", "act"):
            kk.wait_all(e, [stg_f, stg_b])

        wq = []
        for s_ in range(nseq):
            for l in range(nlayers):
                for T in range(4):
                    for name in BLK:
                        wq.append((l, name))
        wstate = {"issued": 0, "used": 0}

        def w_issue(upto):
            while wstate["issued"] < min(upto, len(wq)):
                i = wstate["issued"]
                l, name = wq[i]
                sl = i % NSLOT
                sz = blk_size(name)
                kk.dma("sp", wst[sl], wsl[sl].t[:, 0:sz], wscr[l, BIDX[name], :, 0:sz], w=wsl[sl])
                wstate["issued"] += 1

        def wget(name, keep=0):
            i = wstate["used"]
            assert wq[i][1] == name, (wq[i], name)
            assert keep < NSLOT
            w_issue(i - keep + NSLOT)
            wstate["used"] += 1
            return wsl[i % NSLOT]

        def rstd_cols(ss, n, inv_d):
            t1 = newcol(n)
            kk.op("pool", lambda h: h.tensor_scalar(out=t1.ap, in0=ss.ap, scalar1=inv_d, scalar2=EPS, op0=ALU.mult, op1=ALU.add), r=ss, w=t1)
            t2 = newcol(n)
            kk.op("pool", lambda h: h.tensor_tensor(out=t2.ap, in0=t1.ap, in1=mhalf.t[:, 0:n], op=ALU.pow), r=[t1, mhalf], w=t2)
            return t2

        def norm_to_hT(T):
            ar.reset(0)
            junk = ar.alloc([D], BF16)
            hns = [ar.alloc([D], BF16) for _ in range(4)]
            ss = newcol(4)
            for j in range(4):
                t = 4 * T + j
                kk.op("act", lambda h, t=t, j=j: h.activation(out=junk.ap, in_=xres.t[:, t, :], func=AF.Square, accum_out=ss.ap[:, j:j + 1]), r=xres.bufs[t], w=[junk, ss])
            rs = rstd_cols(ss, 4, 1.0 / D)
            for j in range(4):
                t = 4 * T + j
                kk.op("dve", lambda h, t=t, j=j: h.tensor_scalar(out=hns[j].ap, in0=xres.t[:, t, :], scalar1=rs.ap[:, j:j + 1], scalar2=None, op0=ALU.mult), r=[xres.bufs[t], rs], w=hns[j])
            for j in range(4):
                p = nxt("T")
                for kc in range(8):
                    kk.op("pe", lambda h, kc=kc, j=j, p=p: h.transpose(out=p.t[:, kc * 128:(kc + 1) * 128], in_=hns[j].ap[:, kc * 128:(kc + 1) * 128], identity=ident.t[:]),
                          r=[hns[j], ident], w=p, inc=(kc == 7))
                kk.op("act", lambda h, j=j, p=p: h.activation(out=hT.t[:, :, j * 128:(j + 1) * 128], in_=p.t[:].rearrange("p (k c) -> p k c", c=128), func=AF.Copy), r=p, w=hT)

        def proj_tok(wap, ncol, j, p, rhsT=None):
            for kc in range(8):
                kk.op("pe", lambda h, kc=kc: h.matmul(p.t[:, 0:ncol], lhsT=hT.t[:, kc, j * 128:(j + 1) * 128], rhs=wap[:, kc, :], start=(kc == 0), stop=(kc == 7)),
                      r=[hT, curw[0]], w=p, inc=(kc == 7))

        def proj_feat(lhs_of_kc, p, nk=8, rhs_of_kc=None, rbufs=None):
            for kc in range(nk):
                rhs = hT.t[:, kc, :] if rhs_of_kc is None else rhs_of_kc(kc)
                kk.op("pe", lambda h, kc=kc, rhs=rhs: h.matmul(p.t[:, 0:TT], lhsT=lhs_of_kc(kc), rhs=rhs, start=(kc == 0), stop=(kc == nk - 1)),
                      r=[hT if rbufs is None else rbufs, curw[0]], w=p, inc=(kc == nk - 1))

        curw = [None]

        def transposes_to(src_of, n, dst_ap_of_group, rbufs, wbufs, evac="act", parts=128, width=128):
            i = 0
            while i < n:
                m = min(8, n - i)
                p = nxt("T")
                for q in range(m):
                    kk.op("pe", lambda h, q=q, i=i: h.transpose(out=p.t[0:width, q * 128:(q + 1) * 128], in_=src_of(i + q), identity=ident.t[:]),
                          r=[rbufs, ident], w=p, inc=(q == m - 1))
                dst_ap_of_group(i, m, p)
                i += m

        try:
          chk("prepass")
          for s_ in range(nseq):
              for q in range(4):
                  kk.dma("pool", xst, xres.t[:, 4 * q:4 * q + 4, :], x_d[s_, q * 512:(q + 1) * 512, :].rearrange("(t p) d -> p t d", p=128),
                         w=xres.bufs[4 * q:4 * q + 4], skip_same=(q > 0))
              ar.reset(0)
              chk("xload")
              posi = ar.alloc([NT], I32 if False else F32)
              posi_i = Tmp(posi.ap.bitcast(I32), posi.bufs)
              for t in range(NT):
                  kk.dma("pool", posst, posi_i.ap[:, t:t + 1], pos_d[s_, t * 128:(t + 1) * 128].rearrange("(p o) -> p o", o=1), w=posi, skip_same=(t > 0))
              posf = ar.alloc([NT], F32)
              kk.op("dve", lambda h: h.tensor_copy(out=posf.ap, in_=posi_i.ap), r=posi, w=posf)
              rope_base = ar.off
              for (tab, inv, half) in ((ropeR, invr, 64), (ropeM, invm, 32)):
                  ar.reset(rope_base)
                  ang = ar.alloc([NT, half], F32)
                  for t in range(NT):
                      kk.op("dve", lambda h, t=t: h.tensor_scalar(out=ang.ap[:, t, :], in0=inv.t[:, 0:half], scalar1=posf.ap[:, t:t + 1], scalar2=None, op0=ALU.mult), r=[inv, posf], w=ang)
                  a2 = ar.alloc([2, NT, half], F32)
                  inv2pi = float(1.0 / (2 * np.pi))
                  kk.op("dve", lambda h: h.tensor_scalar(out=a2.ap[:, 1], in0=ang.ap, scalar1=inv2pi, scalar2=None, op0=ALU.mult), r=ang, w=a2)
                  kk.op("dve", lambda h: h.tensor_scalar(out=a2.ap[:, 0], in0=ang.ap, scalar1=inv2pi, scalar2=0.25, op0=ALU.mult, op1=ALU.add), r=ang, w=a2)
                  kf = ar.alloc([2, NT, half], F32)
                  ki = Tmp(kf.ap.bitcast(I32), kf.bufs)
                  kk.op("dve", lambda h: h.tensor_copy(out=ki.ap, in_=a2.ap), r=a2, w=kf)
                  kf2 = ar.alloc([2, NT, half], F32)
                  kk.op("dve", lambda h: h.tensor_copy(out=kf2.ap, in_=ki.ap), r=kf, w=kf2)
                  kk.op("dve", lambda h: h.tensor_tensor(out=a2.ap, in0=a2.ap, in1=kf2.ap, op=ALU.subtract), r=[a2, kf2], w=a2)
                  kk.op("dve", lambda h: h.tensor_scalar(out=kf2.ap, in0=a2.ap, scalar1=0.5, scalar2=None, op0=ALU.is_gt), r=a2, w=kf2)
                  kk.op("dve", lambda h: h.tensor_tensor(out=a2.ap, in0=a2.ap, in1=kf2.ap, op=ALU.subtract), r=[a2, kf2], w=a2)
                  s2 = ar.alloc([2, NT, half], F32)
                  kk.op("act", lambda h: h.activation(out=s2.ap, in_=a2.ap, func=AF.Sin, scale=float(2 * np.pi * (1 - 1e-6))), r=a2, w=s2)
                  for c in range(2):
                      kk.op("dve", lambda h, c=c: h.tensor_copy(out=tab.t[:, :, c, :], in_=s2.ap[:, c]), r=s2, w=tab)

              chk("rope")
              for l in range(nlayers):
                  kk.op("pool", lambda h: h.memset(shg_f.t[:], 0.0), w=shg_f)
                  kk.op("pool", lambda h: h.memset(shg_b.t[:], 0.0), w=shg_b)
                  kk.op("pool", lambda h: h.memset(sr_f.t[:], 0.0), w=sr_f)
                  kk.op("pool", lambda h: h.memset(sr_b.t[:], 0.0), w=sr_b)
                  for T in range(4):
                      norm_to_hT(T)
                      chk("norm")
                      ar.reset(0)
                      v_hg = ar.alloc([4, 512], BF16)
                      gate_a = ar.alloc([4, 512], BF16)
                      y_a = ar.alloc([4, 512], BF16)
                      sg = ar.alloc([512], F32)
                      t0 = ar.alloc([512], F32)
                      t1 = ar.alloc([512], F32)
                      t2 = ar.alloc([512], F32)
                      t3 = ar.alloc([512], F32)
                      q_f = ar.alloc([512], F32)
                      k_f = ar.alloc([512], F32)
                      hsets = []
                      for hh in range(2):
                          hsets.append(dict(ec=ar.alloc([512], F32), qe=ar.alloc([512], BF16), ke=ar.alloc([512], BF16), qc=ar.alloc([512], BF16),
                                            kdT=ar.alloc([512], BF16), kd=ar.alloc([4, 128], BF16), at=ar.alloc([4, 64], BF16)))
                      bigf = big.t[:, 4:24, :].rearrange("p a b -> p (a b)")
                      bb = [yB, yC, mTb]
                      for hh in range(2):
                          o_ = hh * 3840
                          hsets.append(dict(qe=Tmp(bigf[:, o_:o_ + 512], bb), ke=Tmp(bigf[:, o_ + 512:o_ + 1024], bb), qc=Tmp(bigf[:, o_ + 1024:o_ + 1536], bb),
                                            kdT=Tmp(bigf[:, o_ + 1536:o_ + 2048], bb), kd=Tmp(bigf[:, o_ + 2048:o_ + 2560].rearrange("p (j c) -> p j c", c=128), bb),
                                            at=Tmp(bigf[:, o_ + 2560:o_ + 2816].rearrange("p (j c) -> p j c", c=64), bb),
                                            ec=Tmp(bigf[:, o_ + 2816:o_ + 3840].bitcast(F32), bb)))
                      accS = [pacc[0], pacc[1], pS]
                      accS_rr = [0]

                      def nxt_accS():
                          p = accS[accS_rr[0] % 3]
                          accS_rr[0] += 1
                          return p

                      def c3(ap):
                          return ap.rearrange("p (c t) -> p c t", t=64)

                      def hgA(hd, wblk):
                          hh = hd % 2
                          wv_ = wblk.t[:, 0:4096].rearrange("p (k c) -> p k c", c=512)
                          curw[0] = wblk
                          pq = nxt_accS()
                          proj_feat(lambda kc: wv_[:, kc, hh * 128:(hh + 1) * 128], pq)
                          pf = nxt_accS()
                          proj_feat(lambda kc: wv_[:, kc, 256 + hh * 128:256 + (hh + 1) * 128], pf)
                          return pq, pf

                      def hgB(hd, pq, pf):
                          S_ = hsets[hd]
                          ec, qe, ke, qc, kdT = S_["ec"], S_["qe"], S_["ke"], S_["qc"], S_["kdT"]
                          kk.op("act", lambda h: h.activation(out=t0.ap, in_=pq.t[:], func=AF.Sigmoid), r=pq, w=t0)
                          kk.op("dve", lambda h: h.tensor_tensor(out=q_f.ap, in0=pq.t[:], in1=t0.ap, op=ALU.mult), r=[pq, t0], w=q_f)
                          kk.op("act", lambda h: h.activation(out=t0.ap, in_=pf.t[:], func=AF.Sigmoid), r=pf, w=t0)
                          kk.op("dve", lambda h: h.tensor_scalar(out=t1.ap, in0=t0.ap, scalar1=lbt.t[:, l, 1, hd:hd + 1], scalar2=lbt.t[:, l, 0, hd:hd + 1], op0=ALU.mult, op1=ALU.add), r=[t0, lbt], w=t1)
                          kk.op("act", lambda h: h.activation(out=t0.ap, in_=t1.ap, func=AF.Ln), r=t1, w=t0)
                          kk.op("dve", lambda h: h.tensor_scalar(out=k_f.ap, in0=t1.ap, scalar1=-1.0, scalar2=1.0, op0=ALU.mult, op1=ALU.add), r=t1, w=k_f)
                          kk.op("dve", lambda h: h.tensor_tensor_scan(out=t1.ap, data0=resetc.t[:], data1=t0.ap, initial=0.0, op0=ALU.mult, op1=ALU.add), r=[resetc, t0], w=t1)
                          kk.op("dve", lambda h: h.tensor_tensor(out=c3(t0.ap), in0=c3(t1.ap), in1=c3(t1.ap)[:, :, 31:32].broadcast_to([128, 8, 64]), op=ALU.subtract), r=t1, w=t0)
                          kk.op("dve", lambda h: h.tensor_tensor(out=c3(t2.ap), in0=c3(t1.ap), in1=c3(t1.ap)[:, :, 63:64].broadcast_to([128, 8, 64]), op=ALU.subtract), r=t1, w=t2)
                          kk.op("act", lambda h: h.activation(out=t2.ap, in_=t2.ap, func=AF.Exp, scale=-1.0), r=t2, w=t2)
                          kk.op("act", lambda h: h.activation(out=t3.ap, in_=t0.ap, func=AF.Exp), r=t0, w=t3)
                          kk.op("act", lambda h: h.activation(out=t0.ap, in_=t0.ap, func=AF.Exp, scale=-1.0), r=t0, w=t0)
                          kk.op("act", lambda h: h.activation(out=ec.ap, in_=t1.ap, func=AF.Exp), r=t1, w=ec)
                          kk.op("dve", lambda h: h.tensor_tensor(out=kdT.ap, in0=k_f.ap, in1=t2.ap, op=ALU.mult), r=[k_f, t2], w=kdT)
                          kk.op("dve", lambda h: h.tensor_tensor(out=qe.ap, in0=q_f.ap, in1=t3.ap, op=ALU.mult), r=[q_f, t3], w=qe)
                          kk.op("dve", lambda h: h.tensor_tensor(out=ke.ap, in0=k_f.ap, in1=t0.ap, op=ALU.mult), r=[k_f, t0], w=ke)
                          kk.op("pool", lambda h: h.tensor_tensor(out=qc.ap, in0=q_f.ap, in1=ec.ap, op=ALU.mult), r=[q_f, ec], w=qc)

                      def hgC(hd):
                          S_ = hsets[hd]
                          qe, ke, kdT, kd, at = S_["qe"], S_["ke"], S_["kdT"], S_["kd"], S_["at"]
                          pT_ = nxt("T")
                          for j in range(4):
                              kk.op("pe", lambda h, j=j: h.transpose(out=pT_.t[:, j * 128:(j + 1) * 128], in_=kdT.ap[:, j * 128:(j + 1) * 128], identity=ident.t[:]),
                                    r=[kdT, ident], w=pT_, inc=(j == 3))
                          kk.op("act", lambda h: h.activation(out=kd.ap, in_=pT_.t[:, 0:512].rearrange("p (j c) -> p j c", c=128), func=AF.Copy), r=pT_, w=kd)
                          pa = nxt("mm")
                          for ci in range(8):
                              j, hf_ = ci // 2, ci % 2
                              kk.op("pe", lambda h, ci=ci, j=j, hf_=hf_: h.matmul(pa.t[hf_ * 64:(hf_ + 1) * 64, j * 64:(j + 1) * 64], lhsT=ke.ap[:, ci * 64:(ci + 1) * 64], rhs=qe.ap[:, ci * 64:(ci + 1) * 64], start=True, stop=True),
                                    r=[ke, qe], w=pa, inc=(ci == 7))
                          kk.op("dve", lambda h: h.tensor_tensor(out=at.ap, in0=pa.t[:, 0:256].rearrange("p (j t) -> p j t", t=64), in1=hgmask.t[:].unsqueeze(1).broadcast_to([128, 4, 64]), op=ALU.mult), r=[pa, hgmask], w=at)

                      def fill_hi(wblk):
                          curw[0] = wblk
                          wv_ = wblk.t[:, 0:4096].rearrange("p (k c) -> p k c", c=512)
                          for j in range(4):
                              p = nxt("mm")
                              proj_tok(wv_, 512, j, p)
                              kk.op("act", lambda h, j=j, p=p: h.activation(out=v_hg.ap[:, j, :], in_=p.t[:], func=AF.Copy), r=p, w=v_hg)

                      def fill_hgate(wblk):
                          curw[0] = wblk
                          wv_ = wblk.t[:, 0:4096].rearrange("p (k c) -> p k c", c=512)
                          for j in range(4):
                              p = nxt("mm")
                              proj_tok(wv_, 512, j, p)
                              kk.op("act", lambda h, p=p: h.activation(out=sg.ap, in_=p.t[:], func=AF.Sigmoid), r=p, w=sg)
                              kk.op("dve", lambda h, j=j, p=p: h.tensor_tensor(out=gate_a.ap[:, j, :], in0=p.t[:], in1=sg.ap, op=ALU.mult), r=[p, sg], w=gate_a)

                      wqf0 = wget("hqf0")
                      pqf = hgA(0, wqf0)
                      hgB(0, *pqf)
                      pqf = hgA(1, wqf0)
                      w_hi = wget("hi", keep=1)
                      fill_hi(w_hi)
                      hgC(0)
                      hgB(1, *pqf)
                      wqf1 = wget("hqf1")
                      pqf = hgA(2, wqf1)
                      w_hg = wget("hgate", keep=1)
                      fill_hgate(w_hg)
                      hgC(1)
                      hgB(2, *pqf)
                      pqf = hgA(3, wqf1)
                      hgC(2)
                      hgB(3, *pqf)
                      hgC(3)
                      for hp in range(2):
                          pos_ = {0: pacc[0], 1: pacc[1]}
                          for ci in range(8):
                              j, hf_ = ci // 2, ci % 2
                              P0, P1 = hf_ * 64, (hf_ + 1) * 64
                              for hh in range(2):
                                  hd = 2 * hp + hh
                                  S_ = hsets[hd]
                                  ec, qc, kd, at, po = S_["ec"], S_["qc"], S_["kd"], S_["at"], pos_[hh]
                                  kk.op("pe", lambda h, po=po, at=at, j=j, hd=hd, P0=P0, P1=P1: h.matmul(po.t[P0:P1, j * 128:(j + 1) * 128], lhsT=at.ap[P0:P1, j, :], rhs=v_hg.ap[P0:P1, j, hd * 128:(hd + 1) * 128], start=True, stop=False),
                                        r=[at, v_hg], w=po, inc=False)
                                  kk.op("pe", lambda h, po=po, qc=qc, ci=ci, j=j, hd=hd, P0=P0, P1=P1: h.matmul(po.t[P0:P1, j * 128:(j + 1) * 128], lhsT=qc.ap[:, ci * 64:(ci + 1) * 64], rhs=shg_b.t[:, hd, :], start=False, stop=True),
                                        r=[qc, shg_b.bufs[hd]], w=po, inc=True)
                                  pSx = nxt("mm")
                                  kk.op("pe", lambda h, kd=kd, j=j, hd=hd, P0=P0, P1=P1, pSx=pSx: h.matmul(pSx.t[:, 0:128], lhsT=kd.ap[P0:P1, j, :], rhs=v_hg.ap[P0:P1, j, hd * 128:(hd + 1) * 128], start=True, stop=True),
                                        r=[kd, v_hg], w=pSx)
                                  kk.op("dve", lambda h, ec=ec, ci=ci, hd=hd, pSx=pSx: h.scalar_tensor_tensor(out=shg_f.t[:, hd, :], in0=shg_f.t[:, hd, :], scalar=ec.ap[:, ci * 64 + 63:ci * 64 + 64], in1=pSx.t[:, 0:128], op0=ALU.mult, op1=ALU.add),
                                        r=[ec, pSx, shg_f.bufs[hd]], w=shg_f.bufs[hd])
                                  kk.op("act", lambda h, hd=hd: h.activation(out=shg_b.t[:, hd, :], in_=shg_f.t[:, hd, :], func=AF.Copy), r=shg_f.bufs[hd], w=shg_b.bufs[hd])
                          for hh in range(2):
                              hd = 2 * hp + hh
                              po = pos_[hh]
                              ss = newcol(4)
                              for j in range(4):
                                  kk.op("act", lambda h, j=j, po=po, ss=ss: h.activation(out=t3.ap[:, 0:128], in_=po.t[:, j * 128:(j + 1) * 128], func=AF.Square, accum_out=ss.ap[:, j:j + 1]), r=po, w=[t3, ss])
                              rs = rstd_cols(ss, 4, 1.0 / 128)
                              for j in range(4):
                                  kk.op("dve", lambda h, j=j, po=po, rs=rs, hd=hd: h.scalar_tensor_tensor(out=y_a.ap[:, j, hd * 128:(hd + 1) * 128], in0=po.t[:, j * 128:(j + 1) * 128], scalar=rs.ap[:, j:j + 1], in1=gate_a.ap[:, j, hd * 128:(hd + 1) * 128], op0=ALU.mult, op1=ALU.mult),
                                        r=[po, rs, gate_a], w=y_a)
                      for j in range(4):
                          p = nxt("T")
                          for kc in range(4):
                              kk.op("pe", lambda h, j=j, kc=kc, p=p: h.transpose(out=p.t[:, kc * 128:(kc + 1) * 128], in_=y_a.ap[:, j, kc * 128:(kc + 1) * 128], identity=ident.t[:]),
                                    r=[y_a, ident], w=p, inc=(kc == 3))
                          kk.op("act", lambda h, j=j, p=p: h.activation(out=big.t[:, 0:4, j * 128:(j + 1) * 128], in_=p.t[:, 0:512].rearrange("p (k c) -> p k c", c=128), func=AF.Copy), r=p, w=yA)

                      chk("hg")
                      wrq, wrk = wget("rq"), wget("rk", keep=1)
                      wrv0, wrv1, wrg0, wrg1 = None, None, None, None
                      ar.reset(0)
                      qT = ar.alloc([4, 4, 128], BF16)
                      kT = ar.alloc([4, 4, 128], BF16)
                      qdT = ar.alloc([4, 4, 128], BF16)
                      kdr = ar.alloc([4, 4, 128], BF16)
                      v_r = ar.alloc([4, 1024], BF16)
                      gate_b = ar.alloc([4, 1024], BF16)
                      tA = ar.alloc([512], F32)
                      tB = ar.alloc([512], F32)
                      qk_r = [ar.alloc([512], BF16) for _ in range(2)]

                      def v4(ap):
                          return ap.rearrange("p (h two d) -> p h two d", two=2, d=64)
                      for j in range(4):
                          t = 4 * T + j
                          cosb = ropeR.t[:, t, 0, :].unsqueeze(1).unsqueeze(1).broadcast_to([128, 4, 2, 64])
                          sinb = ropeR.t[:, t, 1, :].unsqueeze(1).broadcast_to([128, 4, 64])
                          for qi, wblk in enumerate((wrq, wrk)):
                              curw[0] = wblk
                              p = nxt("mm")
                              proj_tok(wblk.t[:, 0:4096].rearrange("p (k c) -> p k c", c=512), 512, j, p)
                              dst = qk_r[qi]
                              kk.op("dve", lambda h, p=p: h.tensor_tensor(out=v4(tA.ap), in0=v4(p.t[:]), in1=cosb, op=ALU.mult), r=[p, ropeR], w=tA)
                              kk.op("dve", lambda h, p=p: h.tensor_tensor(out=v4(tB.ap)[:, :, 0, :], in0=v4(p.t[:])[:, :, 1, :], in1=sinb, op=ALU.mult), r=[p, ropeR], w=tB)
                              kk.op("dve", lambda h, p=p: h.tensor_tensor(out=v4(tB.ap)[:, :, 1, :], in0=v4(p.t[:])[:, :, 0, :], in1=sinb, op=ALU.mult), r=[p, ropeR], w=tB)
                              kk.op("dve", lambda h, dst=dst: h.tensor_tensor(out=v4(dst.ap)[:, :, 0, :], in0=v4(tA.ap)[:, :, 0, :], in1=v4(tB.ap)[:, :, 0, :], op=ALU.subtract), r=[tA, tB], w=dst)
                              kk.op("dve", lambda h, dst=dst: h.tensor_tensor(out=v4(dst.ap)[:, :, 1, :], in0=v4(tA.ap)[:, :, 1, :], in1=v4(tB.ap)[:, :, 1, :], op=ALU.add), r=[tA, tB], w=dst)
                          chk("ret1")
                          q_r, k_r = qk_r
                          kk.op("dve", lambda h, j=j: h.tensor_tensor(out=kdr.ap[:, j], in0=k_r.ap.rearrange("p (h d) -> p h d", d=128), in1=kdecc.t[:].unsqueeze(2).broadcast_to([128, 4, 128]), op=ALU.mult), r=[k_r, kdecc], w=kdr)
                          p = nxt("T")
                          for hd in range(4):
                              kk.op("pe", lambda h, hd=hd, p=p: h.transpose(out=p.t[:, hd * 128:(hd + 1) * 128], in_=q_r.ap[:, hd * 128:(hd + 1) * 128], identity=ident.t[:]), r=[q_r, ident], w=p, inc=False)
                          for hd in range(4):
                              kk.op("pe", lambda h, hd=hd, p=p: h.transpose(out=p.t[:, 512 + hd * 128:512 + (hd + 1) * 128], in_=k_r.ap[:, hd * 128:(hd + 1) * 128], identity=ident.t[:]), r=[k_r, ident], w=p, inc=(hd == 3))
                          chk("ret1b")
                          kk.op("act", lambda h, j=j, p=p: h.activation(out=qT.ap[:, j], in_=p.t[:, 0:512].rearrange("p (h t) -> p h t", t=128), func=AF.Copy), r=p, w=qT)
                          kk.op("act", lambda h, j=j, p=p: h.activation(out=kT.ap[:, j], in_=p.t[:, 512:1024].rearrange("p (h t) -> p h t", t=128), func=AF.Copy), r=p, w=kT)
                          chk("ret1c")
                          kk.op("dve", lambda h, j=j, p=p: h.tensor_tensor(out=qdT.ap[:, j], in0=p.t[:, 0:512].rearrange("p (h t) -> p h t", t=128), in1=qdec.t[:], op=ALU.mult), r=[p, qdec], w=qdT)
                          chk("ret1d")
                          if j == 1:
                              chk("ret1e")
                      chk("ret2")
                      wrv0, wrv1 = wget("rv0"), wget("rv1", keep=1)
                      for j in range(4):
                          for n_, wblk in enumerate((wrv0, wrv1)):
                              curw[0] = wblk
                              p = nxt("mm")
                              proj_tok(wblk.t[:, 0:4096].rearrange("p (k c) -> p k c", c=512), 512, j, p)
                              kk.op("act", lambda h, j=j, n_=n_, p=p: h.activation(out=v_r.ap[:, j, n_ * 512:(n_ + 1) * 512], in_=p.t[:], func=AF.Copy), r=p, w=v_r)
                      wrg0, wrg1 = wget("rg0"), wget("rg1", keep=1)
                      for j in range(4):
                          for n_, wblk in enumerate((wrg0, wrg1)):
                              curw[0] = wblk
                              p = nxt("mm")
                              proj_tok(wblk.t[:, 0:4096].rearrange("p (k c) -> p k c", c=512), 512, j, p)
                              kk.op("act", lambda h, p=p: h.activation(out=tA.ap, in_=p.t[:], func=AF.Sigmoid), r=p, w=tA)
                              kk.op("dve", lambda h, j=j, n_=n_, p=p: h.tensor_tensor(out=gate_b.ap[:, j, n_ * 512:(n_ + 1) * 512], in0=p.t[:], in1=tA.ap, op=ALU.mult), r=[p, tA], w=gate_b)
                      chk("ret3")
                      at_r = ar.alloc([4, 128], BF16)
                      y_bs = [ar.alloc([1024], BF16) for _ in range(2)]
                      junk = ar.alloc([256], F32)
                      pend_yT = [None]

                      def emit_yT(j, y_b):
                          p = nxt("T")
                          for kc in range(8):
                              kk.op("pe", lambda h, kc=kc, p=p: h.transpose(out=p.t[:, kc * 128:(kc + 1) * 128], in_=y_b.ap[:, kc * 128:(kc + 1) * 128], identity=ident.t[:]), r=[y_b, ident], w=p, inc=(kc == 7))
                          kk.op("act", lambda h, j=j, p=p: h.activation(out=big.t[:, 4:12, j * 128:(j + 1) * 128], in_=p.t[:].rearrange("p (k c) -> p k c", c=128), func=AF.Copy), r=p, w=yB)
                      g128 = [float(np.exp(128.0 * LOG_GAMMA[h_])) for h_ in range(4)]
                      for j in range(4):
                          pa = nxt("mm")
                          for hd in range(4):
                              kk.op("pe", lambda h, j=j, hd=hd, pa=pa: h.matmul(pa.t[:, hd * 128:(hd + 1) * 128], lhsT=kT.ap[:, j, hd, :], rhs=qT.ap[:, j, hd, :], start=True, stop=True),
                                    r=[kT, qT], w=pa, inc=(hd == 3))
                          kk.op("dve", lambda h, pa=pa: h.tensor_tensor(out=at_r.ap, in0=pa.t[:].rearrange("p (h t) -> p h t", t=128), in1=dmask.t[:], op=ALU.mult), r=[pa, dmask], w=at_r)
                          pos2 = [nxt("acc"), nxt("acc")]
                          ss = newcol(4)
                          for hd in range(4):
                              po = pos2[hd // 2]
                              oc = (hd % 2) * 256
                              kk.op("pe", lambda h, j=j, hd=hd, po=po, oc=oc: h.matmul(po.t[:, oc:oc + 256], lhsT=at_r.ap[:, hd, :], rhs=v_r.ap[:, j, hd * 256:(hd + 1) * 256], start=True, stop=False),
                                    r=[at_r, v_r], w=po, inc=False)
                              kk.op("pe", lambda h, j=j, hd=hd, po=po, oc=oc: h.matmul(po.t[:, oc:oc + 256], lhsT=qdT.ap[:, j, hd, :], rhs=sr_b.t[:, hd, :], start=False, stop=True),
                                    r=[qdT, sr_b.bufs[hd]], w=po, inc=True)
                              pSx = nxt("mm") if hd % 2 == 0 else pS
                              kk.op("pe", lambda h, j=j, hd=hd, pSx=pSx: h.matmul(pSx.t[:, 0:256], lhsT=kdr.ap[:, j, hd, :], rhs=v_r.ap[:, j, hd * 256:(hd + 1) * 256], start=True, stop=True),
                                    r=[kdr, v_r], w=pSx)
                              kk.op("dve", lambda h, hd=hd, pSx=pSx: h.scalar_tensor_tensor(out=sr_f.t[:, hd, :], in0=sr_f.t[:, hd, :], scalar=g128[hd], in1=pSx.t[:, 0:256], op0=ALU.mult, op1=ALU.add),
                                    r=[pSx, sr_f.bufs[hd]], w=sr_f.bufs[hd])
                              kk.op("act", lambda h, hd=hd: h.activation(out=sr_b.t[:, hd, :], in_=sr_f.t[:, hd, :], func=AF.Copy), r=sr_f.bufs[hd], w=sr_b.bufs[hd])
                              kk.op("act", lambda h, hd=hd, po=po, oc=oc, ss=ss: h.activation(out=junk.ap, in_=po.t[:, oc:oc + 256], func=AF.Square, accum_out=ss.ap[:, hd:hd + 1]), r=po, w=[junk, ss])
                          chk("ret4")
                          if pend_yT[0] is not None:
                              emit_yT(*pend_yT[0])
                          y_b = y_bs[j % 2]
                          rs = rstd_cols(ss, 4, 1.0 / 256)
                          for hd in range(4):
                              po = pos2[hd // 2]
                              oc = (hd % 2) * 256
                              kk.op("dve", lambda h, j=j, hd=hd, po=po, oc=oc, rs=rs, y_b=y_b: h.scalar_tensor_tensor(out=y_b.ap[:, hd * 256:(hd + 1) * 256], in0=po.t[:, oc:oc + 256], scalar=rs.ap[:, hd:hd + 1], in1=gate_b.ap[:, j, hd * 256:(hd + 1) * 256], op0=ALU.mult, op1=ALU.mult),
                                    r=[po, rs, gate_b], w=y_b)
                          pend_yT[0] = (j, y_b)
                      emit_yT(*pend_yT[0])

                      chk("ret")
                      wc = wget("c")
                      wm = wget("mla", keep=1)
                      ar.reset(0)
                      qnT = ar.alloc([2, 512], BF16)
                      qabsT = ar.alloc([4, 512], BF16)
                      qpeT = ar.alloc([4, 512], BF16, parts=64)
                      qn = ar.alloc([256], BF16)
                      kpe = ar.alloc([64], BF16)
                      junk = ar.alloc([256], F32)
                      tA = ar.alloc([256], F32)
                      tB = ar.alloc([256], F32)
                      qpe = ar.alloc([256], BF16)
                      qnopeT = ar.alloc([512], BF16)
                      wcv = wc.t[:, 0:8 * 448].rearrange("p (k c) -> p k c", c=448)
                      wuq = wm.t[:, 0:1536].rearrange("p (k c) -> p k c", c=768)
                      wukv = wm.t[:, 1536:2560]
                      wukT = wm.t[:, 2560:3072].rearrange("p (h c) -> p h c", c=128)

                      def rope_small(dst, src_ap, nh, t, rb, wb):
                          def v(ap):
                              return ap.rearrange("p (h two d) -> p h two d", two=2, d=32)
                          cosb = ropeM.t[:, t, 0, :].unsqueeze(1).unsqueeze(1).broadcast_to([128, nh, 2, 32])
                          sinb = ropeM.t[:, t, 1, :].unsqueeze(1).broadcast_to([128, nh, 32])
                          n = nh * 64
                          kk.op("dve", lambda h: h.tensor_tensor(out=v(tA.ap[:, 0:n]), in0=v(src_ap), in1=cosb, op=ALU.mult), r=[rb, ropeM], w=tA)
                          kk.op("dve", lambda h: h.tensor_tensor(out=v(tB.ap[:, 0:n])[:, :, 0, :], in0=v(src_ap)[:, :, 1, :], in1=sinb, op=ALU.mult), r=[rb, ropeM], w=tB)
                          kk.op("dve", lambda h: h.tensor_tensor(out=v(tB.ap[:, 0:n])[:, :, 1, :], in0=v(src_ap)[:, :, 0, :], in1=sinb, op=ALU.mult), r=[rb, ropeM], w=tB)
                          kk.op("dve", lambda h: h.tensor_tensor(out=v(dst)[:, :, 0, :], in0=v(tA.ap[:, 0:n])[:, :, 0, :], in1=v(tB.ap[:, 0:n])[:, :, 0, :], op=ALU.subtract), r=[tA, tB], w=wb)
                          kk.op("dve", lambda h: h.tensor_tensor(out=v(dst)[:, :, 1, :], in0=v(tA.ap[:, 0:n])[:, :, 1, :], in1=v(tB.ap[:, 0:n])[:, :, 1, :], op=ALU.add), r=[tA, tB], w=wb)

                      qns = [ar.alloc([256], BF16) for _ in range(4)]
                      kpes = [ar.alloc([64], BF16) for _ in range(4)]
                      curw[0] = wc
                      ps_c = [nxt("wide") for _ in range(4)]
                      for j in range(4):
                          proj_tok(wcv, 448, j, ps_c[j])
                      ssq, sskv = newcol(4), newcol(4)
                      for j in range(4):
                          p = ps_c[j]
                          kk.op("act", lambda h, p=p, j=j: h.activation(out=junk.ap, in_=p.t[:, 0:256], func=AF.Square, accum_out=ssq.ap[:, j:j + 1]), r=p, w=[junk, ssq])
                          kk.op("act", lambda h, p=p, j=j: h.activation(out=junk.ap[:, 0:128], in_=p.t[:, 256:384], func=AF.Square, accum_out=sskv.ap[:, j:j + 1]), r=p, w=[junk, sskv])
                      rsq = rstd_cols(ssq, 4, 1.0 / 256)
                      rskv = rstd_cols(sskv, 4, 1.0 / 128)
                      for j in range(4):
                          t = 4 * T + j
                          p = ps_c[j]
                          kk.op("dve", lambda h, p=p, j=j: h.tensor_scalar(out=qns[j].ap, in0=p.t[:, 0:256], scalar1=rsq.ap[:, j:j + 1], scalar2=None, op0=ALU.mult), r=[p, rsq], w=qns[j])
                          kk.op("dve", lambda h, p=p, j=j, t=t: h.tensor_scalar(out=kvtok.t[:, t, :], in0=p.t[:, 256:384], scalar1=rskv.ap[:, j:j + 1], scalar2=None, op0=ALU.mult), r=[p, rskv], w=kvtok.bufs[t])
                          rope_small(kpes[j].ap, p.t[:, 384:448], 1, t, p, kpes[j])
                      for j in range(4):
                          t = 4 * T + j
                          qn_, kpe_ = qns[j], kpes[j]
                          pt_ = nxt("T")
                          kk.op("pe", lambda h, pt_=pt_, qn_=qn_: h.transpose(out=pt_.t[:, 0:128], in_=qn_.ap[:, 0:128], identity=ident.t[:]), r=[qn_, ident], w=pt_, inc=False)
                          kk.op("pe", lambda h, pt_=pt_, qn_=qn_: h.transpose(out=pt_.t[:, 128:256], in_=qn_.ap[:, 128:256], identity=ident.t[:]), r=[qn_, ident], w=pt_, inc=False)
                          kk.op("pe", lambda h, pt_=pt_, t=t: h.transpose(out=pt_.t[:, 256:384], in_=kvtok.t[:, t, :], identity=ident.t[:]), r=[kvtok.bufs[t], ident], w=pt_, inc=False)
                          kk.op("pe", lambda h, pt_=pt_, kpe_=kpe_: h.transpose(out=pt_.t[0:64, 384:512], in_=kpe_.ap, identity=ident.t[:]), r=[kpe_, ident], w=pt_, inc=True)
                          kk.op("act", lambda h, j=j, pt_=pt_: h.activation(out=qnT.ap[:, :, j * 128:(j + 1) * 128], in_=pt_.t[:, 0:256].rearrange("p (k c) -> p k c", c=128), func=AF.Copy), r=pt_, w=qnT)
                          kk.op("act", lambda h, t=t, pt_=pt_: h.activation(out=kvnT.t[:, t * 128:(t + 1) * 128], in_=pt_.t[:, 256:384], func=AF.Copy), r=pt_, w=kvnT.bufs[t])
                          kk.op("act", lambda h, t=t, pt_=pt_: h.activation(out=kpeT.t[:, t * 128:(t + 1) * 128], in_=pt_.t[0:64, 384:512], func=AF.Copy), r=pt_, w=kpeT.bufs[t])
                      curw[0] = wm
                      for hd in range(4):
                          p = nxt("mm")
                          proj_feat(lambda kc, hd=hd: wuq[:, kc, hd * 192:hd * 192 + 128], p, nk=2, rhs_of_kc=lambda kc: qnT.ap[:, kc, :], rbufs=qnT)
                          kk.op("act", lambda h, p=p: h.activation(out=qnopeT.ap, in_=p.t[:], func=AF.Copy, scale=S192), r=p, w=qnopeT)
                          p2 = nxt("mm")
                          kk.op("pe", lambda h, hd=hd, p2=p2: h.matmul(p2.t[:], lhsT=wukT[:, hd, :], rhs=qnopeT.ap, start=True, stop=True), r=[wm, qnopeT], w=p2)
                          kk.op("act", lambda h, hd=hd, p2=p2: h.activation(out=qabsT.ap[:, hd, :], in_=p2.t[:], func=AF.Copy), r=p2, w=qabsT)
                      wuq_pe = wuq.rearrange("p k (h d) -> p k h d", d=192)[:, :, :, 128:192]
                      qpes = [ar.alloc([256], BF16) for _ in range(4)]
                      ps_q = [nxt("wide") for _ in range(4)]
                      for j in range(4):
                          p = ps_q[j]
                          for kc in range(2):
                              kk.op("pe", lambda h, kc=kc, j=j, p=p: h.matmul(p.t[:, 0:256].rearrange("p (h d) -> p h d", d=64), lhsT=qnT.ap[:, kc, j * 128:(j + 1) * 128], rhs=wuq_pe[:, kc], start=(kc == 0), stop=(kc == 1)),
                                    r=[qnT, wm], w=p, inc=(kc == 1))
                      for j in range(4):
                          rope_small(qpes[j].ap, ps_q[j].t[:, 0:256], 4, 4 * T + j, ps_q[j], qpes[j])
                      for j in range(4):
                          qpe_ = qpes[j]
                          pt_ = nxt("T")
                          for hd in range(4):
                              kk.op("pe", lambda h, hd=hd, pt_=pt_, qpe_=qpe_: h.transpose(out=pt_.t[0:64, hd * 128:(hd + 1) * 128], in_=qpe_.ap[:, hd * 64:(hd + 1) * 64], identity=ident.t[:]), r=[qpe_, ident], w=pt_, inc=(hd == 3))
                          kk.op("act", lambda h, j=j, pt_=pt_: h.activation(out=qpeT.ap[:, :, j * 128:(j + 1) * 128], in_=pt_.t[0:64, 0:512].rearrange("p (h c) -> p h c", c=128), func=AF.Copy, scale=S192), r=pt_, w=qpeT)
                      pTs = [ar.alloc([512], BF16, align=True) for _ in range(3)]
                      ar.alloc([0], BF16, align=True)
                      olT = ar.alloc([512], BF16)
                      rinv = ar.alloc([512], F32)
                      prr = 0
                      for hd in range(4):
                          po_l, po_s = nxt("acc"), nxt("acc")
                          njb = 4 * T + 4
                          def emit_st(jb, hd=hd):
                              c0 = max(0, jb - 4 * T) * 128
                              p = nxt("mm")
                              kk.op("pe", lambda h, jb=jb, hd=hd, c0=c0, p=p: h.matmul(p.t[:, c0:512], lhsT=kvnT.t[:, jb * 128:(jb + 1) * 128], rhs=qabsT.ap[:, hd, c0:512], start=True, stop=False),
                                    r=[kvnT.bufs[jb], qabsT], w=p, inc=False)
                              kk.op("pe", lambda h, jb=jb, hd=hd, c0=c0, p=p: h.matmul(p.t[:, c0:512], lhsT=kpeT.t[:, jb * 128:(jb + 1) * 128], rhs=qpeT.ap[:, hd, c0:512], start=False, stop=True),
                                    r=[kpeT.bufs[jb], qpeT], w=p, inc=True)
                              return p
                          stq = [emit_st(0)]
                          for jb in range(njb):
                              c0 = max(0, jb - 4 * T) * 128
                              if jb + 1 < njb:
                                  stq.append(emit_st(jb + 1))
                              p = stq.pop(0)
                              pT_ = pTs[prr % 3]
                              prr += 1
                              kk.op("act", lambda h, c0=c0, p=p, pT_=pT_: h.activation(out=pT_.ap[:, c0:512], in_=p.t[:, c0:512], func=AF.Exp), r=p, w=pT_)
                              if jb >= 4 * T:
                                  kk.op("pool", lambda h, c0=c0, pT_=pT_: h.tensor_tensor(out=pT_.ap[:, c0:c0 + 128], in0=pT_.ap[:, c0:c0 + 128], in1=causal.t[:], op=ALU.mult), r=[pT_, causal], w=pT_)
                              kk.op("pe", lambda h, jb=jb, c0=c0, pT_=pT_, po_l=po_l: h.matmul(po_l.t[:, c0:512], lhsT=kvtok.t[:, jb, :], rhs=pT_.ap[:, c0:512], start=(jb == 0), stop=(jb == njb - 1)),
                                    r=[kvtok.bufs[jb], pT_], w=po_l, inc=(jb == njb - 1))
                              kk.op("pe", lambda h, jb=jb, c0=c0, pT_=pT_, po_s=po_s: h.matmul(po_s.t[:, c0:512], lhsT=ones.t[:], rhs=pT_.ap[:, c0:512], start=(jb == 0), stop=(jb == njb - 1)),
                                    r=[ones, pT_], w=po_s, inc=True)
                          kk.op("act", lambda h, po_l=po_l: h.activation(out=olT.ap, in_=po_l.t[:], func=AF.Copy), r=po_l, w=olT)
                          kk.op("act", lambda h, po_s=po_s: h.activation(out=rinv.ap, in_=po_s.t[:], func=AF.Ln), r=po_s, w=rinv)
                          kk.op("act", lambda h: h.activation(out=rinv.ap, in_=rinv.ap, func=AF.Exp, scale=-1.0), r=rinv, w=rinv)
                          p = nxt("mm")
                          kk.op("pe", lambda h, hd=hd, p=p: h.matmul(p.t[:], lhsT=wukv[:, hd * 256 + 128:hd * 256 + 256], rhs=olT.ap, start=True, stop=True), r=[wm, olT], w=p)
                          kk.op("dve", lambda h, hd=hd, p=p: h.tensor_tensor(out=big.t[:, 12 + hd, :], in0=p.t[:], in1=rinv.ap, op=ALU.mult), r=[p, rinv], w=yC)

                      chk("mla")
                      ar.reset(0)
                      gs = [ar.alloc([512], F32) for _ in range(3)]
                      ts = [ar.alloc([512], F32) for _ in range(3)]
                      m1 = ar.alloc([512], F32)
                      for jc in range(8):
                          wM = wget(f"M{jc}")
                          curw[0] = wM
                          wg = wM.t[:, 0:3072].rearrange("p (k b c) -> p k b c", b=3, c=128)
                          wb = wM.t[:, 3072:5120].rearrange("p (k c) -> p k c", c=128)
                          for b_, (k0, nk, yb) in enumerate(((0, 4, yA), (4, 8, yB), (12, 4, yC))):
                              pg = nxt("wide")
                              proj_feat(lambda kc, b_=b_: wg[:, kc, b_, :], pg)
                              kk.op("act", lambda h, b_=b_, pg=pg: h.activation(out=gs[b_].ap, in_=pg.t[:], func=AF.Sigmoid), r=pg, w=gs[b_])
                              pp = nxt("wide")
                              proj_feat(lambda kc, k0=k0: wb[:, k0 + kc, :], pp, nk=nk, rhs_of_kc=lambda kc, k0=k0: big.t[:, k0 + kc, :], rbufs=yb)
                              kk.op("dve", lambda h, b_=b_, pp=pp: h.tensor_tensor(out=ts[b_].ap, in0=pp.t[:], in1=gs[b_].ap, op=ALU.mult), r=[pp, gs[b_]], w=ts[b_])
                          kk.op("pool", lambda h: h.tensor_tensor(out=m1.ap, in0=ts[0].ap, in1=ts[1].ap, op=ALU.add), r=[ts[0], ts[1]], w=m1)
                          kk.op("pool", lambda h, jc=jc: h.tensor_tensor(out=big.t[:, 16 + jc, :], in0=m1.ap, in1=ts[2].ap, op=ALU.add), r=[m1, ts[2]], w=mTb)
                      chk("merge")
                      for n_ in range(2):
                          wo = wget(f"wout{n_}")
                          wov = wo.t[:, 0:4096].rearrange("p (k c) -> p k c", c=512)
                          for j in range(4):
                              t = 4 * T + j
                              p = nxt("wide")
                              for kc in range(8):
                                  kk.op("pe", lambda h, kc=kc, j=j, p=p: h.matmul(p.t[:], lhsT=big.t[:, 16 + kc, j * 128:(j + 1) * 128], rhs=wov[:, kc, :], start=(kc == 0), stop=(kc == 7)),
                                        r=[mTb, wo], w=p, inc=(kc == 7))
                              kk.op("dve", lambda h, t=t, n_=n_, p=p: h.tensor_tensor(out=xres.t[:, t, n_ * 512:(n_ + 1) * 512], in0=xres.t[:, t, n_ * 512:(n_ + 1) * 512], in1=p.t[:], op=ALU.add),
                                    r=[p, xres.bufs[t]], w=xres.bufs[t])
                      chk("wout")
                      norm_to_hT(T)
                      ar.reset(5120)
                      sgs = [ar.alloc([512], F32) for _ in range(2)]
                      t1s = [ar.alloc([512], F32) for _ in range(2)]
                      for jb in range(11):
                          wf = wget(f"ffin{jb}")
                          curw[0] = wf
                          wfv = wf.t[:, 0:4096].rearrange("p (k c) -> p k c", c=512)
                          for fc in range(2):
                              ff = jb * 2 + fc
                              pg = nxt("wide")
                              proj_feat(lambda kc, fc=fc: wfv[:, kc, fc * 128:(fc + 1) * 128], pg)
                              pu = nxt("wide")
                              proj_feat(lambda kc, fc=fc: wfv[:, kc, 256 + fc * 128:256 + (fc + 1) * 128], pu)
                              sg, t1_ = sgs[ff % 2], t1s[ff % 2]
                              kk.op("act", lambda h, pg=pg, sg=sg: h.activation(out=sg.ap, in_=pg.t[:], func=AF.Sigmoid), r=pg, w=sg)
                              kk.op("dve", lambda h, pg=pg, sg=sg, t1_=t1_: h.tensor_tensor(out=t1_.ap, in0=pg.t[:], in1=sg.ap, op=ALU.mult), r=[pg, sg], w=t1_)
                              kk.op("dve", lambda h, pu=pu, t1_=t1_, ff=ff: h.tensor_tensor(out=big.t[:, ff, :], in0=pu.t[:], in1=t1_.ap, op=ALU.mult), r=[pu, t1_], w=big.bufs)
                      for n_ in range(2):
                          wparts = [wget(f"ffout{n_ * 3 + i}", keep=i) for i in range(2)]
                          for j in range(4):
                              pass
                          pj = [nxt("mm"), nxt("mm"), nxt("mm"), pS]
                          kcs = [(0, 8), (8, 8), (16, 6)]
                          for pi in range(3):
                              wpart = wparts[pi] if pi < 2 else wget(f"ffout{n_ * 3 + 2}", keep=2)
                              kc0, nk = kcs[pi]
                              wpv = wpart.t[:, 0:nk * 512].rearrange("p (k c) -> p k c", c=512)
                              for j in range(4):
                                  for kc in range(nk):
                                      g = kc0 + kc
                                      kk.op("pe", lambda h, g=g, kc=kc, j=j, wpv=wpv: h.matmul(pj[j].t[:], lhsT=big.t[:, g, j * 128:(j + 1) * 128], rhs=wpv[:, kc, :], start=(g == 0), stop=(g == 21)),
                                            r=[big.bufs, wpart], w=pj[j], inc=(kc == nk - 1))
                          for j in range(4):
                              t = 4 * T + j
                              kk.op("dve", lambda h, t=t, n_=n_, j=j: h.tensor_tensor(out=xres.t[:, t, n_ * 512:(n_ + 1) * 512], in0=xres.t[:, t, n_ * 512:(n_ + 1) * 512], in1=pj[j].t[:], op=ALU.add),
                                    r=[pj[j], xres.bufs[t]], w=xres.bufs[t])
              ar.reset(0)
              obuf = [ar.alloc([D], F32) for _ in range(2)]
              junk = ar.alloc([D], BF16)
              fnw = ar.alloc([D], F32)
              kk.dma("pool", fnwst, fnw.ap, fnw_d.partition_broadcast(128), w=fnw)
              for t in range(NT):
                  xb = xres.bufs[t]
                  ss = newcol(1)
                  kk.op("act", lambda h, t=t, ss=ss: h.activation(out=junk.ap, in_=xres.t[:, t, :], func=AF.Square, accum_out=ss.ap), r=xb, w=[junk, ss])
                  rs = rstd_cols(ss, 1, 1.0 / D)
                  ob = obuf[t % 2]
                  kk.op("dve", lambda h, t=t, rs=rs, ob=ob: h.scalar_tensor_tensor(out=ob.ap, in0=xres.t[:, t, :], scalar=rs.ap, in1=fnw.ap, op0=ALU.mult, op1=ALU.mult), r=[xb, rs, fnw], w=ob)
                  kk.dma("pool", osts[t % 2], out_d[s_, t * 128:(t + 1) * 128, :], ob.ap, r=ob)
        except _Stop:
            for t in range(NT):
                kk.dma("sp", osts[t % 2], out_d[0, t * 128:(t + 1) * 128, :], xres.t[:, t, :], r=xres.bufs[t])
        for ost in osts:
            kk.E["pool"].h.wait_ge(ost.sem, ost.cnt)
            kk.E["sp"].h.wait_ge(ost.sem, ost.cnt)
        build.info = dict(ninst=kk.ninst, nsem=kk.nsem, arena_hi=ar.hi, sbuf_left=nc.sbuf_bytes_remaining)
    return nc


def host_consts():
    bf = ml_dtypes.bfloat16
    c = {}
    c["c_ident"] = np.eye(128, dtype=np.float32).astype(bf)
    s = np.arange(128)[:, None] % 64
    t = np.arange(64)[None, :]
    c["c_hgmask"] = (s <= t).astype(np.float32).astype(bf)
    s = np.arange(128)[:, None]
    t = np.arange(128)[None, :]
    c["c_causal"] = (s <= t).astype(np.float32).astype(bf)
    c["c_ones"] = np.ones((128, 128), np.float32).astype(bf)
    r = np.ones((128, 512), np.float32)
    r[:, ::64] = 0.0
    c["c_reset"] = r
    lg = np.array(LOG_GAMMA, np.float64)
    dm = np.zeros((128, 4, 128), np.float64)
    for h in range(4):
        dm[:, h, :] = np.where(s <= t, np.exp((t - s) * lg[h]), 0.0) * (128.0 ** -0.5)
    c["c_dmask"] = dm.astype(np.float32)
    qd = np.zeros((128, 4, 128), np.float64)
    for h in range(4):
        qd[:, h, :] = np.exp((np.arange(128)[None, :] + 1.0) * lg[h])
    c["c_qdec"] = qd.astype(np.float32)
    kd = np.zeros((128, 4), np.float64)
    for h in range(4):
        kd[:, h] = np.exp((127.0 - np.arange(128)) * lg[h]) * (128.0 ** -0.5)
    c["c_kdec"] = kd.astype(np.float32)
    c["c_invr"] = np.broadcast_to((10000.0 ** (-np.arange(64, dtype=np.float32) / 64)).astype(np.float32), (128, 64)).copy()
    c["c_invm"] = np.broadcast_to((10000.0 ** (-np.arange(32, dtype=np.float32) / 32)).astype(np.float32), (128, 32)).copy()
    return c


_NC_CACHE = {}


def kernel(**inputs):
    ncores = 8
    B = inputs["x"].shape[0]
    nseq = B // ncores
    key = (nseq, 2)
    if key not in _NC_CACHE:
        _NC_CACHE[key] = build(nseq=nseq, nlayers=2)
    nc = _NC_CACHE[key]
    consts = host_consts()
    in_maps = []
    for c in range(ncores):
        m = {}
        for k, v in inputs.items():
            v = np.asarray(v)
            if k in ("x", "positions"):
                m[k] = np.ascontiguousarray(v[c * nseq:(c + 1) * nseq])
            else:
                m[k] = np.ascontiguousarray(v)
        m.update(consts)
        in_maps.append(m)
    res = run_bass_kernel_spmd(nc, in_maps, core_ids=list(range(ncores)))
    out = np.concatenate([np.asarray(r["out"]) for r in res.results], axis=0)
    return out.astype(np.float32)
```
